# Optimizing a Trainium2 kernel written in Bass

```python
import math
import numpy as np
import jax
import jax.numpy as jnp
from jax import lax

D_MODEL = 1024
BATCH = 8
SEQ = 8192
DEPTH = 4

GRID_W = 64
CTX_LEN = 256
N_MIXERS = 3
FF_DIM = 4 * D_MODEL
NORM_EPS = 1e-6
GM_CHUNK = 128
GM_HALF = 3 * D_MODEL
GM_GROUPS = 8
NA_HEAD_DIM = 64
NA_HEADS = D_MODEL // NA_HEAD_DIM
NA_KH_MAX = 8
NA_KW = 16
NA_QBLOCK = 16
NA_KSPAN = 2 * NA_QBLOCK
ML_HEADS = 8
ML_V_DIM = D_MODEL // ML_HEADS
ML_QK_DIM = ML_V_DIM // 2
ML_INNER = ML_HEADS * ML_V_DIM
ML_CHUNK = 64
ROPE_BASE = 10000.0
N_A = (DEPTH + 2) // 3
N_B = (DEPTH + 1) // 3
N_C = DEPTH // 3

kernel_name = 'hybrid_gmlp_natten_mlstm_prefix_dit'


def rmsnorm(x, g):
    x32 = x.astype(jnp.float32)
    y = x32 * lax.rsqrt(jnp.mean(x32 * x32, axis=-1, keepdims=True) + NORM_EPS)
    return (y * g.astype(jnp.float32)).astype(x.dtype)


def layernorm(x, g):
    x32 = x.astype(jnp.float32)
    xc = x32 - jnp.mean(x32, axis=-1, keepdims=True)
    y = xc * lax.rsqrt(jnp.mean(xc * xc, axis=-1, keepdims=True) + NORM_EPS)
    return (y * g.astype(jnp.float32)).astype(x.dtype)


def sqrelu_mlp(h, w1, w2):
    return jnp.square(jax.nn.relu(h @ w1)) @ w2


def chunk_gmlp(h, w_in, b_in, ln_g, w_s, b_s, w_out):
    bsz, t, _ = h.shape
    z = jax.nn.gelu(h @ w_in + b_in)
    u, v = jnp.split(z, 2, axis=-1)
    v = layernorm(v, ln_g)
    v = v.reshape(bsz, t // GM_CHUNK, GM_CHUNK, GM_GROUPS, GM_HALF // GM_GROUPS)
    v = jnp.einsum('gpq,bnqgc->bnpgc', w_s, v) + b_s.T[None, None, :, :, None]
    return (u * v.reshape(bsz, t, GM_HALF)) @ w_out


def na_tables():
    cols = np.arange(GRID_W)
    win_start = np.clip(cols - NA_KW // 2, 0, GRID_W - NA_KW)
    n_blk = GRID_W // NA_QBLOCK
    blk_start = np.clip(win_start[::NA_QBLOCK], 0, GRID_W - NA_KSPAN)
    key_cols = blk_start[:, None] + np.arange(NA_KSPAN)[None, :]
    q_cols = cols.reshape(n_blk, NA_QBLOCK)
    ws = win_start.reshape(n_blk, NA_QBLOCK)[..., None]
    kc = key_cols[:, None, :]
    valid = (kc >= ws) & (kc < ws + NA_KW)
    dcol = np.clip(kc - q_cols[..., None], -(NA_KW - 1), NA_KW - 1) + NA_KW - 1
    return key_cols, valid, dcol


def neighbourhood_attention(h, hc, w_qkv, b_qkv, rpb, w_o, b_o, need_ctx):
    bsz, t, _ = h.shape
    lc = hc.shape[1]
    rows = t // GRID_W
    kh = min(NA_KH_MAX, rows)
    scale = NA_HEAD_DIM ** -0.5
    n_blk = GRID_W // NA_QBLOCK
    qkv = (h @ w_qkv + b_qkv).reshape(bsz, rows, GRID_W, 3, NA_HEADS, NA_HEAD_DIM)
    qkv = jnp.transpose(qkv, (3, 0, 4, 1, 2, 5))
    q, k, v = qkv[0], qkv[1], qkv[2]
    qkv_c = jnp.transpose((hc @ w_qkv + b_qkv).reshape(bsz, lc, 3, NA_HEADS, NA_HEAD_DIM), (2, 0, 3, 1, 4))
    qc, kc, vc = qkv_c[0], qkv_c[1], qkv_c[2]
    key_cols, valid, dcol = na_tables()
    mask = jnp.asarray(valid)[:, :, None, :]
    n_lat = kh * NA_KSPAN

    def row_step(r):
        rs = jnp.clip(r - kh // 2, 0, rows - kh)
        q_r = lax.dynamic_index_in_dim(q, r, axis=2, keepdims=False)
        q_r = q_r.reshape(bsz, NA_HEADS, n_blk, NA_QBLOCK, NA_HEAD_DIM)
        k_b = lax.dynamic_slice_in_dim(k, rs, kh, axis=2)[:, :, :, key_cols]
        v_b = lax.dynamic_slice_in_dim(v, rs, kh, axis=2)[:, :, :, key_cols]
        drow = rs + jnp.arange(kh) - r + NA_KH_MAX - 1
        bias = jnp.transpose(rpb[:, drow][:, :, dcol], (0, 2, 3, 1, 4))
        s_lat = jnp.einsum('bhjqd,bhrjkd->bhjqrk', q_r, k_b).astype(jnp.float32) * scale + bias.astype(jnp.float32)
        s_lat = jnp.where(mask, s_lat, jnp.float32(-1e30)).reshape(bsz, NA_HEADS, n_blk, NA_QBLOCK, n_lat)
        s_ctx = jnp.einsum('bhjqd,bhcd->bhjqc', q_r, kc).astype(jnp.float32) * scale
        p = jax.nn.softmax(jnp.concatenate([s_lat, s_ctx], axis=-1), axis=-1).astype(v.dtype)
        p_lat = p[..., :n_lat].reshape(bsz, NA_HEADS, n_blk, NA_QBLOCK, kh, NA_KSPAN)
        o = jnp.einsum('bhjqrk,bhrjkd->bhjqd', p_lat, v_b) + jnp.einsum('bhjqc,bhcd->bhjqd', p[..., n_lat:], vc)
        return o.reshape(bsz, NA_HEADS, GRID_W, NA_HEAD_DIM)

    out = lax.map(row_step, jnp.arange(rows))
    y = jnp.transpose(out, (1, 0, 3, 2, 4)).reshape(bsz, t, D_MODEL) @ w_o + b_o
    yc = None
    if need_ctx:
        sc = jnp.einsum('bhqd,bhkd->bhqk', qc, kc).astype(jnp.float32) * scale
        pc = jax.nn.softmax(sc, axis=-1).astype(vc.dtype)
        yc = jnp.einsum('bhqk,bhkd->bqhd', pc, vc).reshape(bsz, lc, D_MODEL) @ w_o + b_o
    return y, yc


def axial_rope(x):
    t = x.shape[1]
    pos = jnp.arange(t)
    row = (pos // GRID_W).astype(jnp.float32)
    col = (pos % GRID_W).astype(jnp.float32)
    d_axis = x.shape[-1] // 2
    inv = ROPE_BASE ** (-jnp.arange(0, d_axis, 2, dtype=jnp.float32) / d_axis)

    def rot(xa, p):
        ang = p[:, None] * inv[None, :]
        cos = jnp.cos(ang)[None, :, None, :]
        sin = jnp.sin(ang)[None, :, None, :]
        x1, x2 = jnp.split(xa, 2, axis=-1)
        return jnp.concatenate([x1 * cos - x2 * sin, x1 * sin + x2 * cos], axis=-1)

    x32 = x.astype(jnp.float32)
    return jnp.concatenate([rot(x32[..., :d_axis], row), rot(x32[..., d_axis:], col)], axis=-1).astype(x.dtype)


def mlstm_scan(q, k, v, ig, lf, state):
    bsz, nh, t, _ = q.shape
    dv = v.shape[-1]
    nc = t // ML_CHUNK
    tri = jnp.tril(jnp.ones((ML_CHUNK, ML_CHUNK), dtype=bool))

    def to_chunks(a):
        return jnp.moveaxis(a.reshape(bsz, nh, nc, ML_CHUNK, *a.shape[3:]), 2, 0)

    def step(carry, inp):
        c_st, n_st, m_st = carry
        qc, kc, vc, ic, fc = inp
        b = jnp.cumsum(fc, axis=-1)
        dmat = jnp.where(tri, b[..., :, None] - b[..., None, :] + ic[..., None, :], -jnp.inf)
        g = b + m_st[..., None]
        m_t = jnp.maximum(g, jnp.max(dmat, axis=-1))
        a = jnp.einsum('bhtd,bhsd->bhts', qc, kc) * jnp.exp(dmat - m_t[..., None])
        inter = jnp.exp(g - m_t)
        num = inter[..., None] * jnp.einsum('bhtd,bhde->bhte', qc, c_st) + jnp.einsum('bhts,bhse->bhte', a, vc)
        den = inter * jnp.einsum('bhtd,bhd->bht', qc, n_st) + jnp.sum(a, axis=-1)
        h = num / jnp.maximum(jnp.abs(den), jnp.exp(-m_t))[..., None]
        b_last = b[..., -1]
        a_s = b_last[..., None] - b + ic
        m_new = jnp.maximum(b_last + m_st, jnp.max(a_s, axis=-1))
        w_s = jnp.exp(a_s - m_new[..., None])
        decay = jnp.exp(b_last + m_st - m_new)
        c_new = decay[..., None, None] * c_st + jnp.einsum('bhs,bhsd,bhse->bhde', w_s, kc, vc)
        n_new = decay[..., None] * n_st + jnp.einsum('bhs,bhsd->bhd', w_s, kc)
        return (c_new, n_new, m_new), h

    state, h = lax.scan(step, state, (to_chunks(q), to_chunks(k), to_chunks(v), to_chunks(ig), to_chunks(lf)))
    return jnp.moveaxis(h, 0, 2).reshape(bsz, nh, t, dv), state


def mlstm_mixer(h, hc, w_in, w_gate, b_gate, norm_g, w_out, need_ctx):
    qk_w = ML_HEADS * ML_QK_DIM

    def heads(a):
        return jnp.transpose(a, (0, 2, 1, 3)).astype(jnp.float32)

    def project(z, rope):
        bsz, t, _ = z.shape
        p = z @ w_in
        q = p[..., :qk_w].reshape(bsz, t, ML_HEADS, ML_QK_DIM)
        k = p[..., qk_w:2 * qk_w].reshape(bsz, t, ML_HEADS, ML_QK_DIM)
        v = p[..., 2 * qk_w:2 * qk_w + ML_INNER].reshape(bsz, t, ML_HEADS, ML_V_DIM)
        o = p[..., 2 * qk_w + ML_INNER:]
        if rope:
            q, k = axial_rope(q), axial_rope(k)
        g = jnp.einsum('btd,zdg->zbgt', z, w_gate).astype(jnp.float32) + b_gate.astype(jnp.float32)[:, None, :, None]
        ig = g[:, :, :ML_HEADS]
        lf = jax.nn.log_sigmoid(g[:, :, ML_HEADS:])
        return heads(q), heads(k) * (ML_QK_DIM ** -0.5), heads(v), o, ig, lf

    def readout(hh, o):
        bsz, _, t, _ = hh.shape
        hh = hh * lax.rsqrt(jnp.mean(hh * hh, axis=-1, keepdims=True) + NORM_EPS)
        hh = jnp.transpose(hh, (0, 2, 1, 3)).reshape(bsz, t, ML_INNER) * norm_g.astype(jnp.float32)
        return (hh.astype(o.dtype) * jax.nn.sigmoid(o)) @ w_out

    def flip(a):
        return jnp.flip(a, axis=2)

    bsz = h.shape[0]
    q, k, v, o, ig, lf = project(h, True)
    qc, kc, vc, oc, igc, lfc = project(hc, False)
    zero = (jnp.zeros((bsz, ML_HEADS, ML_QK_DIM, ML_V_DIM), jnp.float32),
            jnp.zeros((bsz, ML_HEADS, ML_QK_DIM), jnp.float32),
            jnp.zeros((bsz, ML_HEADS), jnp.float32))
    hc_f, st_f = mlstm_scan(qc, kc, vc, igc[0], lfc[0], zero)
    h_f, _ = mlstm_scan(q, k, v, ig[0], lf[0], st_f)
    hc_b, st_b = mlstm_scan(flip(qc), flip(kc), flip(vc), flip(igc[1]), flip(lfc[1]), zero)
    h_b, _ = mlstm_scan(flip(q), flip(k), flip(v), flip(ig[1]), flip(lf[1]), st_b)
    y = readout(h_f + flip(h_b), o)
    yc = readout(hc_f + flip(hc_b), oc) if need_ctx else None
    return y, yc


def setup_inputs(seed: int = 0) -> dict:
    key = jax.random.key(seed)
    keys = jax.random.split(key, 28)

    def nrm(i, shape, scale):
        return jax.random.normal(keys[i], shape, jnp.float32) * scale

    D = D_MODEL
    qk_w = ML_HEADS * ML_QK_DIM
    forget_bias = jnp.linspace(3.0, 6.0, ML_HEADS, dtype=jnp.float32)[None, None, :]
    ml_b_gate = jnp.concatenate([nrm(22, (N_C, 2, ML_HEADS), 0.1),
                                 forget_bias + nrm(23, (N_C, 2, ML_HEADS), 0.1)], axis=-1)
    return {
        'x': nrm(0, (BATCH, SEQ, D), 1.0),
        'c': nrm(1, (BATCH, D), 1.0),
        'ctx': nrm(2, (BATCH, CTX_LEN, D), 1.0),
        'c_ctx': nrm(3, (D,), 1.0),
        'ada_w': nrm(4, (DEPTH, D, 6 * D), 0.5 * D ** -0.5),
        'ada_b': nrm(5, (DEPTH, 6 * D), 0.02),
        'norm_g': 1.0 + nrm(6, (DEPTH, 4, D), 0.05),
        'ffn_w1': nrm(7, (DEPTH, D, FF_DIM), D ** -0.5),
        'ffn_w2': nrm(8, (DEPTH, FF_DIM, D), FF_DIM ** -0.5),
        'gm_w_in': nrm(9, (N_A, D, 2 * GM_HALF), D ** -0.5),
        'gm_b_in': nrm(10, (N_A, 2 * GM_HALF), 0.02),
        'gm_ln_g': 1.0 + nrm(11, (N_A, GM_HALF), 0.05),
        'gm_ws': nrm(12, (N_A, GM_GROUPS, GM_CHUNK, GM_CHUNK), GM_CHUNK ** -0.5),
        'gm_bs': 1.0 + nrm(13, (N_A, GM_GROUPS, GM_CHUNK), 0.1),
        'gm_w_out': nrm(14, (N_A, GM_HALF, D), GM_HALF ** -0.5),
        'na_w_qkv': nrm(15, (N_B, D, 3 * D), D ** -0.5),
        'na_b_qkv': nrm(16, (N_B, 3 * D), 0.02),
        'na_rpb': nrm(17, (N_B, NA_HEADS, 2 * NA_KH_MAX - 1, 2 * NA_KW - 1), 0.1),
        'na_w_o': nrm(18, (N_B, D, D), D ** -0.5),
        'na_b_o': nrm(19, (N_B, D), 0.02),
        'ml_w_in': nrm(20, (N_C, D, 2 * qk_w + 2 * ML_INNER), D ** -0.5),
        'ml_w_gate': nrm(21, (N_C, 2, D, 2 * ML_HEADS), D ** -0.5),
        'ml_b_gate': ml_b_gate,
        'ml_norm_g': 1.0 + nrm(24, (N_C, ML_INNER), 0.05),
        'ml_w_out': nrm(25, (N_C, ML_INNER, D), ML_INNER ** -0.5),
    }


def reference(x, c, ctx, c_ctx, ada_w, ada_b, norm_g, ffn_w1, ffn_w2,
              gm_w_in, gm_b_in, gm_ln_g, gm_ws, gm_bs, gm_w_out,
              na_w_qkv, na_b_qkv, na_rpb, na_w_o, na_b_o,
              ml_w_in, ml_w_gate, ml_b_gate, ml_norm_g, ml_w_out):
    xc = ctx
    c_act = jax.nn.silu(c)
    cc_act = jax.nn.silu(c_ctx)
    for i in range(DEPTH):
        kind, j = i % N_MIXERS, i // N_MIXERS
        need_ctx = i < DEPTH - 1
        mod = [m[:, None, :] for m in jnp.split(c_act @ ada_w[i] + ada_b[i], 6, axis=-1)]
        modc = jnp.split(cc_act @ ada_w[i] + ada_b[i], 6, axis=-1)
        h = rmsnorm(x, norm_g[i, 0]) * (1.0 + mod[1]) + mod[0]
        hc = rmsnorm(xc, norm_g[i, 0]) * (1.0 + modc[1]) + modc[0]
        if kind == 0:
            y = chunk_gmlp(h, gm_w_in[j], gm_b_in[j], gm_ln_g[j], gm_ws[j], gm_bs[j], gm_w_out[j])
            yc = chunk_gmlp(hc, gm_w_in[j], gm_b_in[j], gm_ln_g[j], gm_ws[j], gm_bs[j], gm_w_out[j]) if need_ctx else None
        elif kind == 1:
            y, yc = neighbourhood_attention(h, hc, na_w_qkv[j], na_b_qkv[j], na_rpb[j], na_w_o[j], na_b_o[j], need_ctx)
        else:
            y, yc = mlstm_mixer(h, hc, ml_w_in[j], ml_w_gate[j], ml_b_gate[j], ml_norm_g[j], ml_w_out[j], need_ctx)
        x = x + mod[2] * rmsnorm(y, norm_g[i, 1])
        h = rmsnorm(x, norm_g[i, 2]) * (1.0 + mod[4]) + mod[3]
        x = x + mod[5] * rmsnorm(sqrelu_mlp(h, ffn_w1[i], ffn_w2[i]), norm_g[i, 3])
        if need_ctx:
            xc = xc + modc[2] * rmsnorm(yc, norm_g[i, 1])
            hc = rmsnorm(xc, norm_g[i, 2]) * (1.0 + modc[4]) + modc[3]
            xc = xc + modc[5] * rmsnorm(sqrelu_mlp(hc, ffn_w1[i], ffn_w2[i]), norm_g[i, 3])
    return x
```

```python
import math
import numpy as np
import concourse.bass as bass
import concourse.mybir as mybir
from concourse.bass_utils import run_bass_kernel_spmd
from contextlib import ExitStack

F32 = mybir.dt.float32
BF16 = mybir.dt.bfloat16
U8 = mybir.dt.uint8
AF = mybir.ActivationFunctionType
ALU = mybir.AluOpType

ENGS = ['pe', 'act', 'dve', 'pool', 'sp']
NSLOT = 12
SAME_ENGINE_SYNC = False

D = 1024
KC = 8
NL = 8192
NCX = 256
NT = NL + NCX
EPS = 1e-6
ARENA_BYTES = 204 * 1024


class Buf:
    __slots__ = ('name', 'lw', 'rd')

    def __init__(self, name=''):
        self.name = name
        self.lw = None
        self.rd = {}


def bufs(n, name=''):
    return [Buf(name + str(i)) for i in range(n)]


class KB:
    def __init__(self, nc):
        self.nc = nc
        self.streams = {e: [] for e in ENGS}
        self.seen = {e: {} for e in ENGS}
        self.dseen = {e: set() for e in ENGS}
        self.slot_rr = {e: 0 for e in ENGS}
        self.slot_cnt = {e: [0] * NSLOT for e in ENGS}
        self.slot_last = {e: [None] * NSLOT for e in ENGS}
        self.n_ops = 0

    def _collect(self, eng, reads, writes, strict=False):
        deps_e = {}
        deps_d = set()

        def addtok(t):
            if t is None:
                return
            if t[0] == 'e':
                if t[2] > deps_e.get(t[1], -1):
                    deps_e[t[1]] = t[2]
            else:
                deps_d.add(t)

        for b in reads:
            addtok(b.lw)
        for b in writes:
            addtok(b.lw)
            for k, v in b.rd.items():
                if isinstance(k, tuple):
                    deps_d.add(k)
                else:
                    addtok(('e', k, v))
        waits = []
        for E, idx in deps_e.items():
            if E == eng and (eng == 'pe' or not (SAME_ENGINE_SYNC or strict)):
                continue
            if self.seen[eng].get(E, -1) >= idx:
                continue
            self.seen[eng][E] = idx
            self.streams[E][idx]['inc'] = True
            waits.append(('e', E, idx))
        for t in deps_d:
            if t in self.dseen[eng]:
                continue
            self.dseen[eng].add(t)
            waits.append(t)
        return waits

    def _commit(self, tok, reads, writes):
        for b in writes:
            b.lw = tok
            b.rd = {}
        for b in reads:
            if b in writes:
                continue
            if tok[0] == 'e':
                if b.rd.get(tok[1], -1) < tok[2]:
                    b.rd[tok[1]] = tok[2]
            else:
                b.rd[tok] = 1

    def op(self, eng, fn, reads=(), writes=(), strict=False):
        waits = self._collect(eng, reads, writes, strict)
        idx = len(self.streams[eng])
        self.streams[eng].append(dict(waits=waits, fn=fn, inc=False, dma=None))
        self._commit(('e', eng, idx), reads, writes)
        self.n_ops += 1

    def dma(self, q, out, in_, reads=(), writes=()):
        waits = self._collect(q, reads, writes)
        slot = self.slot_rr[q]
        self.slot_rr[q] = (slot + 1) % NSLOT
        prev = self.slot_last[q][slot]
        if prev is not None and prev not in self.dseen[q]:
            self.dseen[q].add(prev)
            waits.append(prev)
        self.slot_cnt[q][slot] += 1
        tok = ('d', q, slot, 16 * self.slot_cnt[q][slot])
        self.slot_last[q][slot] = tok
        self.streams[q].append(dict(waits=waits, fn=lambda e, o=out, i=in_: e.dma_start(out=o, in_=i),
                                    inc=False, dma=(q, slot)))
        self._commit(tok, reads, writes)
        self.n_ops += 1

    def barrier(self):
        lasts = {}
        for E in ENGS:
            for idx in range(len(self.streams[E]) - 1, -1, -1):
                if self.streams[E][idx]['fn'] is not None and self.streams[E][idx]['dma'] is None:
                    lasts[E] = idx
                    break
        dtoks = [t for q in ENGS for t in self.slot_last[q] if t is not None]
        for W in ENGS:
            waits = []
            for E, idx in lasts.items():
                if E == W:
                    continue
                if self.seen[W].get(E, -1) >= idx:
                    continue
                self.seen[W][E] = idx
                self.streams[E][idx]['inc'] = True
                waits.append(('e', E, idx))
            for t in dtoks:
                if t in self.dseen[W]:
                    continue
                self.dseen[W].add(t)
                waits.append(t)
            if waits:
                self.streams[W].append(dict(waits=waits, fn=None, inc=False, dma=None))

    def _pref(self):
        pref = {}
        for e in ENGS:
            c = 0
            arr = []
            for ent in self.streams[e]:
                if ent['inc']:
                    assert ent['dma'] is None and ent['fn'] is not None
                    c += 1
                arr.append(c)
            pref[e] = arr
        return pref

    def check(self):
        pref = self._pref()
        esem = {e: 0 for e in ENGS}
        dsem = {(q, i): 0 for q in ENGS for i in range(NSLOT)}
        pc = {e: 0 for e in ENGS}
        progress = True
        while progress:
            progress = False
            for e in ENGS:
                st = self.streams[e]
                while pc[e] < len(st):
                    ent = st[pc[e]]
                    ok = True
                    for w in ent['waits']:
                        if w[0] == 'e':
                            if esem[w[1]] < pref[w[1]][w[2]]:
                                ok = False
                        elif dsem[(w[1], w[2])] < w[3]:
                            ok = False
                    if not ok:
                        break
                    if ent['dma'] is not None:
                        dsem[ent['dma']] += 16
                    elif ent['inc']:
                        esem[e] += 1
                    pc[e] += 1
                    progress = True
        for e in ENGS:
            if pc[e] < len(self.streams[e]):
                raise RuntimeError(f"DEADLOCK: {e} stuck at {pc[e]}/{len(self.streams[e])}")

    def emit(self, es):
        self.check()
        nc = self.nc
        esem = {e: es.enter_context(nc.semaphore("s_" + e)) for e in ENGS}
        dsem = {q: [es.enter_context(nc.semaphore(f"d_{q}{i}")) for i in range(NSLOT)]
                for q in ENGS if any(self.slot_cnt[q])}
        pref = self._pref()
        block = es.enter_context(nc.Block())
        handles = {'pe': block.tensor, 'act': block.scalar, 'dve': block.vector, 'pool': block.gpsimd,
                   'sp': block.sync}
        for e in ENGS:
            stream = self.streams[e]
            if not stream:
                continue

            def body(h, stream=stream, e=e):
                for ent in stream:
                    for w in ent['waits']:
                        if w[0] == 'e':
                            h.wait_ge(esem[w[1]], pref[w[1]][w[2]])
                        else:
                            h.wait_ge(dsem[w[1]][w[2]], w[3])
                    if ent['fn'] is None:
                        continue
                    ins = ent['fn'](h)
                    if ent['dma'] is not None:
                        q, slot = ent['dma']
                        ins.then_inc(dsem[q][slot], 16)
                    elif ent['inc']:
                        ins.then_inc(esem[e], 1)
            handles[e](body)


class Ctx:
    def __init__(self, nc, es):
        self.nc = nc
        self.kb = KB(nc)
        self.arena = es.enter_context(nc.sbuf_tensor("arena", [128, ARENA_BYTES], U8))
        self.ps = es.enter_context(nc.psum_tensor("ps", [128, 4096], F32))
        self.off = 0
        self.persist_end = 0
        self.psb = bufs(8, 'ps')
        self.ps_rr = 0
        self.pool_rr = {}
        self.default_banks = list(range(8))
        self.tmp_rr = 0
        self.dram = {}

    def tile(self, free_shape, dt):
        n = int(np.prod(free_shape))
        esz = 4 if dt == F32 else 2
        nbytes = (n * esz + 31) // 32 * 32
        assert self.off + nbytes <= ARENA_BYTES, (self.off, nbytes)
        v = self.arena[:, self.off:self.off + n * esz].bitcast(dt)
        self.off += nbytes
        if len(free_shape) == 2:
            v = v.rearrange("p (a b) -> p a b", a=free_shape[0])
        elif len(free_shape) == 3:
            v = v.rearrange("p (a b c) -> p a b c", a=free_shape[0], b=free_shape[1])
        elif len(free_shape) == 4:
            v = v.rearrange("p (a b c d) -> p a b c d", a=free_shape[0], b=free_shape[1], c=free_shape[2])
        return v

    def end_persist(self):
        self.persist_end = self.off

    def new_phase(self):
        self.kb.barrier()
        self.off = self.persist_end
        self.psb = bufs(8, 'ps')
        self.default_banks = list(range(8))

    def psum(self, ncols, banks=None):
        assert ncols <= 512
        if banks is None:
            banks = self.default_banks
        key = tuple(banks)
        j = self.pool_rr.get(key, 0)
        self.pool_rr[key] = (j + 1) % len(banks)
        i = banks[j]
        return self.ps[:, i * 512:i * 512 + ncols], [self.psb[i]]


def mm(C, out, ob, lhsT, lb, rhs, rb, start, stop):
    C.kb.op('pe', lambda e: e.matmul(out, lhsT, rhs, start=start, stop=stop), reads=lb + rb, writes=ob)


def load_weight(C, dst, dbufs, src, kc_n, queue='pool'):
    sv = src.rearrange("(k p) n -> p k n", p=128)
    for k in range(kc_n):
        C.kb.dma(queue, dst[:, k, :], sv[:, k, :], writes=[dbufs[k]])


def phase_consts(C, T):
    kb = C.kb
    P = {}
    P['ones_bf'] = C.tile([128], BF16)
    P['ones_bf_b'] = Buf()
    P['one_row'] = C.tile([512], BF16)
    P['one_row_b'] = Buf()
    P['eps'] = C.tile([1], F32)
    P['eps_b'] = Buf()
    P['ident'] = C.tile([128], BF16)
    P['ident_b'] = Buf()
    kb.op('pool', lambda e: e.memset(P['ones_bf'], 1.0 / 1024), writes=[P['ones_bf_b']])
    kb.op('pool', lambda e: e.memset(P['one_row'], 1.0), writes=[P['one_row_b']])
    kb.op('pool', lambda e: e.memset(P['eps'], EPS), writes=[P['eps_b']])
    kb.op('pool', lambda e: e.memset(P['ident'], 0.0), writes=[P['ident_b']])
    kb.op('pool', lambda e: e.affine_select(out=P['ident'], in_=P['ident'], pattern=[[-1, 128]],
                                            compare_op=ALU.not_equal, fill=1.0, base=0, channel_multiplier=1),
          reads=[P['ident_b']], writes=[P['ident_b']])
    cv = C.tile([8, 2], F32)
    ca = C.tile([8, 2], F32)
    MOD = C.tile([4, 48, 2], F32)
    SC = C.tile([4, 6, 8, 2], F32)
    NG = C.tile([4, 4, 8], F32)
    AB = C.tile([4, 48], F32)
    P['SC'] = SC
    P['SC_b'] = Buf()
    C.end_persist()
    b_cv, b_ca, b_mod, b_ng, b_ab = Buf(), Buf(), Buf(), Buf(), Buf()
    kb.dma('sp', cv, T['cvec'][:, :, :], writes=[b_cv])
    kb.dma('sp', NG, T['norm_gT'][:, :, :, :], writes=[b_ng])
    kb.dma('sp', AB, T['ada_bT'][:, :, :], writes=[b_ab])
    kb.op('act', lambda e: e.activation(out=ca, in_=cv, func=AF.Silu), reads=[b_cv], writes=[b_ca])
    Wt = [C.tile([8, 768], F32) for _ in range(2)]
    Wb = bufs(2)
    it = 0
    for l in range(4):
        pst, pb = C.psum(96)
        for nb in range(8):
            w = Wt[it % 2]
            wb = Wb[it % 2]
            it += 1
            src = T['ada_w'][l].rearrange("(k p) n -> p k n", p=128)[:, :, nb * 768:(nb + 1) * 768]
            kb.dma('sp', w, src, writes=[wb])
            for j in range(6):
                n = nb * 6 + j
                for k in range(8):
                    mm(C, pst[:, 2 * n:2 * n + 2], pb, w[:, k, j * 128:(j + 1) * 128], [wb], ca[:, k, :], [b_ca],
                       k == 0, k == 7)
        pv = pst.rearrange("p (n w) -> p n w", w=2)
        kb.op('dve', lambda e, l=l, pv=pv: e.tensor_tensor(out=MOD[:, l], in0=pv,
                                                            in1=AB[:, l].unsqueeze(2).broadcast_to([128, 48, 2]),
                                                            op=ALU.add),
              reads=pb + [b_ab], writes=[b_mod], strict=True)

        def m(j6, l=l):
            return MOD[:, l, j6 * 8:(j6 + 1) * 8, :]

        def g(j, l=l):
            return NG[:, l, j, :].unsqueeze(2).broadcast_to([128, 8, 2])
        rd = [b_mod, b_ng]
        wr = [P['SC_b']]
        kb.op('dve', lambda e, l=l, m=m, g=g: e.scalar_tensor_tensor(out=SC[:, l, 0], in0=m(1), scalar=1.0, in1=g(0),
                                                                    op0=ALU.add, op1=ALU.mult), reads=rd, writes=wr, strict=True)
        kb.op('dve', lambda e, l=l, m=m: e.tensor_copy(out=SC[:, l, 1], in_=m(0)), reads=rd, writes=wr, strict=True)
        kb.op('dve', lambda e, l=l, m=m, g=g: e.tensor_tensor(out=SC[:, l, 2], in0=m(2), in1=g(1), op=ALU.mult),
              reads=rd, writes=wr, strict=True)
        kb.op('dve', lambda e, l=l, m=m, g=g: e.scalar_tensor_tensor(out=SC[:, l, 3], in0=m(4), scalar=1.0, in1=g(2),
                                                                    op0=ALU.add, op1=ALU.mult), reads=rd, writes=wr, strict=True)
        kb.op('dve', lambda e, l=l, m=m: e.tensor_copy(out=SC[:, l, 4], in_=m(3)), reads=rd, writes=wr, strict=True)
        kb.op('dve', lambda e, l=l, m=m, g=g: e.tensor_tensor(out=SC[:, l, 5], in0=m(5), in1=g(3), op=ALU.mult),
              reads=rd, writes=wr, strict=True)
    return P


def sc(P, l, kind, c, w):
    return P['SC'][:, l, kind, c, w:w + 1]


class Small:
    def __init__(self, C, n, free, dt):
        self.t = [C.tile(free, dt) for _ in range(n)]
        self.b = bufs(n)
        self.i = 0

    def get(self):
        i = self.i
        self.i = (i + 1) % len(self.t)
        return self.t[i], self.b[i]


def rstd_from_psum(C, P, pst, pb, RS):
    rs, rb = RS.get()
    n = pst.shape[-1]
    rs = rs[:, 0:n]
    C.kb.op('act', lambda e: e.activation(out=rs, in_=pst, func=AF.Sqrt, bias=P['eps'][:, 0:1]),
            reads=pb + [P['eps_b']], writes=[rb])
    C.kb.op('dve', lambda e: e.reciprocal(out=rs, in_=rs), reads=[rb], writes=[rb])
    return rs, rb


def prenorm(C, P, XL, XLb, n, H, Hb, l, kA, kB, w, SQ, RS, TMP):
    kb = C.kb
    pst, pb = C.psum(n)
    sq = []
    for c in range(8):
        s, sb_ = SQ.get()
        s = s[:, 0:n]
        kb.op('act', lambda e, s=s, c=c: e.activation(out=s, in_=XL[:, c, 0:n], func=AF.Square), reads=[XLb],
              writes=[sb_])
        sq.append((s, sb_))
        if c >= 1:
            s0, sb0 = sq[c - 1]
            mm(C, pst, pb, P['ones_bf'], [P['ones_bf_b']], s0, [sb0], c - 1 == 0, False)
    s0, sb0 = sq[7]
    mm(C, pst, pb, P['ones_bf'], [P['ones_bf_b']], s0, [sb0], False, True)
    rs, rb = rstd_from_psum(C, P, pst, pb, RS)
    for c in range(8):
        t, tb = TMP.get()
        t = t[:, 0:n]
        kb.op('dve', lambda e, t=t, c=c: e.scalar_tensor_tensor(out=t, in0=XL[:, c, 0:n], scalar=sc(P, l, kA, c, w),
                                                               in1=rs, op0=ALU.mult, op1=ALU.mult),
              reads=[XLb, rb, P['SC_b']], writes=[tb])
        kb.op('pool', lambda e, t=t, c=c: e.tensor_scalar(out=H[:, c, 0:n], in0=t, scalar1=sc(P, l, kB, c, w),
                                                         scalar2=None, op0=ALU.add),
              reads=[tb, P['SC_b']], writes=[Hb[c]])


class PostRes:
    def __init__(self, C, P, n, Yf, Yfb, SQ, RS, TMP, XR):
        self.C, self.P, self.n = C, P, n
        self.Yf, self.Yfb, self.SQ, self.RS, self.TMP, self.XR = Yf, Yfb, SQ, RS, TMP, XR
        self.pst, self.pb = C.psum(n)
        self.pending = None
        self.cnt = 0

    def _flush(self, last):
        if self.pending is not None:
            s0, sb0 = self.pending
            n = self.n
            for h in range(2):
                mm(self.C, self.pst, self.pb, self.P['ones_bf'], [self.P['ones_bf_b']], s0[:, h * n:(h + 1) * n], [sb0],
                   self.cnt == 0, last and h == 1)
                self.cnt += 1
            self.pending = None

    def pair(self, m, yp, ypb, bias=None, bias_b=None):
        C, n = self.C, self.n
        self._flush(False)
        s, sb_ = self.SQ.get()
        s = s[:, 0:2 * n]
        if bias is None:
            yv = yp.rearrange("p (a t) -> p a t", a=2)
            C.kb.op('act', lambda e: e.activation(out=self.Yf[:, m:m + 2, 0:n], in_=yv, func=AF.Identity), reads=ypb,
                    writes=[self.Yfb[m], self.Yfb[m + 1]])
            C.kb.op('act', lambda e: e.activation(out=s, in_=yp, func=AF.Square), reads=ypb, writes=[sb_])
        else:
            for h in range(2):
                C.kb.op('act', lambda e, h=h: e.activation(out=self.Yf[:, m + h, 0:n], in_=yp[:, h * n:(h + 1) * n],
                                                          func=AF.Identity, bias=bias[:, m + h:m + h + 1]),
                        reads=ypb + [bias_b], writes=[self.Yfb[m + h]])
                C.kb.op('act', lambda e, h=h: e.activation(out=s[:, h * n:(h + 1) * n], in_=yp[:, h * n:(h + 1) * n],
                                                          func=AF.Square, bias=bias[:, m + h:m + h + 1]),
                        reads=ypb + [bias_b], writes=[sb_])
        self.pending = (s, sb_)

    def finish(self, l, kG, w, src, dst, col0, srcb, dstb):
        C, P, n = self.C, self.P, self.n
        self._flush(True)
        rs, rb = rstd_from_psum(C, P, self.pst, self.pb, self.RS)
        for c in range(8):
            xr, xrb = self.XR.get()
            xr = xr[:, 0:n]
            C.kb.dma('sp', xr, src[c * 128:(c + 1) * 128, col0:col0 + n], reads=srcb, writes=[xrb])
            t, tb = self.TMP.get()
            t = t[:, 0:n]
            C.kb.op('dve', lambda e, t=t, c=c: e.scalar_tensor_tensor(out=t, in0=self.Yf[:, c, 0:n],
                                                                   scalar=sc(P, l, kG, c, w), in1=rs,
                                                                   op0=ALU.mult, op1=ALU.mult),
                    reads=[self.Yfb[c], rb, P['SC_b']], writes=[tb])
            C.kb.op('pool', lambda e, t=t, xr=xr: e.tensor_tensor(out=xr, in0=xr, in1=t, op=ALU.add),
                    reads=[tb, xrb], writes=[xrb])
            C.kb.dma('sp', dst[c * 128:(c + 1) * 128, col0:col0 + n], xr, reads=[xrb], writes=dstb)


MAXG = None
NA_PARTS = 3
STOP = 99
FULLBANK = False
SUB = 99


def groups(G, with_ctx=True, full=False):
    if MAXG is not None and not full:
        return [(g * G, G, 0) for g in range(MAXG)] + ([(NL, min(G, NCX), 1)] if with_ctx else [])
    out = []
    for g in range(NL // G):
        out.append((g * G, G, 0))
    if with_ctx:
        for g in range(NCX // min(G, NCX)):
            n = min(G, NCX)
            out.append((NL + g * n, n, 1))
    return out


def xbufs(T, key, col0, n):
    return T[key + '_b'][col0 // 128:(col0 + n) // 128]


def phase_ffn(C, P, T, l, src, dst, with_ctx, final=False):
    kb = C.kb
    C.new_phase()
    G = 256
    W1 = C.tile([8, 4096], BF16)
    W2 = C.tile([32, 1024], BF16)
    W1b, W2b = bufs(8), bufs(32)
    load_weight(C, W1, W1b, T['ffn_w1'][l], 8)
    load_weight(C, W2, W2b, T['ffn_w2'][l], 32)
    XL = [C.tile([8, G], F32) for _ in range(2)]
    XLb = bufs(2)
    H = C.tile([8, G], BF16)
    Hb = bufs(8)
    H1 = C.tile([32, G], BF16)
    H1b = bufs(32)
    Yf = C.tile([8, G], F32)
    Yfb = bufs(8)
    SQ = Small(C, 3, [2 * G], BF16)
    RS = Small(C, 2, [G], F32)
    TMP = Small(C, 4, [G], F32)
    XR = Small(C, 4, [G], F32)
    RL = Small(C, 3, [2 * G], F32)
    grp = groups(G, with_ctx)
    srcv = T[src].rearrange("(c p) t -> p c t", p=128)

    def load(i):
        col0, n, w = grp[i]
        kb.dma('sp', XL[i % 2][:, :, 0:n], srcv[:, :, col0:col0 + n], reads=xbufs(T, src, col0, n),
               writes=[XLb[i % 2]])
    load(0)
    for i, (col0, n, w) in enumerate(grp):
        if i + 1 < len(grp):
            load(i + 1)
        prenorm(C, P, XL[i % 2], XLb[i % 2], n, H, Hb, l, 3, 4, w, SQ, RS, TMP)
        for mp in range(16):
            pt, pb = C.psum(2 * n)
            for h in range(2):
                m = 2 * mp + h
                for k in range(8):
                    mm(C, pt[:, h * n:(h + 1) * n], pb, W1[:, k, m * 128:(m + 1) * 128], [W1b[k]], H[:, k, 0:n],
                       [Hb[k]], k == 0, k == 7)
            r, rb = RL.get()
            r = r[:, 0:2 * n]
            kb.op('act', lambda e, r=r, pt=pt: e.activation(out=r, in_=pt, func=AF.Relu), reads=pb, writes=[rb])
            rv = r.rearrange("p (a t) -> p a t", a=2)
            kb.op('pool', lambda e, rv=rv, mp=mp: e.tensor_tensor(out=H1[:, 2 * mp:2 * mp + 2, 0:n], in0=rv, in1=rv,
                                                                 op=ALU.mult),
                  reads=[rb], writes=[H1b[2 * mp], H1b[2 * mp + 1]])
        pr = PostRes(C, P, n, Yf, Yfb, SQ, RS, TMP, XR)
        for mp in range(4):
            pt, pb = C.psum(2 * n)
            for h in range(2):
                m = 2 * mp + h
                for k in range(32):
                    mm(C, pt[:, h * n:(h + 1) * n], pb, W2[:, k, m * 128:(m + 1) * 128], [W2b[k]], H1[:, k, 0:n],
                       [H1b[k]], k == 0, k == 31)
            pr.pair(2 * mp, pt, pb)
        if final and w == 0:
            pr.finish(l, 5, w, T[src], T['yout'], col0, xbufs(T, src, col0, n), xbufs(T, 'yout', col0, n))
        else:
            pr.finish(l, 5, w, T[src], T[dst], col0, xbufs(T, src, col0, n), xbufs(T, dst, col0, n))


GELU_A = math.sqrt(0.044715)
GELU_B = 2.0 * math.sqrt(2.0 / math.pi)


def gelu_psum(C, zp, zpb, out, outb, GT, a=1):
    kb = C.kb
    n = zp.shape[-1]
    t, tb = GT.get()
    t = t[:, 0:n]
    if a > 1:
        t = t.rearrange("p (a t) -> p a t", a=a)
        zp = zp.rearrange("p (a t) -> p a t", a=a)
    kb.op('act', lambda e: e.activation(out=t, in_=zp, func=AF.Square, scale=GELU_A), reads=zpb, writes=[tb])
    kb.op('dve', lambda e: e.scalar_tensor_tensor(out=t, in0=t, scalar=1.0, in1=zp, op0=ALU.add, op1=ALU.mult),
          reads=zpb + [tb], writes=[tb])
    kb.op('act', lambda e: e.activation(out=t, in_=t, func=AF.Sigmoid, scale=GELU_B), reads=[tb], writes=[tb])
    kb.op('dve', lambda e: e.tensor_tensor(out=out, in0=t, in1=zp, op=ALU.mult), reads=zpb + [tb], writes=outb)


def phase_gmlp_a(C, P, T, l, j, src, with_ctx):
    kb = C.kb
    C.new_phase()
    G = 256
    Win = C.tile([8, 6144], BF16)
    Winb = bufs(8)
    load_weight(C, Win, Winb, T['gm_w_in'][j], 8)
    brow = C.tile([6144], BF16)
    browb = Buf()
    kb.dma('pool', brow[0:1, :], T['gm_b_in'][j:j + 1, :], writes=[browb])
    wsT = C.tile([8, 128], BF16)
    wsTb = Buf()
    kb.dma('pool', wsT, T['gm_wsT'][j].rearrange("q (g p) -> q g p", g=8), writes=[wsTb])
    bsbc = C.tile([8, 128], F32)
    bsbcb = Buf()
    kb.dma('sp', bsbc, T['gm_bs'][j:j + 1, :].partition_broadcast(128).rearrange("q (g p) -> q g p", g=8)
           if False else T['gm_bs_bc'][j].rearrange("q (g p) -> q g p", g=8), writes=[bsbcb])
    lng = C.tile([24], F32)
    lngb = Buf()
    kb.dma('sp', lng, T['gm_ln_gT'][j], writes=[lngb])
    XL = [C.tile([8, G], F32) for _ in range(1)]
    XLb = bufs(1)
    H = C.tile([8, G], BF16)
    Hb = bufs(8)
    U = [C.tile([24, G], BF16) for _ in range(2)]
    Ub = [bufs(24), bufs(24)]
    ZG = [C.tile([3072], F32) for _ in range(1)]
    ZGb = [bufs(6) for _ in range(1)]
    VH = [C.tile([3072], BF16) for _ in range(2)]
    VHb = bufs(2)
    MV = Small(C, 2, [8], F32)
    SQ = Small(C, 3, [G], BF16)
    RS = Small(C, 2, [G], F32)
    TMP = Small(C, 3, [G], F32)
    GT = Small(C, 3, [512], F32)
    grp = groups(G, with_ctx)
    srcv = T[src].rearrange("(c p) t -> p c t", p=128)
    pdv = T['pd'].rearrange("(c p) t -> p c t", p=128)
    vit = 0
    for i, (col0, n, w) in enumerate(grp):
        kb.dma('sp', XL[0][:, :, 0:n], srcv[:, :, col0:col0 + n], reads=xbufs(T, src, col0, n), writes=[XLb[0]])
        prenorm(C, P, XL[0], XLb[0], n, H, Hb, l, 0, 1, w, SQ, RS, TMP)
        Ui, Ubi = U[i % 2], Ub[i % 2]
        vhs = []
        for s in range(n // 128):
            zg, zgb = ZG[0], ZGb[0]
            for q in range(6):
                pt, pb = C.psum(512)
                for k in range(8):
                    mm(C, pt, pb, H[:, k, s * 128:(s + 1) * 128], [Hb[k]],
                       Win[:, k, 3072 + q * 512:3072 + (q + 1) * 512], [Winb[k]], k == 0, False)
                mm(C, pt, pb, P['one_row'][0:1, 0:128], [P['one_row_b']],
                   brow[0:1, 3072 + q * 512:3072 + (q + 1) * 512], [browb], False, True)
                gelu_psum(C, pt, pb, zg[:, q * 512:(q + 1) * 512], [zgb[q]], GT)
            mv, mvb = MV.get()
            vh, vhb = VH[vit % 2], VHb[vit % 2]
            vit += 1
            kb.op('dve', lambda e, mv=mv, zg=zg: e.reduce_sum(out=mv[:, 0:1], in_=zg, axis=mybir.AxisListType.X),
                  reads=zgb, writes=[mvb], strict=True)
            kb.op('act', lambda e, mv=mv, zg=zg, vh=vh: e.activation(out=vh, in_=zg, func=AF.Square,
                                                                   accum_out=mv[:, 1:2]),
                  reads=zgb + [mvb], writes=[mvb, vhb], strict=True)
            kb.op('dve', lambda e, mv=mv: e.tensor_scalar(out=mv[:, 2:3], in0=mv[:, 0:1], scalar1=1.0 / 3072,
                                                          scalar2=None, op0=ALU.mult), reads=[mvb], writes=[mvb], strict=True)
            kb.op('dve', lambda e, mv=mv: e.scalar_tensor_tensor(out=mv[:, 3:4], in0=mv[:, 2:3], scalar=-1.0,
                                                                 in1=mv[:, 2:3], op0=ALU.mult, op1=ALU.mult),
                  reads=[mvb], writes=[mvb], strict=True)
            kb.op('dve', lambda e, mv=mv: e.scalar_tensor_tensor(out=mv[:, 4:5], in0=mv[:, 1:2], scalar=1.0 / 3072,
                                                                 in1=mv[:, 3:4], op0=ALU.mult, op1=ALU.add),
                  reads=[mvb], writes=[mvb], strict=True)
            kb.op('act', lambda e, mv=mv: e.activation(out=mv[:, 5:6], in_=mv[:, 4:5], func=AF.Sqrt,
                                                       bias=P['eps'][:, 0:1]), reads=[mvb, P['eps_b']], writes=[mvb], strict=True)
            kb.op('dve', lambda e, mv=mv: e.reciprocal(out=mv[:, 5:6], in_=mv[:, 5:6]), reads=[mvb], writes=[mvb], strict=True)
            kb.op('dve', lambda e, mv=mv: e.scalar_tensor_tensor(out=mv[:, 6:7], in0=mv[:, 2:3], scalar=-1.0,
                                                                 in1=mv[:, 5:6], op0=ALU.mult, op1=ALU.mult),
                  reads=[mvb], writes=[mvb], strict=True)
            kb.op('act', lambda e, vh=vh, zg=zg, mv=mv: e.activation(out=vh, in_=zg, func=AF.Identity,
                                                                   scale=mv[:, 5:6], bias=mv[:, 6:7]),
                  reads=zgb + [mvb], writes=[vhb])
            vhs.append((vh, vhb))
            if 'dbg_vh' in T and w == 1 and s == 1:
                kb.dma('sp', T['dbg_vh'][:, :], vh, reads=[vhb], writes=[Buf()])
                kb.dma('sp', T['dbg_mv'][:, :], mv, reads=[mvb], writes=[Buf()])
                kb.dma('sp', T['dbg_zg'][:, :], zg, reads=zgb, writes=[Buf()])
        for mp in range(12):
            pt, pb = C.psum(2 * n)
            for h in range(2):
                m = 2 * mp + h
                po = pt[:, h * n:(h + 1) * n]
                for k in range(8):
                    mm(C, po, pb, Win[:, k, m * 128:(m + 1) * 128], [Winb[k]], H[:, k, 0:n], [Hb[k]], k == 0, False)
                mm(C, po, pb, brow[0:1, m * 128:(m + 1) * 128], [browb], P['one_row'][0:1, 0:n], [P['one_row_b']],
                   False, True)
            gelu_psum(C, pt, pb, Ui[:, 2 * mp:2 * mp + 2, 0:n], [Ubi[2 * mp], Ubi[2 * mp + 1]], GT, a=2)
        for s in range(n // 128):
            vh, vhb = vhs[s]
            for c4 in range(6):
                pt, pb = C.psum(512)
                for jj in range(4):
                    cc = c4 * 4 + jj
                    mm(C, pt[:, jj * 128:(jj + 1) * 128], pb, vh[:, cc * 128:(cc + 1) * 128], [vhb],
                       wsT[:, cc // 3, :], [wsTb], True, True)
                t, tb = GT.get()
                for jj in range(4):
                    cc = c4 * 4 + jj
                    kb.op('dve', lambda e, t=t, pt=pt, cc=cc, jj=jj: e.scalar_tensor_tensor(
                        out=t[:, jj * 128:(jj + 1) * 128], in0=pt[:, jj * 128:(jj + 1) * 128],
                        scalar=lng[:, cc:cc + 1], in1=bsbc[:, cc // 3, :], op0=ALU.mult, op1=ALU.add),
                        reads=pb + [lngb, bsbcb], writes=[tb])
                tv = t.rearrange("p (a t) -> p a t", a=4)
                kb.op('pool', lambda e, tv=tv, c4=c4, s=s, Ui=Ui: e.tensor_tensor(
                    out=Ui[:, c4 * 4:c4 * 4 + 4, s * 128:(s + 1) * 128],
                    in0=Ui[:, c4 * 4:c4 * 4 + 4, s * 128:(s + 1) * 128], in1=tv, op=ALU.mult),
                    reads=[tb] + Ubi[c4 * 4:c4 * 4 + 4], writes=Ubi[c4 * 4:c4 * 4 + 4])
        kb.dma('sp', pdv[:, :, col0:col0 + n], Ui[:, :, 0:n], reads=Ubi, writes=xbufs(T, 'pd', col0, n))


def phase_gmlp_b(C, P, T, l, j, src, dst, with_ctx):
    kb = C.kb
    C.new_phase()
    G = 256
    Wo = C.tile([24, 1024], BF16)
    Wob = bufs(24)
    load_weight(C, Wo, Wob, T['gm_w_out'][j], 24)
    PT = [C.tile([24, G], BF16) for _ in range(2)]
    PTb = bufs(2)
    Yf = C.tile([8, G], F32)
    Yfb = bufs(8)
    SQ = Small(C, 3, [2 * G], BF16)
    RS = Small(C, 2, [G], F32)
    TMP = Small(C, 4, [G], F32)
    XR = Small(C, 4, [G], F32)
    grp = groups(G, with_ctx)
    pdv = T['pd'].rearrange("(c p) t -> p c t", p=128)

    def load(i):
        col0, n, w = grp[i]
        kb.dma('sp', PT[i % 2][:, :, 0:n], pdv[:, :, col0:col0 + n], reads=xbufs(T, 'pd', col0, n),
               writes=[PTb[i % 2]])
    load(0)
    for i, (col0, n, w) in enumerate(grp):
        if i + 1 < len(grp):
            load(i + 1)
        pr = PostRes(C, P, n, Yf, Yfb, SQ, RS, TMP, XR)
        for mp in range(4):
            pt, pb = C.psum(2 * n)
            for h in range(2):
                m = 2 * mp + h
                for k in range(24):
                    mm(C, pt[:, h * n:(h + 1) * n], pb, Wo[:, k, m * 128:(m + 1) * 128], [Wob[k]],
                       PT[i % 2][:, k, 0:n], [PTb[i % 2]], k == 0, k == 23)
            pr.pair(2 * mp, pt, pb)
        pr.finish(l, 2, w, T[src], T[dst], col0, xbufs(T, src, col0, n), xbufs(T, dst, col0, n))


def phase_na_a(C, P, T, l, src):
    kb = C.kb
    C.new_phase()
    G = 256
    W = C.tile([8, 3072], BF16)
    Wb = bufs(8)
    load_weight(C, W, Wb, T['na_w_qkv'][0], 8)
    bqk = C.tile([24], F32)
    bqkb = Buf()
    kb.dma('sp', bqk, T['na_bqkvT'][:, :], writes=[bqkb])
    bq8 = C.tile([8], F32)
    kb.op('dve', lambda e: e.tensor_scalar(out=bq8, in0=bqk[:, 0:8], scalar1=0.125, scalar2=None, op0=ALU.mult),
          reads=[bqkb], writes=[bqkb], strict=True)
    brow = C.tile([1024], BF16)
    browb = Buf()
    kb.dma('pool', brow[0:1, :], T['na_b_qkv'][0:1, 2048:3072], writes=[browb])
    XL = [C.tile([8, G], F32) for _ in range(2)]
    XLb = bufs(2)
    H = C.tile([8, G], BF16)
    Hb = bufs(8)
    QT = [C.tile([8, G], BF16) for _ in range(2)]
    QTb = bufs(2)
    KT = [C.tile([8, G], BF16) for _ in range(2)]
    KTb = bufs(2)
    Vt = [C.tile([16, 65], BF16) for _ in range(3)]
    Vtb = bufs(3)
    for i in range(3):
        kb.op('pool', lambda e, i=i: e.memset(Vt[i], 1.0), writes=[Vtb[i]])
    SQ = Small(C, 3, [G], BF16)
    RS = Small(C, 2, [G], F32)
    TMP = Small(C, 3, [G], F32)
    grp = groups(G, True, full=True)
    srcv = T[src].rearrange("(c p) t -> p c t", p=128)
    qv = T['qT'].rearrange("(c p) t -> p c t", p=128)
    kv = T['kT'].rearrange("(c p) t -> p c t", p=128)

    def load(i):
        col0, n, w = grp[i]
        kb.dma('sp', XL[i % 2][:, :, 0:n], srcv[:, :, col0:col0 + n], reads=xbufs(T, src, col0, n),
               writes=[XLb[i % 2]])
    load(0)
    vi = 0
    for i, (col0, n, w) in enumerate(grp):
        if i + 1 < len(grp):
            load(i + 1)
        prenorm(C, P, XL[i % 2], XLb[i % 2], n, H, Hb, l, 0, 1, w, SQ, RS, TMP)
        for which, dstT, dstb in ((0, QT[i % 2], QTb[i % 2]), (1, KT[i % 2], KTb[i % 2])):
            for mp in range(4):
                pt, pb = C.psum(2 * n)
                for h in range(2):
                    m = 2 * mp + h
                    mcol = which * 1024 + m * 128
                    for k in range(8):
                        mm(C, pt[:, h * n:(h + 1) * n], pb, W[:, k, mcol:mcol + 128], [Wb[k]], H[:, k, 0:n], [Hb[k]],
                           k == 0, k == 7)
                for h in range(2):
                    m = 2 * mp + h
                    if which == 0:
                        kb.op('act', lambda e, pt=pt, h=h, m=m, dstT=dstT: e.activation(
                            out=dstT[:, m, 0:n], in_=pt[:, h * n:(h + 1) * n], func=AF.Identity, scale=0.125,
                            bias=bq8[:, m:m + 1]), reads=pb + [bqkb], writes=[dstb])
                    else:
                        kb.op('act', lambda e, pt=pt, h=h, m=m, dstT=dstT: e.activation(
                            out=dstT[:, m, 0:n], in_=pt[:, h * n:(h + 1) * n], func=AF.Identity,
                            bias=bqk[:, 8 + m:9 + m]), reads=pb + [bqkb], writes=[dstb])
        kb.dma('sp', qv[:, :, col0:col0 + n], QT[i % 2][:, :, 0:n], reads=[QTb[i % 2]], writes=xbufs(T, 'qT', col0, n))
        kb.dma('sp', kv[:, :, col0:col0 + n], KT[i % 2][:, :, 0:n], reads=[KTb[i % 2]], writes=xbufs(T, 'kT', col0, n))
        for s in range(n // 128):
            vt, vtb = Vt[vi % 3], Vtb[vi % 3]
            vi += 1
            for nq in range(2):
                pt, pb = C.psum(512)
                for k in range(8):
                    mm(C, pt, pb, H[:, k, s * 128:(s + 1) * 128], [Hb[k]],
                       W[:, k, 2048 + nq * 512:2048 + (nq + 1) * 512], [Wb[k]], k == 0, False)
                mm(C, pt, pb, P['one_row'][0:1, 0:128], [P['one_row_b']], brow[0:1, nq * 512:(nq + 1) * 512], [browb],
                   False, True)
                kb.op('act', lambda e, pt=pt, vt=vt, nq=nq: e.activation(
                    out=vt[:, nq * 8:(nq + 1) * 8, 0:64], in_=pt.rearrange("p (h d) -> p h d", h=8), func=AF.Identity),
                    reads=pb, writes=[vtb])
            t0 = col0 + s * 128
            kb.dma('sp', T['vD'][t0:t0 + 128, :], vt.rearrange("p h d -> p (h d)"), reads=[vtb],
                   writes=xbufs(T, 'vD', t0, 128))


NA_NEG = -30000.0


def na_bias_table(rpb):
    rpb = np.asarray(rpb, dtype=np.float32)
    qc = np.arange(64)[None, :]
    kc = np.arange(64)[:, None]
    ws = np.clip(qc - 8, 0, 48)
    valid = (kc >= ws) & (kc < ws + 16)
    dcol = np.clip(kc - qc, -15, 15) + 15
    tab = np.full((2, 64, 16, 16, 64), NA_NEG, dtype=np.float32)

    def fill(half, pid, d):
        vals = rpb[:, d, :][:, dcol]
        vals = np.where(valid[None], vals, NA_NEG)
        tab[half, :, pid, :, :] = vals.transpose(1, 0, 2)
    for d in range(14):
        fill(0, d, d)
        fill(1, d, d + 1)
    fill(1, 14, 3)
    fill(0, 15, 10)
    return np.ascontiguousarray(tab.reshape(128, 16 * 16 * 64))


def phase_na_b(C, P, T, l, src, dst):
    kb = C.kb
    C.new_phase()
    Wo = C.tile([8, 1024], BF16)
    Wob = bufs(8)
    load_weight(C, Wo, Wob, T['na_w_o'][0], 8)
    bo = C.tile([8], F32)
    bob = Buf()
    kb.dma('sp', bo, T['na_b_oT'][:, :], writes=[bob])
    BT = C.tile([16, 16, 64], BF16)
    BTb = bufs(16)
    btv = T['na_bt'].rearrange("p (i x) -> p i x", i=16)
    BTf = BT.rearrange("p i h q -> p i (h q)")
    for i in range(16):
        kb.dma('pool', BTf[:, i, :], btv[:, i, :], writes=[BTb[i]])
    KcT = C.tile([8, 256], BF16)
    Vc = C.tile([2, 1040], BF16)
    Kcb, Vcb = Buf(), Buf()
    kvd = T['kT'].rearrange("(c p) t -> p c t", p=128)
    qvd = T['qT'].rearrange("(c p) t -> p c t", p=128)
    kb.dma('sp', KcT, kvd[:, :, NL:NT], reads=xbufs(T, 'kT', NL, NCX), writes=[Kcb])
    kb.dma('sp', Vc, T['vD'][NL:NT, :].rearrange("(j p) c -> p j c", p=128), reads=xbufs(T, 'vD', NL, NCX),
           writes=[Vcb])
    KT = [C.tile([8, 1024], BF16) for _ in range(2)]
    KTb = bufs(2)
    Vw = [C.tile([8, 1040], BF16) for _ in range(2)]
    Vwb = bufs(2)
    QT = [C.tile([8, 512], BF16) for _ in range(2)]
    QTb = bufs(2)
    PTs = Small(C, 3, [448], BF16)
    Ot = Small(C, 2, [1024], BF16)
    RD = Small(C, 3, [8], F32)
    OT = [C.tile([8, 256], BF16) for _ in range(2)]
    OTb = bufs(2)
    Yf = C.tile([8, 256], F32)
    Yfb = bufs(8)
    SQ = Small(C, 3, [512], BF16)
    RS = Small(C, 2, [256], F32)
    TMP = Small(C, 4, [256], F32)
    XR = Small(C, 4, [256], F32)
    S_BANKS = [0, 1, 2]
    PV_BANKS = [3, 4, 5]
    C.default_banks = [6, 7]
    ident = P['ident']
    psT_all = C.ps.bitcast(BF16)

    def attend_block(qt, qtb, qcol, lat_tiles, ot, otb):
        nt = len(lat_tiles)
        ntot = nt + 2
        hgroups = [list(range(0, 7)), list(range(7, 14)), list(range(14, 16))]
        pend = None

        def do_pv(pt, ptb, po, pvb, h):
            for t, (ktf, ktb, bid, vf, vb) in enumerate(lat_tiles):
                mm(C, po, pvb, pt[:, t * 64:(t + 1) * 64], [ptb], vf(h), [vb], t == 0, False)
            for c in range(2):
                mm(C, po, pvb, pt[:, (nt + c) * 64:(nt + c + 1) * 64], [ptb], Vc[:, c, h * 65:(h + 1) * 65], [Vcb],
                   (nt == 0 and c == 0), c == 1)

        def do_norm(pv, pvb, hg):
            nh = len(hg)
            rd, rdb = RD.get()
            pv3 = pv[0:64, :].rearrange("p (h d) -> p h d", d=65)
            kb.op('dve', lambda e: e.reciprocal(out=rd[0:64, 0:nh], in_=pv3[:, :, 64]), reads=pvb, writes=[rdb])
            h0 = hg[0]
            kb.op('dve', lambda e: e.tensor_tensor(
                out=ot[0:64, h0 * 64:(h0 + nh) * 64].rearrange("p (h d) -> p h d", d=64), in0=pv3[:, :, 0:64],
                in1=rd[0:64, 0:nh].unsqueeze(2).broadcast_to([64, nh, 64]), op=ALU.mult),
                reads=pvb + [rdb], writes=[otb], strict=True)

        for hg in hgroups:
            pv, pvb = C.psum(len(hg) * 65, banks=PV_BANKS)
            for hh, h in enumerate(hg):
                hp, ch = h % 2, h // 2
                q_ap = qt[hp * 64:(hp + 1) * 64, ch, qcol:qcol + 64]
                st, stb = C.psum(ntot * 64, banks=S_BANKS)
                for t, (ktf, ktb, bid, vf, vb) in enumerate(lat_tiles):
                    mm(C, st[:, t * 64:(t + 1) * 64], stb, ktf(h), [ktb], q_ap, [qtb], True, False)
                    mm(C, st[:, t * 64:(t + 1) * 64], stb, ident, [P['ident_b']], BT[:, bid, h, :], [BTb[bid]],
                       False, True)
                for c in range(2):
                    mm(C, st[:, (nt + c) * 64:(nt + c + 1) * 64], stb,
                       KcT[hp * 64:(hp + 1) * 64, ch, c * 128:(c + 1) * 128], [Kcb], q_ap, [qtb], True, True)
                pt, ptb = PTs.get()
                pt = pt[:, 0:ntot * 64]
                kb.op('act', lambda e, pt=pt, st=st: e.activation(out=pt, in_=st, func=AF.Exp), reads=stb, writes=[ptb])
                if pend is not None:
                    do_pv(*pend[0])
                    if pend[1] is not None:
                        do_norm(*pend[1])
                last_in_group = hh == len(hg) - 1
                pend = ((pt, ptb, pv[0:64, hh * 65:(hh + 1) * 65], pvb, h), (pv, pvb, hg) if last_in_group else None)
        do_pv(*pend[0])
        do_norm(*pend[1])

    def transpose_block(ot, otb, OTt, OTtb, slot):
        pt, pb = C.psum(256)
        bank = int(pb[0].name[2:])
        ptb16 = psT_all[:, bank * 1024:bank * 1024 + 512]
        for c in range(8):
            C.kb.op('pe', lambda e, c=c: e.transpose(ptb16[:, c * 64:(c + 1) * 64], ot[0:64, c * 128:(c + 1) * 128],
                                                     ident[0:64, 0:64]),
                    reads=[otb, P['ident_b']], writes=pb)
        kb.op('act', lambda e: e.activation(out=OTt[:, :, slot * 64:(slot + 1) * 64],
                                            in_=ptb16.rearrange("p (c t) -> p c t", c=8), func=AF.Identity),
              reads=pb, writes=[OTtb])

    def project(OTt, OTtb, n, w, col0):
        pr = PostRes(C, P, n, Yf, Yfb, SQ, RS, TMP, XR)
        for mp in range(4):
            pt, pb = C.psum(2 * n, banks=PV_BANKS + S_BANKS)
            for h in range(2):
                m = 2 * mp + h
                for k in range(8):
                    mm(C, pt[:, h * n:(h + 1) * n], pb, Wo[:, k, m * 128:(m + 1) * 128], [Wob[k]], OTt[:, k, 0:n],
                       [OTtb], k == 0, k == 7)
            pr.pair(2 * mp, pt, pb, bias=bo, bias_b=bob)
        pr.finish(l, 2, w, T[src], T[dst], col0, xbufs(T, src, col0, n), xbufs(T, dst, col0, n))

    ngroups = 16 if MAXG is None else MAXG
    oti = 0
    for g in range(ngroups):
        lo = max(0, 8 * g - 4)
        hi = min(128, 8 * g + 12)
        kt, ktb = KT[g % 2], KTb[g % 2]
        vw, vwb = Vw[g % 2], Vwb[g % 2]
        qt, qtb = QT[g % 2], QTb[g % 2]
        nw = (hi - lo) * 64
        kb.dma('sp', kt[:, :, 0:nw], kvd[:, :, lo * 64:hi * 64], reads=xbufs(T, 'kT', lo * 64, nw), writes=[ktb])
        kb.dma('sp', vw[:, 0:nw // 128, :], T['vD'][lo * 64:hi * 64, :].rearrange("(j p) c -> p j c", p=128),
               reads=xbufs(T, 'vD', lo * 64, nw), writes=[vwb])
        kb.dma('sp', qt, qvd[:, :, g * 512:(g + 1) * 512], reads=xbufs(T, 'qT', g * 512, 512), writes=[qtb])
        for rl in range(8):
            r = 8 * g + rl
            rs = min(max(r - 4, 0), 120)
            d0 = rs - r + 7
            tiles = []
            if rs % 2 == 0:
                for i4 in range(4):
                    tiles.append(((rs - lo) // 2 + i4, d0 + 2 * i4))
            else:
                assert d0 == 3
                j0 = (rs - 1 - lo) // 2
                tiles = [(j0, 14), (j0 + 1, 4), (j0 + 2, 6), (j0 + 3, 8), (j0 + 4, 15)]
            lat = []
            for (j, bid) in tiles:
                lat.append((lambda h, j=j: kt[(h % 2) * 64:(h % 2 + 1) * 64, h // 2, j * 128:(j + 1) * 128], ktb, bid,
                            lambda h, j=j: vw[:, j, h * 65:(h + 1) * 65], vwb))
            ot, otb = Ot.get()
            attend_block(qt, qtb, rl * 64, lat, ot, otb)
            OTt, OTtb = OT[oti % 2], OTb[oti % 2]
            transpose_block(ot, otb, OTt, OTtb, rl % 4)
            if rl % 4 == 3:
                project(OTt, OTtb, 256, 0, g * 512 + (rl // 4) * 256)
                oti += 1
    qt, qtb = QT[0], QTb[0]
    kb.dma('sp', qt[:, :, 0:256], qvd[:, :, NL:NT], reads=xbufs(T, 'qT', NL, NCX), writes=[qtb])
    OTt, OTtb = OT[oti % 2], OTb[oti % 2]
    for blk in range(4):
        ot, otb = Ot.get()
        attend_block(qt, qtb, blk * 64, [], ot, otb)
        transpose_block(ot, otb, OTt, OTtb, blk)
    project(OTt, OTtb, 256, 1, NL)


def ml_rope_tables():
    inv = 10000.0 ** (-np.arange(0, 32, 2, dtype=np.float64) / 32.0)
    p = np.arange(128)
    d = p % 64
    axis = d // 32
    i = d % 16
    half = (d % 32) // 16
    t = np.arange(NL)
    pos = np.where(axis[:, None] == 0, (t // 64)[None, :], (t % 64)[None, :]).astype(np.float64)
    ang = pos * inv[i][:, None]
    Cc = np.ones((128, NT), np.float64)
    Ss = np.zeros((128, NT), np.float64)
    Cc[:, :NL] = np.cos(ang)
    Ss[:, :NL] = np.sin(ang) * np.where(half[:, None] == 0, -1.0, 1.0)
    tabs = np.stack([Cc, Ss, Cc * 0.125, Ss * 0.125], axis=0).astype(np.float32)
    col = np.arange(1024)
    dd = col % 64
    perm = np.where((dd % 32) < 16, col + 16, col - 16)
    return np.ascontiguousarray(tabs), perm


def phase_ml_a(C, P, T, l, src):
    kb = C.kb
    C.new_phase()
    G = 256
    W = C.tile([8, 3072], BF16)
    Wb = bufs(8)
    load_weight(C, W, Wb, T['ml_w_in'][0], 8)
    Wsw = C.tile([8, 1024], BF16)
    Wswb = bufs(8)
    load_weight(C, Wsw, Wswb, T['ml_w_qk_sw'], 8)
    Wg = C.tile([8, 32], BF16)
    Wgb = bufs(8)
    load_weight(C, Wg, Wgb, T['ml_w_gate2'], 8)
    bg = C.tile([32], BF16)
    bgb = Buf()
    kb.dma('pool', bg[0:1, :], T['ml_b_gate2'][0:1, :], writes=[bgb])
    XL = [C.tile([8, G], F32) for _ in range(2)]
    XLb = bufs(2)
    H = C.tile([8, G], BF16)
    Hb = bufs(8)
    QK = [C.tile([8, G], BF16) for _ in range(2)]
    QKb = bufs(2)
    OT = [C.tile([8, G], BF16) for _ in range(2)]
    OTb = bufs(2)
    TB = [C.tile([4, G], F32) for _ in range(2)]
    TBb = bufs(2)
    Vt = [C.tile([8, 129], BF16) for _ in range(3)]
    Vtb = bufs(3)
    for i in range(3):
        kb.op('pool', lambda e, i=i: e.memset(Vt[i], 1.0), writes=[Vtb[i]])
    Kt = [C.tile([512], BF16) for _ in range(2)]
    Ktb = bufs(2)
    Gt = [C.tile([32], F32) for _ in range(2)]
    Gtb = bufs(2)
    GE = Small(C, 2, [32], F32)
    SQ = Small(C, 3, [G], BF16)
    RS = Small(C, 2, [G], F32)
    TMP = Small(C, 4, [G], F32)
    grp = groups(G, True, full=True)
    srcv = T[src].rearrange("(c p) t -> p c t", p=128)
    qkv = T['mqk'].rearrange("(c p) t -> p c t", p=128)
    ov = T['moT'].rearrange("(c p) t -> p c t", p=128)
    tabv = T['ml_rope'].rearrange("a p t -> p a t")
    psT_all = C.ps.bitcast(BF16)
    ident = P['ident']

    def load(i):
        col0, n, w = grp[i]
        kb.dma('sp', XL[i % 2][:, :, 0:n], srcv[:, :, col0:col0 + n], reads=xbufs(T, src, col0, n),
               writes=[XLb[i % 2]])
        kb.dma('sp', TB[i % 2][:, :, 0:n], tabv[:, :, col0:col0 + n], writes=[TBb[i % 2]])
    load(0)
    vi = 0
    for i, (col0, n, w) in enumerate(grp):
        if i + 1 < len(grp):
            load(i + 1)
        prenorm(C, P, XL[i % 2], XLb[i % 2], n, H, Hb, l, 0, 1, w, SQ, RS, TMP)
        qk, qkb = QK[i % 2], QKb[i % 2]
        tb, tbb = TB[i % 2], TBb[i % 2]
        for m in range(8):
            pt, pb = C.psum(2 * n)
            for k in range(8):
                mm(C, pt[:, 0:n], pb, W[:, k, m * 128:(m + 1) * 128], [Wb[k]], H[:, k, 0:n], [Hb[k]], k == 0, k == 7)
            for k in range(8):
                mm(C, pt[:, n:2 * n], pb, Wsw[:, k, m * 128:(m + 1) * 128], [Wswb[k]], H[:, k, 0:n], [Hb[k]],
                   k == 0, k == 7)
            ti = 0 if m < 4 else 2
            t1, t1b = TMP.get()
            t2, t2b = TMP.get()
            kb.op('dve', lambda e, t1=t1, pt=pt, ti=ti, tb=tb: e.tensor_tensor(out=t1[:, 0:n], in0=pt[:, 0:n],
                                                                             in1=tb[:, ti, 0:n], op=ALU.mult),
                  reads=pb + [tbb], writes=[t1b])
            kb.op('dve', lambda e, t2=t2, pt=pt, ti=ti, tb=tb: e.tensor_tensor(out=t2[:, 0:n], in0=pt[:, n:2 * n],
                                                                             in1=tb[:, ti + 1, 0:n], op=ALU.mult),
                  reads=pb + [tbb], writes=[t2b])
            kb.op('pool', lambda e, t1=t1, t2=t2, m=m, qk=qk: e.tensor_tensor(out=qk[:, m, 0:n], in0=t1[:, 0:n],
                                                                            in1=t2[:, 0:n], op=ALU.add),
                  reads=[t1b, t2b], writes=[qkb])
        kb.dma('sp', qkv[:, :, col0:col0 + n], qk[:, :, 0:n], reads=[qkb], writes=xbufs(T, 'mqk', col0, n))
        ot, otb = OT[i % 2], OTb[i % 2]
        for mp in range(4):
            pt, pb = C.psum(2 * n)
            for h in range(2):
                m = 2 * mp + h
                for k in range(8):
                    mm(C, pt[:, h * n:(h + 1) * n], pb, W[:, k, 2048 + m * 128:2048 + (m + 1) * 128], [Wb[k]],
                       H[:, k, 0:n], [Hb[k]], k == 0, k == 7)
            kb.op('act', lambda e, pt=pt, mp=mp, ot=ot: e.activation(
                out=ot[:, 2 * mp:2 * mp + 2, 0:n], in_=pt.rearrange("p (a t) -> p a t", a=2), func=AF.Sigmoid),
                reads=pb, writes=[otb])
        kb.dma('sp', ov[:, :, col0:col0 + n], ot[:, :, 0:n], reads=[otb], writes=xbufs(T, 'moT', col0, n))
        for s in range(n // 128):
            t0 = col0 + s * 128
            vt, vtb = Vt[vi % 3], Vtb[vi % 3]
            for nq in range(2):
                pt, pb = C.psum(512)
                for k in range(8):
                    mm(C, pt, pb, H[:, k, s * 128:(s + 1) * 128], [Hb[k]],
                       W[:, k, 1024 + nq * 512:1024 + (nq + 1) * 512], [Wb[k]], k == 0, k == 7)
                kb.op('act', lambda e, pt=pt, vt=vt, nq=nq: e.activation(
                    out=vt[:, nq * 4:(nq + 1) * 4, 0:128], in_=pt.rearrange("p (h d) -> p h d", h=4),
                    func=AF.Identity), reads=pb, writes=[vtb])
            kb.dma('sp', T['mvD'][t0:t0 + 128, :], vt.rearrange("p h d -> p (h d)"), reads=[vtb],
                   writes=xbufs(T, 'mvD', t0, 128))
            kt, ktb = Kt[vi % 2], Ktb[vi % 2]
            pt, pb = C.psum(256)
            bank = int(pb[0].name[2:])
            ptb16 = psT_all[:, bank * 1024:bank * 1024 + 512]
            for c in range(4):
                kb.op('pe', lambda e, c=c, ptb16=ptb16, qk=qk, s=s: e.transpose(
                    ptb16[:, c * 128:(c + 1) * 128], qk[:, 4 + c, s * 128:(s + 1) * 128], ident),
                    reads=[qkb, P['ident_b']], writes=pb)
            kb.op('act', lambda e, kt=kt, ptb16=ptb16: e.activation(out=kt, in_=ptb16, func=AF.Identity), reads=pb,
                  writes=[ktb])
            kb.dma('sp', T['mkD'][t0:t0 + 128, :], kt, reads=[ktb], writes=xbufs(T, 'mkD', t0, 128))
            gt, gtb = Gt[vi % 2], Gtb[vi % 2]
            pt, pb = C.psum(32)
            for k in range(8):
                mm(C, pt, pb, H[:, k, s * 128:(s + 1) * 128], [Hb[k]], Wg[:, k, :], [Wgb[k]], k == 0, False)
            mm(C, pt, pb, P['one_row'][0:1, 0:128], [P['one_row_b']], bg[0:1, :], [bgb], False, True)
            ge, geb = GE.get()
            p4 = pt.rearrange("p (a b h) -> p a b h", a=2, b=2)
            g4 = gt.rearrange("p (a b h) -> p a b h", a=2, b=2)
            e4 = ge.rearrange("p (a b h) -> p a b h", a=2, b=2)
            kb.op('act', lambda e, p4=p4, e4=e4: e.activation(out=e4[:, :, 1, :], in_=p4[:, :, 1, :], func=AF.Exp,
                                                             scale=-1.0), reads=pb, writes=[geb])
            kb.op('act', lambda e, e4=e4: e.activation(out=e4[:, :, 1, :], in_=e4[:, :, 1, :], func=AF.Ln, bias=1.0),
                  reads=[geb], writes=[geb], strict=True)
            kb.op('dve', lambda e, g4=g4, e4=e4: e.tensor_scalar(out=g4[:, :, 1, :], in0=e4[:, :, 1, :], scalar1=-1.0,
                                                                scalar2=None, op0=ALU.mult), reads=[geb], writes=[gtb])
            kb.op('dve', lambda e, g4=g4, p4=p4: e.tensor_copy(out=g4[:, :, 0, :], in_=p4[:, :, 0, :]), reads=pb,
                  writes=[gtb])
            kb.dma('sp', T['mgD'][t0:t0 + 128, :], gt, reads=[gtb], writes=xbufs(T, 'mgD', t0, 128))
            vi += 1


def phase_ml_b(C, P, T):
    kb = C.kb
    C.new_phase()
    NEG = -30000.0
    cA = [C.tile([128], F32) for _ in range(2)]
    cB = [C.tile([128], F32) for _ in range(2)]
    cM = [C.tile([128], F32) for _ in range(2)]
    cI = C.tile([128], F32)
    cO = C.tile([128], F32)
    onesb = C.tile([128], BF16)
    cb = Buf()
    for t_, val in ((cA[0], 1.0), (cA[1], 1.0), (cB[0], 1.0), (cB[1], 1.0), (cM[0], 0.0), (cM[1], 0.0), (cI, 0.0),
                    (cO, 1.0)):
        kb.op('pool', lambda e, t_=t_, val=val: e.memset(t_, val), writes=[cb])
    kb.op('pool', lambda e: e.memset(onesb, 1.0), writes=[cb])

    def asel(t_, cmp, fill, base=0, cm=1, pat=-1):
        kb.op('pool', lambda e: e.affine_select(out=t_, in_=t_, pattern=[[pat, 128]], compare_op=cmp, fill=fill,
                                                base=base, channel_multiplier=cm), reads=[cb], writes=[cb],
              strict=True)
    asel(cA[0], ALU.is_gt, 0.0, base=0, cm=1, pat=-1)
    asel(cA[1], ALU.is_gt, 0.0, base=0, cm=-1, pat=1)
    asel(cB[0], ALU.is_gt, 0.0, base=1, cm=-1, pat=1)
    asel(cB[1], ALU.is_gt, 0.0, base=1, cm=1, pat=-1)
    asel(cM[0], ALU.is_gt, NEG, base=1, cm=-1, pat=1)
    asel(cM[1], ALU.is_gt, NEG, base=1, cm=1, pat=-1)
    asel(cI, ALU.not_equal, 1.0)
    St = C.tile([8, 129], F32)
    Cbf = C.tile([8, 128], BF16)
    Nbc = C.tile([8, 128], BF16)
    Stb, Cbfb, Nbcb = Buf(), Buf(), Buf()
    NB = 2
    Q = [C.tile([8, 128], BF16) for _ in range(NB)]
    K_ = [C.tile([8, 128], BF16) for _ in range(NB)]
    KT = [C.tile([8, 64], BF16) for _ in range(NB)]
    V = [C.tile([8, 129], BF16) for _ in range(NB)]
    GA = [C.tile([32], F32) for _ in range(NB)]
    Qb, Kb_, KTb, Vb, GAb = bufs(NB), bufs(NB), bufs(NB), bufs(NB), bufs(NB)
    R = C.tile([8, 128], F32)
    R2 = C.tile([8, 128], F32)
    Rb, R2b = Buf(), Buf()
    E = C.tile([8, 128], F32)
    EB = C.tile([8, 128], F32)
    Eb, EBb = Buf(), Buf()
    AT = C.tile([8, 128], BF16)
    ATb = Buf()
    KW = C.tile([8, 64], BF16)
    KWb = Buf()
    SM = Small(C, 2, [32], F32)
    NUM = C.tile([8, 128], F32)
    DEN = C.tile([8, 128], F32)
    NUMb, DENb = Buf(), Buf()
    HO = [C.tile([8, 128], F32) for _ in range(2)]
    HOb = bufs(2)
    mq = T['mqk']
    hidx = 0
    it = 0
    ntl = 64 if MAXG is None else MAXG
    for dr in range(2):
        for t_ in (St, Cbf, Nbc):
            kb.op('pool', lambda e, t_=t_: e.memset(t_, 0.0), reads=[], writes=[Stb, Cbfb, Nbcb])
        lat = list(range(ntl)) if dr == 0 else list(range(ntl - 1, -1, -1))
        seq = ([64, 65] if dr == 0 else [65, 64]) + lat
        hD = T['mhf'] if dr == 0 else T['mhb']
        hkey = 'mhf' if dr == 0 else 'mhb'
        for ti in seq:
            t0 = ti * 128
            b = it % NB
            it += 1
            q, k_, kt, v, ga = Q[b], K_[b], KT[b], V[b], GA[b]
            kb.dma('sp', q[0:64], mq[0:512, t0:t0 + 128].rearrange("(h d) t -> d h t", d=64),
                   reads=xbufs(T, 'mqk', t0, 128), writes=[Qb[b]])
            kb.dma('sp', k_[0:64], mq[512:1024, t0:t0 + 128].rearrange("(h d) t -> d h t", d=64),
                   reads=xbufs(T, 'mqk', t0, 128), writes=[Kb_[b]])
            kb.dma('sp', kt, T['mkD'][t0:t0 + 128, :].rearrange("p (h d) -> p h d", h=8),
                   reads=xbufs(T, 'mkD', t0, 128), writes=[KTb[b]])
            kb.dma('sp', v, T['mvD'][t0:t0 + 128, :].rearrange("p (h d) -> p h d", h=8),
                   reads=xbufs(T, 'mvD', t0, 128), writes=[Vb[b]])
            kb.dma('sp', ga, T['mgD'][t0:t0 + 128, :], reads=xbufs(T, 'mgD', t0, 128), writes=[GAb[b]])
            ig = ga[:, dr * 16:dr * 16 + 8]
            lf = ga[:, dr * 16 + 8:dr * 16 + 16]
            kb.op('dve', lambda e, lf=lf, dr=dr: e.tensor_tensor(out=R, in0=cB[dr].unsqueeze(1).broadcast_to([128, 8, 128]),
                                                          in1=lf.unsqueeze(2).broadcast_to([128, 8, 128]),
                                                          op=ALU.mult), reads=[cb, GAb[b]], writes=[Rb])
            kb.op('pool', lambda e, ig=ig, dr=dr: e.tensor_tensor(out=R2, in0=cM[dr].unsqueeze(1).broadcast_to([128, 8, 128]),
                                                           in1=ig.unsqueeze(2).broadcast_to([128, 8, 128]),
                                                           op=ALU.add), reads=[cb, GAb[b]], writes=[R2b])
            psm, psmb = C.psum(16)
            mm(C, psm[:, 0:8], psmb, cA[dr], [cb], lf, [GAb[b]], True, True)
            mm(C, psm[:, 8:16], psmb, cO, [cb], lf, [GAb[b]], True, True)
            sm, smb = SM.get()
            kb.op('dve', lambda e, sm=sm, psm=psm, ig=ig: e.tensor_tensor(out=sm[:, 0:8], in0=psm[:, 0:8], in1=ig,
                                                                         op=ALU.add), reads=psmb + [GAb[b]],
                  writes=[smb])
            kb.op('act', lambda e, sm=sm: e.activation(out=sm[:, 0:8], in_=sm[:, 0:8], func=AF.Exp), reads=[smb],
                  writes=[smb])
            kb.op('act', lambda e, sm=sm, psm=psm: e.activation(out=sm[:, 8:16], in_=psm[:, 8:16], func=AF.Exp),
                  reads=psmb, writes=[smb], strict=True)
            pE, pB, pS = [], [], []
            for hf in range(2):
                pe_, peb = C.psum(512)
                Rv = R[:, hf * 4:(hf + 1) * 4, :].rearrange("p h t -> p (h t)")
                R2v = R2[:, hf * 4:(hf + 1) * 4, :].rearrange("p h t -> p (h t)")
                mm(C, pe_, peb, cA[dr], [cb], Rv, [Rb], True, False)
                mm(C, pe_, peb, cI, [cb], R2v, [R2b], False, True)
                kb.op('act', lambda e, pe_=pe_, hf=hf: e.activation(
                    out=E[:, hf * 4:(hf + 1) * 4, :].rearrange("p h t -> p (h t)"), in_=pe_, func=AF.Exp),
                    reads=peb, writes=[Eb])
                pb_, pbb = C.psum(512)
                mm(C, pb_, pbb, cO, [cb], Rv, [Rb], True, True)
                kb.op('act', lambda e, pb_=pb_, hf=hf: e.activation(
                    out=EB[:, hf * 4:(hf + 1) * 4, :].rearrange("p h t -> p (h t)"), in_=pb_, func=AF.Exp),
                    reads=pbb, writes=[EBb])
            for hf in range(2):
                ps_, psb_ = C.psum(512)
                for hh in range(4):
                    h = hf * 4 + hh
                    mm(C, ps_[:, hh * 128:(hh + 1) * 128], psb_, k_[0:64, h, :], [Kb_[b]], q[0:64, h, :], [Qb[b]],
                       True, True)
                kb.op('dve', lambda e, ps_=ps_, hf=hf: e.tensor_tensor(
                    out=AT[:, hf * 4:(hf + 1) * 4, :].rearrange("p h t -> p (h t)"), in0=ps_,
                    in1=E[:, hf * 4:(hf + 1) * 4, :].rearrange("p h t -> p (h t)"), op=ALU.mult),
                    reads=psb_ + [Eb], writes=[ATb])
            ho, hob = HO[hidx % 2], HOb[hidx % 2]
            hidx += 1
            for hf in range(2):
                hs = slice(hf * 4, (hf + 1) * 4)
                pn, pnb = C.psum(512)
                for hh in range(4):
                    h = hf * 4 + hh
                    mm(C, pn[:, hh * 128:(hh + 1) * 128], pnb, v[:, h, 0:128], [Vb[b]], AT[:, h, :], [ATb], True, True)
                pd_, pdb = C.psum(512)
                mm(C, pd_, pdb, onesb, [cb], AT[:, hs, :].rearrange("p h t -> p (h t)"), [ATb], True, True)
                pi, pib = C.psum(512)
                for hh in range(4):
                    h = hf * 4 + hh
                    mm(C, pi[:, hh * 128:(hh + 1) * 128], pib, Cbf[0:64, h, :], [Cbfb], q[0:64, h, :], [Qb[b]],
                       True, True)
                pq, pqb = C.psum(512)
                for hh in range(4):
                    h = hf * 4 + hh
                    mm(C, pq[:, hh * 128:(hh + 1) * 128], pqb, Nbc[0:64, h, :], [Nbcb], q[0:64, h, :], [Qb[b]],
                       True, True)
                ebv = EB[:, hs, :].rearrange("p h t -> p (h t)")
                numv = NUM[:, hs, :].rearrange("p h t -> p (h t)")
                denv = DEN[:, hs, :].rearrange("p h t -> p (h t)")
                hov = ho[:, hs, :].rearrange("p h t -> p (h t)")
                kb.op('dve', lambda e, pi=pi, ebv=ebv, numv=numv: e.tensor_tensor(out=numv, in0=pi, in1=ebv,
                                                                                 op=ALU.mult),
                      reads=pib + [EBb], writes=[NUMb])
                kb.op('dve', lambda e, pn=pn, numv=numv: e.tensor_tensor(out=numv, in0=pn, in1=numv, op=ALU.add),
                      reads=pnb + [NUMb], writes=[NUMb])
                kb.op('dve', lambda e, pq=pq, ebv=ebv, denv=denv: e.tensor_tensor(out=denv, in0=pq, in1=ebv,
                                                                                 op=ALU.mult),
                      reads=pqb + [EBb], writes=[DENb])
                kb.op('dve', lambda e, pd_=pd_, denv=denv: e.tensor_tensor(out=denv, in0=pd_, in1=denv, op=ALU.add),
                      reads=pdb + [DENb], writes=[DENb])
                kb.op('act', lambda e, denv=denv: e.activation(out=denv, in_=denv, func=AF.Abs), reads=[DENb],
                      writes=[DENb])
                kb.op('dve', lambda e, denv=denv: e.tensor_scalar(out=denv, in0=denv, scalar1=1.0, scalar2=None,
                                                                  op0=ALU.max), reads=[DENb], writes=[DENb])
                kb.op('dve', lambda e, denv=denv: e.reciprocal(out=denv, in_=denv), reads=[DENb], writes=[DENb])
                kb.op('pool', lambda e, denv=denv, numv=numv, hov=hov: e.tensor_tensor(out=hov, in0=numv, in1=denv,
                                                                                      op=ALU.mult),
                      reads=[DENb, NUMb], writes=[hob])
            kb.dma('sp', hD[:, t0:t0 + 128].rearrange("(h p) t -> p h t", p=128), ho, reads=[hob],
                   writes=xbufs(T, hkey, t0, 128))
            sm_w = sm[:, 0:8]
            kb.op('dve', lambda e, sm_w=sm_w, kt=kt: e.tensor_tensor(
                out=KW, in0=kt, in1=sm_w.unsqueeze(2).broadcast_to([128, 8, 64]), op=ALU.mult),
                reads=[KTb[b], smb], writes=[KWb])
            for (h0, nh) in ((0, 3), (3, 3), (6, 2)):
                pu, pub = C.psum(nh * 129)
                for hh in range(nh):
                    h = h0 + hh
                    mm(C, pu[0:64, hh * 129:(hh + 1) * 129], pub, KW[:, h, :], [KWb], v[:, h, :], [Vb[b]], True, True)
                stv = St[0:64, h0:h0 + nh, :]
                kb.op('dve', lambda e, stv=stv, sm=sm, h0=h0, nh=nh: e.tensor_tensor(
                    out=stv, in0=stv, in1=sm[0:64, 8 + h0:8 + h0 + nh].unsqueeze(2).broadcast_to([64, nh, 129]),
                    op=ALU.mult), reads=[Stb, smb], writes=[Stb], strict=True)
                kb.op('dve', lambda e, stv=stv, pu=pu, nh=nh: e.tensor_tensor(
                    out=stv, in0=pu[0:64, :].rearrange("p (h d) -> p h d", h=nh), in1=stv, op=ALU.add),
                    reads=pub + [Stb], writes=[Stb], strict=True)
            kb.op('act', lambda e: e.activation(out=Cbf[0:64], in_=St[0:64, :, 0:128], func=AF.Identity),
                  reads=[Stb], writes=[Cbfb])
            kb.op('pool', lambda e: e.tensor_copy(out=Nbc[0:64], in_=St[0:64, :, 128:129].broadcast_to([64, 8, 128])),
                  reads=[Stb], writes=[Nbcb])


def phase_ml_c(C, P, T, l, src, dst):
    kb = C.kb
    C.new_phase()
    G = 256
    Wo = C.tile([8, 1024], BF16)
    Wob = bufs(8)
    load_weight(C, Wo, Wob, T['ml_w_out'][0], 8)
    ng = C.tile([8], F32)
    ngb = Buf()
    kb.dma('sp', ng, T['ml_norm_gT'][:, :], writes=[ngb])
    ones128 = C.tile([128], BF16)
    o1b = Buf()
    kb.op('pool', lambda e: e.memset(ones128, 1.0 / 128), writes=[o1b])
    HF = [C.tile([8, G], F32) for _ in range(2)]
    HB = [C.tile([8, G], F32) for _ in range(2)]
    OG = [C.tile([8, G], BF16) for _ in range(2)]
    HFb, HBb, OGb = bufs(2), bufs(2), bufs(2)
    Hm = C.tile([8, G], BF16)
    Hmb = bufs(8)
    Yf = C.tile([8, G], F32)
    Yfb = bufs(8)
    SQ = Small(C, 3, [2 * G], BF16)
    RS = Small(C, 3, [G], F32)
    TMP = Small(C, 4, [G], F32)
    XR = Small(C, 4, [G], F32)
    grp = groups(G, True)
    hfv = T['mhf'].rearrange("(c p) t -> p c t", p=128)
    hbv = T['mhb'].rearrange("(c p) t -> p c t", p=128)
    ov = T['moT'].rearrange("(c p) t -> p c t", p=128)

    def load(i):
        col0, n, w = grp[i]
        kb.dma('sp', HF[i % 2][:, :, 0:n], hfv[:, :, col0:col0 + n], reads=xbufs(T, 'mhf', col0, n),
               writes=[HFb[i % 2]])
        kb.dma('sp', HB[i % 2][:, :, 0:n], hbv[:, :, col0:col0 + n], reads=xbufs(T, 'mhb', col0, n),
               writes=[HBb[i % 2]])
        kb.dma('sp', OG[i % 2][:, :, 0:n], ov[:, :, col0:col0 + n], reads=xbufs(T, 'moT', col0, n),
               writes=[OGb[i % 2]])
    load(0)
    for i, (col0, n, w) in enumerate(grp):
        if i + 1 < len(grp):
            load(i + 1)
        hf, hb, og = HF[i % 2], HB[i % 2], OG[i % 2]
        kb.op('pool', lambda e, hf=hf, hb=hb: e.tensor_tensor(out=hf[:, :, 0:n], in0=hf[:, :, 0:n], in1=hb[:, :, 0:n],
                                                             op=ALU.add), reads=[HFb[i % 2], HBb[i % 2]],
              writes=[HFb[i % 2]])
        for c in range(8):
            s_, sb_ = SQ.get()
            kb.op('act', lambda e, s_=s_, c=c, hf=hf: e.activation(out=s_[:, 0:n], in_=hf[:, c, 0:n], func=AF.Square),
                  reads=[HFb[i % 2]], writes=[sb_])
            pst, pb = C.psum(n)
            mm(C, pst, pb, ones128, [o1b], s_[:, 0:n], [sb_], True, True)
            rs, rb = rstd_from_psum(C, P, pst, pb, RS)
            t, tb = TMP.get()
            kb.op('dve', lambda e, t=t, c=c, hf=hf, rs=rs: e.scalar_tensor_tensor(
                out=t[:, 0:n], in0=hf[:, c, 0:n], scalar=ng[:, c:c + 1], in1=rs, op0=ALU.mult, op1=ALU.mult),
                reads=[HFb[i % 2], rb, ngb], writes=[tb])
            kb.op('pool', lambda e, t=t, c=c, og=og: e.tensor_tensor(out=Hm[:, c, 0:n], in0=t[:, 0:n],
                                                                    in1=og[:, c, 0:n], op=ALU.mult),
                  reads=[tb, OGb[i % 2]], writes=[Hmb[c]])
        pr = PostRes(C, P, n, Yf, Yfb, SQ, RS, TMP, XR)
        for mp in range(4):
            pt, pb = C.psum(2 * n)
            for h in range(2):
                m = 2 * mp + h
                for k in range(8):
                    mm(C, pt[:, h * n:(h + 1) * n], pb, Wo[:, k, m * 128:(m + 1) * 128], [Wob[k]], Hm[:, k, 0:n],
                       [Hmb[k]], k == 0, k == 7)
            pr.pair(2 * mp, pt, pb)
        pr.finish(l, 2, w, T[src], T[dst], col0, xbufs(T, src, col0, n), xbufs(T, dst, col0, n))


def build_program(plan, dbg=False):
    nc = bass.Bass("TRN2", target_bir_lowering=False)
    es = ExitStack()
    T = {}

    def dram(name, shape, dt, kind):
        T[name] = nc.dram_tensor(name, shape, dt, kind=kind).ap()
        return T[name]

    dram('xin', [D, NT], F32, "ExternalInput")
    dram('cvec', [128, 8, 2], F32, "ExternalInput")
    dram('ada_w', [4, D, 6 * D], F32, "ExternalInput")
    dram('ada_bT', [128, 4, 48], F32, "ExternalInput")
    dram('norm_gT', [128, 4, 4, 8], F32, "ExternalInput")
    dram('ffn_w1', [4, D, 4 * D], F32, "ExternalInput")
    dram('ffn_w2', [4, 4 * D, D], F32, "ExternalInput")
    dram('gm_w_in', [2, D, 6 * D], F32, "ExternalInput")
    dram('gm_b_in', [2, 6 * D], F32, "ExternalInput")
    dram('gm_ln_gT', [2, 128, 24], F32, "ExternalInput")
    dram('gm_wsT', [2, 128, 8 * 128], F32, "ExternalInput")
    dram('gm_bs_bc', [2, 128, 8 * 128], F32, "ExternalInput")
    dram('gm_w_out', [2, 3 * D, D], F32, "ExternalInput")
    dram('na_w_qkv', [1, D, 3 * D], F32, "ExternalInput")
    dram('na_b_qkv', [1, 3 * D], F32, "ExternalInput")
    dram('na_bqkvT', [128, 24], F32, "ExternalInput")
    dram('na_bt', [128, 16 * 16 * 64], F32, "ExternalInput")
    dram('na_w_o', [1, D, D], F32, "ExternalInput")
    dram('na_b_oT', [128, 8], F32, "ExternalInput")
    dram('qT', [D, NT], BF16, "Internal")
    dram('kT', [D, NT], BF16, "Internal")
    dram('vD', [NT, 1040], BF16, "Internal")
    dram('ml_w_in', [1, D, 3 * D], F32, "ExternalInput")
    dram('ml_w_qk_sw', [D, D], F32, "ExternalInput")
    dram('ml_w_gate2', [D, 32], F32, "ExternalInput")
    dram('ml_b_gate2', [1, 32], F32, "ExternalInput")
    dram('ml_rope', [4, 128, NT], F32, "ExternalInput")
    dram('ml_norm_gT', [128, 8], F32, "ExternalInput")
    dram('ml_w_out', [1, D, D], F32, "ExternalInput")
    dram('mqk', [D, NT], BF16, "Internal")
    dram('moT', [D, NT], BF16, "Internal")
    dram('mvD', [NT, 8 * 129], BF16, "Internal")
    dram('mkD', [NT, 512], BF16, "Internal")
    dram('mgD', [NT, 32], F32, "Internal")
    dram('mhf', [D, NT], F32, "ExternalOutput" if dbg else "Internal")
    dram('mhb', [D, NT], F32, "ExternalOutput" if dbg else "Internal")
    dram('yout', [D, NL], F32, "ExternalOutput")
    dram('xs', [D, NT], F32, "ExternalOutput" if dbg else "Internal")
    dram('pd', [3 * D, NT], BF16, "ExternalOutput" if dbg else "Internal")
    if dbg:
        dram('dbg_vh', [128, 3072], BF16, "ExternalOutput")
        dram('dbg_mv', [128, 8], F32, "ExternalOutput")
        dram('dbg_zg', [128, 3072], F32, "ExternalOutput")
    for k in ['xin', 'xs', 'pd', 'yout', 'qT', 'kT', 'vD', 'mqk', 'moT', 'mvD', 'mkD', 'mgD', 'mhf', 'mhb']:
        T[k + '_b'] = bufs(NT // 128, k)
    C = Ctx(nc, es)
    P = phase_consts(C, T)
    for ph in plan:
        kind = ph[0]
        if kind == 'ffn':
            _, l, src, dst, with_ctx, final = ph
            phase_ffn(C, P, T, l, src, dst, with_ctx, final)
        elif kind == 'gmlp':
            _, l, j, src, dst, with_ctx = ph
            phase_gmlp_a(C, P, T, l, j, src, with_ctx)
            phase_gmlp_b(C, P, T, l, j, src, dst, with_ctx)
        elif kind == 'na':
            _, l, src, dst = ph
            if NA_PARTS & 1:
                phase_na_a(C, P, T, l, src)
            if NA_PARTS & 2:
                phase_na_b(C, P, T, l, src, dst)
        elif kind == 'ml':
            _, l, src, dst = ph
            phase_ml_a(C, P, T, l, src)
            phase_ml_b(C, P, T)
            phase_ml_c(C, P, T, l, src, dst)
        else:
            raise ValueError(kind)
    C.kb.barrier()
    C.kb.emit(es)
    es.close()
    return nc, C


def host_shared(inp):
    f = np.float32
    m = {}
    m['ada_w'] = inp['ada_w']
    m['ada_bT'] = np.ascontiguousarray(inp['ada_b'].reshape(4, 48, 128).transpose(2, 0, 1), dtype=f)
    m['norm_gT'] = np.ascontiguousarray(inp['norm_g'].reshape(4, 4, 8, 128).transpose(3, 0, 1, 2), dtype=f)
    m['ffn_w1'] = inp['ffn_w1']
    m['ffn_w2'] = inp['ffn_w2']
    m['gm_w_in'] = inp['gm_w_in']
    m['gm_b_in'] = inp['gm_b_in']
    m['gm_w_out'] = inp['gm_w_out']
    m['gm_ln_gT'] = np.ascontiguousarray(inp['gm_ln_g'].reshape(2, 24, 128).transpose(0, 2, 1), dtype=f)
    m['gm_wsT'] = np.ascontiguousarray(inp['gm_ws'].transpose(0, 3, 1, 2).reshape(2, 128, 1024), dtype=f)
    m['na_w_qkv'] = inp['na_w_qkv']
    m['na_b_qkv'] = inp['na_b_qkv']
    m['na_bqkvT'] = np.ascontiguousarray(inp['na_b_qkv'][0].reshape(24, 128).T, dtype=f)
    m['na_bt'] = na_bias_table(inp['na_rpb'][0])
    m['na_w_o'] = inp['na_w_o']
    m['na_b_oT'] = np.ascontiguousarray(inp['na_b_o'][0].reshape(8, 128).T, dtype=f)
    tabs, perm = ml_rope_tables()
    m['ml_w_in'] = inp['ml_w_in']
    m['ml_w_qk_sw'] = np.ascontiguousarray(inp['ml_w_in'][0][:, :1024][:, perm], dtype=f)
    wg = inp['ml_w_gate'][0]
    m['ml_w_gate2'] = np.ascontiguousarray(np.concatenate([wg[0], wg[1]], axis=1), dtype=f)
    m['ml_b_gate2'] = np.ascontiguousarray(inp['ml_b_gate'][0].reshape(1, 32), dtype=f)
    m['ml_rope'] = tabs
    m['ml_norm_gT'] = np.ascontiguousarray(inp['ml_norm_g'][0].reshape(8, 128).T, dtype=f)
    m['ml_w_out'] = inp['ml_w_out']
    m['gm_bs_bc'] = np.ascontiguousarray(np.broadcast_to(inp['gm_bs'].reshape(2, 1, 1024), (2, 128, 1024)), dtype=f)
    return m


def host_inputs(inp, b, shared=None):
    f = np.float32
    m = dict(shared if shared is not None else host_shared(inp))
    m['xin'] = np.ascontiguousarray(np.concatenate([inp['x'][b].T, inp['ctx'][b].T], axis=1), dtype=f)
    cv = np.stack([inp['c'][b], inp['c_ctx']], axis=-1)
    m['cvec'] = np.ascontiguousarray(cv.reshape(8, 128, 2).transpose(1, 0, 2), dtype=f)
    return m


FULL_PLAN = [
    ('gmlp', 0, 0, 'xin', 'xs', True), ('ffn', 0, 'xs', 'xs', True, False),
    ('na', 1, 'xs', 'xs'), ('ffn', 1, 'xs', 'xs', True, False),
    ('ml', 2, 'xs', 'xs'), ('ffn', 2, 'xs', 'xs', True, False),
    ('gmlp', 3, 1, 'xs', 'xs', False), ('ffn', 3, 'xs', 'xs', False, True),
]


def kernel(**inputs):
    inp = {k: np.asarray(v) for k, v in inputs.items()}
    nc, C = build_program(FULL_PLAN)
    shared = host_shared(inp)
    in_maps = [host_inputs(inp, b, shared) for b in range(8)]
    res = run_bass_kernel_spmd(nc, in_maps, core_ids=list(range(8)))
    out = np.stack([np.ascontiguousarray(np.asarray(res.results[b]['yout']).T) for b in range(8)], axis=0)
    return out.astype(np.float32)
```

```python
import math
import numpy as np
import concourse.bass as bass
import concourse.mybir as mybir
from concourse.bass_utils import run_bass_kernel_spmd
from contextlib import ExitStack

F32 = mybir.dt.float32
BF16 = mybir.dt.bfloat16
U8 = mybir.dt.uint8
AF = mybir.ActivationFunctionType
ALU = mybir.AluOpType

ENGS = ['pe', 'act', 'dve', 'pool', 'sp']
NSLOT = 12
SAME_ENGINE_SYNC = False

D = 1024
KC = 8
NL = 8192
NCX = 256
NT = NL + NCX
EPS = 1e-6
ARENA_BYTES = 204 * 1024


class Buf:
    __slots__ = ('name', 'lw', 'rd')

    def __init__(self, name=''):
        self.name = name
        self.lw = None
        self.rd = {}


def bufs(n, name=''):
    return [Buf(name + str(i)) for i in range(n)]


class KB:
    def __init__(self, nc):
        self.nc = nc
        self.streams = {e: [] for e in ENGS}
        self.seen = {e: {} for e in ENGS}
        self.dseen = {e: set() for e in ENGS}
        self.slot_rr = {e: 0 for e in ENGS}
        self.slot_cnt = {e: [0] * NSLOT for e in ENGS}
        self.slot_last = {e: [None] * NSLOT for e in ENGS}
        self.n_ops = 0

    def _collect(self, eng, reads, writes, strict=False):
        deps_e = {}
        deps_d = set()

        def addtok(t):
            if t is None:
                return
            if t[0] == 'e':
                if t[2] > deps_e.get(t[1], -1):
                    deps_e[t[1]] = t[2]
            else:
                deps_d.add(t)

        for b in reads:
            addtok(b.lw)
        for b in writes:
            addtok(b.lw)
            for k, v in b.rd.items():
                if isinstance(k, tuple):
                    deps_d.add(k)
                else:
                    addtok(('e', k, v))
        waits = []
        for E, idx in deps_e.items():
            if E == eng and (eng == 'pe' or not (SAME_ENGINE_SYNC or strict)):
                continue
            if self.seen[eng].get(E, -1) >= idx:
                continue
            self.seen[eng][E] = idx
            self.streams[E][idx]['inc'] = True
            waits.append(('e', E, idx))
        for t in deps_d:
            if t in self.dseen[eng]:
                continue
            self.dseen[eng].add(t)
            waits.append(t)
        return waits

    def _commit(self, tok, reads, writes):
        for b in writes:
            b.lw = tok
            b.rd = {}
        for b in reads:
            if b in writes:
                continue
            if tok[0] == 'e':
                if b.rd.get(tok[1], -1) < tok[2]:
                    b.rd[tok[1]] = tok[2]
            else:
                b.rd[tok] = 1

    def op(self, eng, fn, reads=(), writes=(), strict=False):
        waits = self._collect(eng, reads, writes, strict)
        idx = len(self.streams[eng])
        self.streams[eng].append(dict(waits=waits, fn=fn, inc=False, dma=None))
        self._commit(('e', eng, idx), reads, writes)
        self.n_ops += 1

    def dma(self, q, out, in_, reads=(), writes=()):
        waits = self._collect(q, reads, writes)
        slot = self.slot_rr[q]
        self.slot_rr[q] = (slot + 1) % NSLOT
        prev = self.slot_last[q][slot]
        if prev is not None and prev not in self.dseen[q]:
            self.dseen[q].add(prev)
            waits.append(prev)
        self.slot_cnt[q][slot] += 1
        tok = ('d', q, slot, 16 * self.slot_cnt[q][slot])
        self.slot_last[q][slot] = tok
        self.streams[q].append(dict(waits=waits, fn=lambda e, o=out, i=in_: e.dma_start(out=o, in_=i),
                                    inc=False, dma=(q, slot)))
        self._commit(tok, reads, writes)
        self.n_ops += 1

    def barrier(self):
        lasts = {}
        for E in ENGS:
            for idx in range(len(self.streams[E]) - 1, -1, -1):
                if self.streams[E][idx]['fn'] is not None and self.streams[E][idx]['dma'] is None:
                    lasts[E] = idx
                    break
        dtoks = [t for q in ENGS for t in self.slot_last[q] if t is not None]
        for W in ENGS:
            waits = []
            for E, idx in lasts.items():
                if E == W:
                    continue
                if self.seen[W].get(E, -1) >= idx:
                    continue
                self.seen[W][E] = idx
                self.streams[E][idx]['inc'] = True
                waits.append(('e', E, idx))
            for t in dtoks:
                if t in self.dseen[W]:
                    continue
                self.dseen[W].add(t)
                waits.append(t)
            if waits:
                self.streams[W].append(dict(waits=waits, fn=None, inc=False, dma=None))

    def _pref(self):
        pref = {}
        for e in ENGS:
            c = 0
            arr = []
            for ent in self.streams[e]:
                if ent['inc']:
                    assert ent['dma'] is None and ent['fn'] is not None
                    c += 1
                arr.append(c)
            pref[e] = arr
        return pref

    def check(self):
        pref = self._pref()
        esem = {e: 0 for e in ENGS}
        dsem = {(q, i): 0 for q in ENGS for i in range(NSLOT)}
        pc = {e: 0 for e in ENGS}
        progress = True
        while progress:
            progress = False
            for e in ENGS:
                st = self.streams[e]
                while pc[e] < len(st):
                    ent = st[pc[e]]
                    ok = True
                    for w in ent['waits']:
                        if w[0] == 'e':
                            if esem[w[1]] < pref[w[1]][w[2]]:
                                ok = False
                        elif dsem[(w[1], w[2])] < w[3]:
                            ok = False
                    if not ok:
                        break
                    if ent['dma'] is not None:
                        dsem[ent['dma']] += 16
                    elif ent['inc']:
                        esem[e] += 1
                    pc[e] += 1
                    progress = True
        for e in ENGS:
            if pc[e] < len(self.streams[e]):
                raise RuntimeError(f"DEADLOCK: {e} stuck at {pc[e]}/{len(self.streams[e])}")

    def emit(self, es):
        self.check()
        nc = self.nc
        esem = {e: es.enter_context(nc.semaphore("s_" + e)) for e in ENGS}
        dsem = {q: [es.enter_context(nc.semaphore(f"d_{q}{i}")) for i in range(NSLOT)]
                for q in ENGS if any(self.slot_cnt[q])}
        pref = self._pref()
        block = es.enter_context(nc.Block())
        handles = {'pe': block.tensor, 'act': block.scalar, 'dve': block.vector, 'pool': block.gpsimd,
                   'sp': block.sync}
        for e in ENGS:
            stream = self.streams[e]
            if not stream:
                continue

            def body(h, stream=stream, e=e):
                for ent in stream:
                    for w in ent['waits']:
                        if w[0] == 'e':
                            h.wait_ge(esem[w[1]], pref[w[1]][w[2]])
                        else:
                            h.wait_ge(dsem[w[1]][w[2]], w[3])
                    if ent['fn'] is None:
                        continue
                    ins = ent['fn'](h)
                    if ent['dma'] is not None:
                        q, slot = ent['dma']
                        ins.then_inc(dsem[q][slot], 16)
                    elif ent['inc']:
                        ins.then_inc(esem[e], 1)
            handles[e](body)


class Ctx:
    def __init__(self, nc, es):
        self.nc = nc
        self.kb = KB(nc)
        self.arena = es.enter_context(nc.sbuf_tensor("arena", [128, ARENA_BYTES], U8))
        self.ps = es.enter_context(nc.psum_tensor("ps", [128, 4096], F32))
        self.off = 0
        self.persist_end = 0
        self.psb = bufs(8, 'ps')
        self.ps_rr = 0
        self.pool_rr = {}
        self.default_banks = list(range(8))
        self.tmp_rr = 0
        self.dram = {}

    def tile(self, free_shape, dt):
        n = int(np.prod(free_shape))
        esz = 4 if dt == F32 else 2
        nbytes = (n * esz + 31) // 32 * 32
        assert self.off + nbytes <= ARENA_BYTES, (self.off, nbytes)
        v = self.arena[:, self.off:self.off + n * esz].bitcast(dt)
        self.off += nbytes
        if len(free_shape) == 2:
            v = v.rearrange("p (a b) -> p a b", a=free_shape[0])
        elif len(free_shape) == 3:
            v = v.rearrange("p (a b c) -> p a b c", a=free_shape[0], b=free_shape[1])
        elif len(free_shape) == 4:
            v = v.rearrange("p (a b c d) -> p a b c d", a=free_shape[0], b=free_shape[1], c=free_shape[2])
        return v

    def end_persist(self):
        self.persist_end = self.off

    def new_phase(self):
        self.kb.barrier()
        self.off = self.persist_end
        self.psb = bufs(8, 'ps')
        self.default_banks = list(range(8))

    def psum(self, ncols, banks=None):
        assert ncols <= 512
        if banks is None:
            banks = self.default_banks
        key = tuple(banks)
        j = self.pool_rr.get(key, 0)
        self.pool_rr[key] = (j + 1) % len(banks)
        i = banks[j]
        return self.ps[:, i * 512:i * 512 + ncols], [self.psb[i]]


def mm(C, out, ob, lhsT, lb, rhs, rb, start, stop):
    C.kb.op('pe', lambda e: e.matmul(out, lhsT, rhs, start=start, stop=stop), reads=lb + rb, writes=ob)


def load_weight(C, dst, dbufs, src, kc_n, queue='pool'):
    sv = src.rearrange("(k p) n -> p k n", p=128)
    for k in range(kc_n):
        C.kb.dma(queue, dst[:, k, :], sv[:, k, :], writes=[dbufs[k]])


def phase_consts(C, T):
    kb = C.kb
    P = {}
    P['ones_bf'] = C.tile([128], BF16)
    P['ones_bf_b'] = Buf()
    P['one_row'] = C.tile([512], BF16)
    P['one_row_b'] = Buf()
    P['eps'] = C.tile([1], F32)
    P['eps_b'] = Buf()
    P['ident'] = C.tile([128], BF16)
    P['ident_b'] = Buf()
    kb.op('pool', lambda e: e.memset(P['ones_bf'], 1.0 / 1024), writes=[P['ones_bf_b']])
    kb.op('pool', lambda e: e.memset(P['one_row'], 1.0), writes=[P['one_row_b']])
    kb.op('pool', lambda e: e.memset(P['eps'], EPS), writes=[P['eps_b']])
    kb.op('pool', lambda e: e.memset(P['ident'], 0.0), writes=[P['ident_b']])
    kb.op('pool', lambda e: e.affine_select(out=P['ident'], in_=P['ident'], pattern=[[-1, 128]],
                                            compare_op=ALU.not_equal, fill=1.0, base=0, channel_multiplier=1),
          reads=[P['ident_b']], writes=[P['ident_b']])
    cv = C.tile([8, 2], F32)
    ca = C.tile([8, 2], F32)
    MOD = C.tile([4, 48, 2], F32)
    SC = C.tile([4, 6, 8, 2], F32)
    NG = C.tile([4, 4, 8], F32)
    AB = C.tile([4, 48], F32)
    P['SC'] = SC
    P['SC_b'] = Buf()
    C.end_persist()
    b_cv, b_ca, b_mod, b_ng, b_ab = Buf(), Buf(), Buf(), Buf(), Buf()
    kb.dma('sp', cv, T['cvec'][:, :, :], writes=[b_cv])
    kb.dma('sp', NG, T['norm_gT'][:, :, :, :], writes=[b_ng])
    kb.dma('sp', AB, T['ada_bT'][:, :, :], writes=[b_ab])
    kb.op('act', lambda e: e.activation(out=ca, in_=cv, func=AF.Silu), reads=[b_cv], writes=[b_ca])
    Wt = [C.tile([8, 768], F32) for _ in range(2)]
    Wb = bufs(2)
    it = 0
    for l in range(4):
        pst, pb = C.psum(96)
        for nb in range(8):
            w = Wt[it % 2]
            wb = Wb[it % 2]
            it += 1
            src = T['ada_w'][l].rearrange("(k p) n -> p k n", p=128)[:, :, nb * 768:(nb + 1) * 768]
            kb.dma('sp', w, src, writes=[wb])
            for j in range(6):
                n = nb * 6 + j
                for k in range(8):
                    mm(C, pst[:, 2 * n:2 * n + 2], pb, w[:, k, j * 128:(j + 1) * 128], [wb], ca[:, k, :], [b_ca],
                       k == 0, k == 7)
        pv = pst.rearrange("p (n w) -> p n w", w=2)
        kb.op('dve', lambda e, l=l, pv=pv: e.tensor_tensor(out=MOD[:, l], in0=pv,
                                                            in1=AB[:, l].unsqueeze(2).broadcast_to([128, 48, 2]),
                                                            op=ALU.add),
              reads=pb + [b_ab], writes=[b_mod], strict=True)

        def m(j6, l=l):
            return MOD[:, l, j6 * 8:(j6 + 1) * 8, :]

        def g(j, l=l):
            return NG[:, l, j, :].unsqueeze(2).broadcast_to([128, 8, 2])
        rd = [b_mod, b_ng]
        wr = [P['SC_b']]
        kb.op('dve', lambda e, l=l, m=m, g=g: e.scalar_tensor_tensor(out=SC[:, l, 0], in0=m(1), scalar=1.0, in1=g(0),
                                                                    op0=ALU.add, op1=ALU.mult), reads=rd, writes=wr, strict=True)
        kb.op('dve', lambda e, l=l, m=m: e.tensor_copy(out=SC[:, l, 1], in_=m(0)), reads=rd, writes=wr, strict=True)
        kb.op('dve', lambda e, l=l, m=m, g=g: e.tensor_tensor(out=SC[:, l, 2], in0=m(2), in1=g(1), op=ALU.mult),
              reads=rd, writes=wr, strict=True)
        kb.op('dve', lambda e, l=l, m=m, g=g: e.scalar_tensor_tensor(out=SC[:, l, 3], in0=m(4), scalar=1.0, in1=g(2),
                                                                    op0=ALU.add, op1=ALU.mult), reads=rd, writes=wr, strict=True)
        kb.op('dve', lambda e, l=l, m=m: e.tensor_copy(out=SC[:, l, 4], in_=m(3)), reads=rd, writes=wr, strict=True)
        kb.op('dve', lambda e, l=l, m=m, g=g: e.tensor_tensor(out=SC[:, l, 5], in0=m(5), in1=g(3), op=ALU.mult),
              reads=rd, writes=wr, strict=True)
    return P


def sc(P, l, kind, c, w):
    return P['SC'][:, l, kind, c, w:w + 1]


class Small:
    def __init__(self, C, n, free, dt):
        self.t = [C.tile(free, dt) for _ in range(n)]
        self.b = bufs(n)
        self.i = 0

    def get(self):
        i = self.i
        self.i = (i + 1) % len(self.t)
        return self.t[i], self.b[i]


def rstd_from_psum(C, P, pst, pb, RS):
    rs, rb = RS.get()
    n = pst.shape[-1]
    rs = rs[:, 0:n]
    C.kb.op('act', lambda e: e.activation(out=rs, in_=pst, func=AF.Sqrt, bias=P['eps'][:, 0:1]),
            reads=pb + [P['eps_b']], writes=[rb])
    C.kb.op('dve', lambda e: e.reciprocal(out=rs, in_=rs), reads=[rb], writes=[rb])
    return rs, rb


def prenorm(C, P, XL, XLb, n, H, Hb, l, kA, kB, w, SQ, RS, TMP):
    kb = C.kb
    pst, pb = C.psum(n)
    sq = []
    for c in range(8):
        s, sb_ = SQ.get()
        s = s[:, 0:n]
        kb.op('act', lambda e, s=s, c=c: e.activation(out=s, in_=XL[:, c, 0:n], func=AF.Square), reads=[XLb],
              writes=[sb_])
        sq.append((s, sb_))
        if c >= 1:
            s0, sb0 = sq[c - 1]
            mm(C, pst, pb, P['ones_bf'], [P['ones_bf_b']], s0, [sb0], c - 1 == 0, False)
    s0, sb0 = sq[7]
    mm(C, pst, pb, P['ones_bf'], [P['ones_bf_b']], s0, [sb0], False, True)
    rs, rb = rstd_from_psum(C, P, pst, pb, RS)
    for c in range(8):
        t, tb = TMP.get()
        t = t[:, 0:n]
        kb.op('dve', lambda e, t=t, c=c: e.scalar_tensor_tensor(out=t, in0=XL[:, c, 0:n], scalar=sc(P, l, kA, c, w),
                                                               in1=rs, op0=ALU.mult, op1=ALU.mult),
              reads=[XLb, rb, P['SC_b']], writes=[tb])
        kb.op('act', lambda e, t=t, c=c: e.activation(out=H[:, c, 0:n], in_=t, func=AF.Identity,
                                                      bias=sc(P, l, kB, c, w)),
              reads=[tb, P['SC_b']], writes=[Hb[c]])


class PostRes:
    def __init__(self, C, P, n, Yf, Yfb, SQ, RS, TMP, XR):
        self.C, self.P, self.n = C, P, n
        self.Yf, self.Yfb, self.SQ, self.RS, self.TMP, self.XR = Yf, Yfb, SQ, RS, TMP, XR
        self.pst, self.pb = C.psum(n)
        self.pending = None
        self.cnt = 0

    def _flush(self, last):
        if self.pending is not None:
            s0, sb0 = self.pending
            n = self.n
            for h in range(2):
                mm(self.C, self.pst, self.pb, self.P['ones_bf'], [self.P['ones_bf_b']], s0[:, h * n:(h + 1) * n], [sb0],
                   self.cnt == 0, last and h == 1)
                self.cnt += 1
            self.pending = None

    def pair(self, m, yp, ypb, bias=None, bias_b=None):
        C, n = self.C, self.n
        self._flush(False)
        s, sb_ = self.SQ.get()
        s = s[:, 0:2 * n]
        if bias is None:
            yv = yp.rearrange("p (a t) -> p a t", a=2)
            C.kb.op('act', lambda e: e.activation(out=self.Yf[:, m:m + 2, 0:n], in_=yv, func=AF.Identity), reads=ypb,
                    writes=[self.Yfb[m], self.Yfb[m + 1]])
            C.kb.op('act', lambda e: e.activation(out=s, in_=yp, func=AF.Square), reads=ypb, writes=[sb_])
        else:
            for h in range(2):
                C.kb.op('act', lambda e, h=h: e.activation(out=self.Yf[:, m + h, 0:n], in_=yp[:, h * n:(h + 1) * n],
                                                          func=AF.Identity, bias=bias[:, m + h:m + h + 1]),
                        reads=ypb + [bias_b], writes=[self.Yfb[m + h]])
                C.kb.op('act', lambda e, h=h: e.activation(out=s[:, h * n:(h + 1) * n], in_=yp[:, h * n:(h + 1) * n],
                                                          func=AF.Square, bias=bias[:, m + h:m + h + 1]),
                        reads=ypb + [bias_b], writes=[sb_])
        self.pending = (s, sb_)

    def finish(self, l, kG, w, src, dst, col0, srcb, dstb):
        C, P, n = self.C, self.P, self.n
        self._flush(True)
        rs, rb = rstd_from_psum(C, P, self.pst, self.pb, self.RS)
        for c in range(8):
            xr, xrb = self.XR.get()
            xr = xr[:, 0:n]
            C.kb.dma('sp', xr, src[c * 128:(c + 1) * 128, col0:col0 + n], reads=srcb, writes=[xrb])
            t, tb = self.TMP.get()
            t = t[:, 0:n]
            C.kb.op('dve', lambda e, t=t, c=c: e.scalar_tensor_tensor(out=t, in0=self.Yf[:, c, 0:n],
                                                                   scalar=sc(P, l, kG, c, w), in1=rs,
                                                                   op0=ALU.mult, op1=ALU.mult),
                    reads=[self.Yfb[c], rb, P['SC_b']], writes=[tb])
            C.kb.op('dve', lambda e, t=t, xr=xr: e.tensor_tensor(out=xr, in0=xr, in1=t, op=ALU.add),
                    reads=[tb, xrb], writes=[xrb])
            C.kb.dma('sp', dst[c * 128:(c + 1) * 128, col0:col0 + n], xr, reads=[xrb], writes=dstb)


MAXG = None
NA_PARTS = 3
STOP = 99
FULLBANK = False
SUB = 99


def groups(G, with_ctx=True, full=False):
    if MAXG is not None and not full:
        return [(g * G, G, 0) for g in range(MAXG)] + ([(NL, min(G, NCX), 1)] if with_ctx else [])
    out = []
    for g in range(NL // G):
        out.append((g * G, G, 0))
    if with_ctx:
        for g in range(NCX // min(G, NCX)):
            n = min(G, NCX)
            out.append((NL + g * n, n, 1))
    return out


def xbufs(T, key, col0, n):
    return T[key + '_b'][col0 // 128:(col0 + n) // 128]


def phase_ffn(C, P, T, l, src, dst, with_ctx, final=False):
    kb = C.kb
    C.new_phase()
    G = 256
    W1 = C.tile([8, 4096], BF16)
    W2 = C.tile([32, 1024], BF16)
    W1b, W2b = bufs(8), bufs(32)
    load_weight(C, W1, W1b, T['ffn_w1'][l], 8)
    load_weight(C, W2, W2b, T['ffn_w2'][l], 32)
    XL = [C.tile([8, G], F32) for _ in range(2)]
    XLb = bufs(2)
    H = C.tile([8, G], BF16)
    Hb = bufs(8)
    H1 = C.tile([32, G], BF16)
    H1b = bufs(32)
    Yf = C.tile([8, G], F32)
    Yfb = bufs(8)
    SQ = Small(C, 3, [2 * G], BF16)
    RS = Small(C, 2, [G], F32)
    TMP = Small(C, 4, [G], F32)
    XR = Small(C, 4, [G], F32)
    RL = Small(C, 3, [2 * G], F32)
    grp = groups(G, with_ctx)
    srcv = T[src].rearrange("(c p) t -> p c t", p=128)

    def load(i):
        col0, n, w = grp[i]
        kb.dma('sp', XL[i % 2][:, :, 0:n], srcv[:, :, col0:col0 + n], reads=xbufs(T, src, col0, n),
               writes=[XLb[i % 2]])
    load(0)
    for i, (col0, n, w) in enumerate(grp):
        if i + 1 < len(grp):
            load(i + 1)
        prenorm(C, P, XL[i % 2], XLb[i % 2], n, H, Hb, l, 3, 4, w, SQ, RS, TMP)
        for mp in range(16):
            pt, pb = C.psum(2 * n)
            for h in range(2):
                m = 2 * mp + h
                for k in range(8):
                    mm(C, pt[:, h * n:(h + 1) * n], pb, W1[:, k, m * 128:(m + 1) * 128], [W1b[k]], H[:, k, 0:n],
                       [Hb[k]], k == 0, k == 7)
            r, rb = RL.get()
            r = r[:, 0:2 * n]
            kb.op('act', lambda e, r=r, pt=pt: e.activation(out=r, in_=pt, func=AF.Relu), reads=pb, writes=[rb])
            rv = r.rearrange("p (a t) -> p a t", a=2)
            kb.op('dve', lambda e, rv=rv, mp=mp: e.tensor_tensor(out=H1[:, 2 * mp:2 * mp + 2, 0:n], in0=rv, in1=rv,
                                                                 op=ALU.mult),
                  reads=[rb], writes=[H1b[2 * mp], H1b[2 * mp + 1]])
        pr = PostRes(C, P, n, Yf, Yfb, SQ, RS, TMP, XR)
        for mp in range(4):
            pt, pb = C.psum(2 * n)
            for h in range(2):
                m = 2 * mp + h
                for k in range(32):
                    mm(C, pt[:, h * n:(h + 1) * n], pb, W2[:, k, m * 128:(m + 1) * 128], [W2b[k]], H1[:, k, 0:n],
                       [H1b[k]], k == 0, k == 31)
            pr.pair(2 * mp, pt, pb)
        if final and w == 0:
            pr.finish(l, 5, w, T[src], T['yout'], col0, xbufs(T, src, col0, n), xbufs(T, 'yout', col0, n))
        else:
            pr.finish(l, 5, w, T[src], T[dst], col0, xbufs(T, src, col0, n), xbufs(T, dst, col0, n))


GELU_A = math.sqrt(0.044715)
GELU_B = 2.0 * math.sqrt(2.0 / math.pi)


def gelu_psum(C, zp, zpb, out, outb, GT, a=1):
    kb = C.kb
    n = zp.shape[-1]
    t, tb = GT.get()
    t = t[:, 0:n]
    if a > 1:
        t = t.rearrange("p (a t) -> p a t", a=a)
        zp = zp.rearrange("p (a t) -> p a t", a=a)
    kb.op('act', lambda e: e.activation(out=t, in_=zp, func=AF.Square, scale=GELU_A), reads=zpb, writes=[tb])
    kb.op('dve', lambda e: e.scalar_tensor_tensor(out=t, in0=t, scalar=1.0, in1=zp, op0=ALU.add, op1=ALU.mult),
          reads=zpb + [tb], writes=[tb])
    kb.op('act', lambda e: e.activation(out=t, in_=t, func=AF.Sigmoid, scale=GELU_B), reads=[tb], writes=[tb])
    kb.op('dve', lambda e: e.tensor_tensor(out=out, in0=t, in1=zp, op=ALU.mult), reads=zpb + [tb], writes=outb)


def phase_gmlp_a(C, P, T, l, j, src, with_ctx):
    kb = C.kb
    C.new_phase()
    G = 256
    Win = C.tile([8, 6144], BF16)
    Winb = bufs(8)
    load_weight(C, Win, Winb, T['gm_w_in'][j], 8)
    brow = C.tile([6144], BF16)
    browb = Buf()
    kb.dma('pool', brow[0:1, :], T['gm_b_in'][j:j + 1, :], writes=[browb])
    wsT = C.tile([8, 128], BF16)
    wsTb = Buf()
    kb.dma('pool', wsT, T['gm_wsT'][j].rearrange("q (g p) -> q g p", g=8), writes=[wsTb])
    bsbc = C.tile([8, 128], F32)
    bsbcb = Buf()
    kb.dma('sp', bsbc, T['gm_bs'][j:j + 1, :].partition_broadcast(128).rearrange("q (g p) -> q g p", g=8)
           if False else T['gm_bs_bc'][j].rearrange("q (g p) -> q g p", g=8), writes=[bsbcb])
    lng = C.tile([24], F32)
    lngb = Buf()
    kb.dma('sp', lng, T['gm_ln_gT'][j], writes=[lngb])
    XL = [C.tile([8, G], F32) for _ in range(1)]
    XLb = bufs(1)
    H = C.tile([8, G], BF16)
    Hb = bufs(8)
    U = [C.tile([24, G], BF16) for _ in range(2)]
    Ub = [bufs(24), bufs(24)]
    ZG = [C.tile([3072], F32) for _ in range(1)]
    ZGb = [bufs(6) for _ in range(1)]
    VH = [C.tile([3072], BF16) for _ in range(2)]
    VHb = bufs(2)
    MV = Small(C, 2, [8], F32)
    SQ = Small(C, 3, [G], BF16)
    RS = Small(C, 2, [G], F32)
    TMP = Small(C, 3, [G], F32)
    GT = Small(C, 3, [512], F32)
    grp = groups(G, with_ctx)
    srcv = T[src].rearrange("(c p) t -> p c t", p=128)
    pdv = T['pd'].rearrange("(c p) t -> p c t", p=128)
    vit = 0
    for i, (col0, n, w) in enumerate(grp):
        kb.dma('sp', XL[0][:, :, 0:n], srcv[:, :, col0:col0 + n], reads=xbufs(T, src, col0, n), writes=[XLb[0]])
        prenorm(C, P, XL[0], XLb[0], n, H, Hb, l, 0, 1, w, SQ, RS, TMP)
        Ui, Ubi = U[i % 2], Ub[i % 2]
        vhs = []
        for s in range(n // 128):
            zg, zgb = ZG[0], ZGb[0]
            for q in range(6):
                pt, pb = C.psum(512)
                for k in range(8):
                    mm(C, pt, pb, H[:, k, s * 128:(s + 1) * 128], [Hb[k]],
                       Win[:, k, 3072 + q * 512:3072 + (q + 1) * 512], [Winb[k]], k == 0, False)
                mm(C, pt, pb, P['one_row'][0:1, 0:128], [P['one_row_b']],
                   brow[0:1, 3072 + q * 512:3072 + (q + 1) * 512], [browb], False, True)
                gelu_psum(C, pt, pb, zg[:, q * 512:(q + 1) * 512], [zgb[q]], GT)
            mv, mvb = MV.get()
            vh, vhb = VH[vit % 2], VHb[vit % 2]
            vit += 1
            kb.op('dve', lambda e, mv=mv, zg=zg: e.reduce_sum(out=mv[:, 0:1], in_=zg, axis=mybir.AxisListType.X),
                  reads=zgb, writes=[mvb], strict=True)
            kb.op('act', lambda e, mv=mv, zg=zg, vh=vh: e.activation(out=vh, in_=zg, func=AF.Square,
                                                                   accum_out=mv[:, 1:2]),
                  reads=zgb + [mvb], writes=[mvb, vhb], strict=True)
            kb.op('dve', lambda e, mv=mv: e.tensor_scalar(out=mv[:, 2:3], in0=mv[:, 0:1], scalar1=1.0 / 3072,
                                                          scalar2=None, op0=ALU.mult), reads=[mvb], writes=[mvb], strict=True)
            kb.op('dve', lambda e, mv=mv: e.scalar_tensor_tensor(out=mv[:, 3:4], in0=mv[:, 2:3], scalar=-1.0,
                                                                 in1=mv[:, 2:3], op0=ALU.mult, op1=ALU.mult),
                  reads=[mvb], writes=[mvb], strict=True)
            kb.op('dve', lambda e, mv=mv: e.scalar_tensor_tensor(out=mv[:, 4:5], in0=mv[:, 1:2], scalar=1.0 / 3072,
                                                                 in1=mv[:, 3:4], op0=ALU.mult, op1=ALU.add),
                  reads=[mvb], writes=[mvb], strict=True)
            kb.op('act', lambda e, mv=mv: e.activation(out=mv[:, 5:6], in_=mv[:, 4:5], func=AF.Sqrt,
                                                       bias=P['eps'][:, 0:1]), reads=[mvb, P['eps_b']], writes=[mvb], strict=True)
            kb.op('dve', lambda e, mv=mv: e.reciprocal(out=mv[:, 5:6], in_=mv[:, 5:6]), reads=[mvb], writes=[mvb], strict=True)
            kb.op('dve', lambda e, mv=mv: e.scalar_tensor_tensor(out=mv[:, 6:7], in0=mv[:, 2:3], scalar=-1.0,
                                                                 in1=mv[:, 5:6], op0=ALU.mult, op1=ALU.mult),
                  reads=[mvb], writes=[mvb], strict=True)
            kb.op('act', lambda e, vh=vh, zg=zg, mv=mv: e.activation(out=vh, in_=zg, func=AF.Identity,
                                                                   scale=mv[:, 5:6], bias=mv[:, 6:7]),
                  reads=zgb + [mvb], writes=[vhb])
            vhs.append((vh, vhb))
            if 'dbg_vh' in T and w == 1 and s == 1:
                kb.dma('sp', T['dbg_vh'][:, :], vh, reads=[vhb], writes=[Buf()])
                kb.dma('sp', T['dbg_mv'][:, :], mv, reads=[mvb], writes=[Buf()])
                kb.dma('sp', T['dbg_zg'][:, :], zg, reads=zgb, writes=[Buf()])
        for mp in range(12):
            pt, pb = C.psum(2 * n)
            for h in range(2):
                m = 2 * mp + h
                po = pt[:, h * n:(h + 1) * n]
                for k in range(8):
                    mm(C, po, pb, Win[:, k, m * 128:(m + 1) * 128], [Winb[k]], H[:, k, 0:n], [Hb[k]], k == 0, False)
                mm(C, po, pb, brow[0:1, m * 128:(m + 1) * 128], [browb], P['one_row'][0:1, 0:n], [P['one_row_b']],
                   False, True)
            gelu_psum(C, pt, pb, Ui[:, 2 * mp:2 * mp + 2, 0:n], [Ubi[2 * mp], Ubi[2 * mp + 1]], GT, a=2)
        for s in range(n // 128):
            vh, vhb = vhs[s]
            for c4 in range(6):
                pt, pb = C.psum(512)
                for jj in range(4):
                    cc = c4 * 4 + jj
                    mm(C, pt[:, jj * 128:(jj + 1) * 128], pb, vh[:, cc * 128:(cc + 1) * 128], [vhb],
                       wsT[:, cc // 3, :], [wsTb], True, True)
                t, tb = GT.get()
                for jj in range(4):
                    cc = c4 * 4 + jj
                    kb.op('dve', lambda e, t=t, pt=pt, cc=cc, jj=jj: e.scalar_tensor_tensor(
                        out=t[:, jj * 128:(jj + 1) * 128], in0=pt[:, jj * 128:(jj + 1) * 128],
                        scalar=lng[:, cc:cc + 1], in1=bsbc[:, cc // 3, :], op0=ALU.mult, op1=ALU.add),
                        reads=pb + [lngb, bsbcb], writes=[tb])
                tv = t.rearrange("p (a t) -> p a t", a=4)
                kb.op('pool', lambda e, tv=tv, c4=c4, s=s, Ui=Ui: e.tensor_tensor(
                    out=Ui[:, c4 * 4:c4 * 4 + 4, s * 128:(s + 1) * 128],
                    in0=Ui[:, c4 * 4:c4 * 4 + 4, s * 128:(s + 1) * 128], in1=tv, op=ALU.mult),
                    reads=[tb] + Ubi[c4 * 4:c4 * 4 + 4], writes=Ubi[c4 * 4:c4 * 4 + 4])
        kb.dma('sp', pdv[:, :, col0:col0 + n], Ui[:, :, 0:n], reads=Ubi, writes=xbufs(T, 'pd', col0, n))


def phase_gmlp_b(C, P, T, l, j, src, dst, with_ctx):
    kb = C.kb
    C.new_phase()
    G = 256
    Wo = C.tile([24, 1024], BF16)
    Wob = bufs(24)
    load_weight(C, Wo, Wob, T['gm_w_out'][j], 24)
    PT = [C.tile([24, G], BF16) for _ in range(2)]
    PTb = bufs(2)
    Yf = C.tile([8, G], F32)
    Yfb = bufs(8)
    SQ = Small(C, 3, [2 * G], BF16)
    RS = Small(C, 2, [G], F32)
    TMP = Small(C, 4, [G], F32)
    XR = Small(C, 4, [G], F32)
    grp = groups(G, with_ctx)
    pdv = T['pd'].rearrange("(c p) t -> p c t", p=128)

    def load(i):
        col0, n, w = grp[i]
        kb.dma('sp', PT[i % 2][:, :, 0:n], pdv[:, :, col0:col0 + n], reads=xbufs(T, 'pd', col0, n),
               writes=[PTb[i % 2]])
    load(0)
    for i, (col0, n, w) in enumerate(grp):
        if i + 1 < len(grp):
            load(i + 1)
        pr = PostRes(C, P, n, Yf, Yfb, SQ, RS, TMP, XR)
        for mp in range(4):
            pt, pb = C.psum(2 * n)
            for h in range(2):
                m = 2 * mp + h
                for k in range(24):
                    mm(C, pt[:, h * n:(h + 1) * n], pb, Wo[:, k, m * 128:(m + 1) * 128], [Wob[k]],
                       PT[i % 2][:, k, 0:n], [PTb[i % 2]], k == 0, k == 23)
            pr.pair(2 * mp, pt, pb)
        pr.finish(l, 2, w, T[src], T[dst], col0, xbufs(T, src, col0, n), xbufs(T, dst, col0, n))


def phase_na_a(C, P, T, l, src):
    kb = C.kb
    C.new_phase()
    G = 256
    W = C.tile([8, 3072], BF16)
    Wb = bufs(8)
    load_weight(C, W, Wb, T['na_w_qkv'][0], 8)
    bqk = C.tile([24], F32)
    bqkb = Buf()
    kb.dma('sp', bqk, T['na_bqkvT'][:, :], writes=[bqkb])
    bq8 = C.tile([8], F32)
    kb.op('dve', lambda e: e.tensor_scalar(out=bq8, in0=bqk[:, 0:8], scalar1=0.125, scalar2=None, op0=ALU.mult),
          reads=[bqkb], writes=[bqkb], strict=True)
    brow = C.tile([1024], BF16)
    browb = Buf()
    kb.dma('pool', brow[0:1, :], T['na_b_qkv'][0:1, 2048:3072], writes=[browb])
    XL = [C.tile([8, G], F32) for _ in range(2)]
    XLb = bufs(2)
    H = C.tile([8, G], BF16)
    Hb = bufs(8)
    QT = [C.tile([8, G], BF16) for _ in range(2)]
    QTb = bufs(2)
    KT = [C.tile([8, G], BF16) for _ in range(2)]
    KTb = bufs(2)
    Vt = [C.tile([16, 65], BF16) for _ in range(3)]
    Vtb = bufs(3)
    for i in range(3):
        kb.op('pool', lambda e, i=i: e.memset(Vt[i], 1.0), writes=[Vtb[i]])
    SQ = Small(C, 3, [G], BF16)
    RS = Small(C, 2, [G], F32)
    TMP = Small(C, 3, [G], F32)
    grp = groups(G, True, full=True)
    srcv = T[src].rearrange("(c p) t -> p c t", p=128)
    qv = T['qT'].rearrange("(c p) t -> p c t", p=128)
    kv = T['kT'].rearrange("(c p) t -> p c t", p=128)

    def load(i):
        col0, n, w = grp[i]
        kb.dma('sp', XL[i % 2][:, :, 0:n], srcv[:, :, col0:col0 + n], reads=xbufs(T, src, col0, n),
               writes=[XLb[i % 2]])
    load(0)
    vi = 0
    for i, (col0, n, w) in enumerate(grp):
        if i + 1 < len(grp):
            load(i + 1)
        prenorm(C, P, XL[i % 2], XLb[i % 2], n, H, Hb, l, 0, 1, w, SQ, RS, TMP)
        for which, dstT, dstb in ((0, QT[i % 2], QTb[i % 2]), (1, KT[i % 2], KTb[i % 2])):
            for mp in range(4):
                pt, pb = C.psum(2 * n)
                for h in range(2):
                    m = 2 * mp + h
                    mcol = which * 1024 + m * 128
                    for k in range(8):
                        mm(C, pt[:, h * n:(h + 1) * n], pb, W[:, k, mcol:mcol + 128], [Wb[k]], H[:, k, 0:n], [Hb[k]],
                           k == 0, k == 7)
                for h in range(2):
                    m = 2 * mp + h
                    if which == 0:
                        kb.op('act', lambda e, pt=pt, h=h, m=m, dstT=dstT: e.activation(
                            out=dstT[:, m, 0:n], in_=pt[:, h * n:(h + 1) * n], func=AF.Identity, scale=0.125,
                            bias=bq8[:, m:m + 1]), reads=pb + [bqkb], writes=[dstb])
                    else:
                        kb.op('act', lambda e, pt=pt, h=h, m=m, dstT=dstT: e.activation(
                            out=dstT[:, m, 0:n], in_=pt[:, h * n:(h + 1) * n], func=AF.Identity,
                            bias=bqk[:, 8 + m:9 + m]), reads=pb + [bqkb], writes=[dstb])
        kb.dma('sp', qv[:, :, col0:col0 + n], QT[i % 2][:, :, 0:n], reads=[QTb[i % 2]], writes=xbufs(T, 'qT', col0, n))
        kb.dma('sp', kv[:, :, col0:col0 + n], KT[i % 2][:, :, 0:n], reads=[KTb[i % 2]], writes=xbufs(T, 'kT', col0, n))
        for s in range(n // 128):
            vt, vtb = Vt[vi % 3], Vtb[vi % 3]
            vi += 1
            for nq in range(2):
                pt, pb = C.psum(512)
                for k in range(8):
                    mm(C, pt, pb, H[:, k, s * 128:(s + 1) * 128], [Hb[k]],
                       W[:, k, 2048 + nq * 512:2048 + (nq + 1) * 512], [Wb[k]], k == 0, False)
                mm(C, pt, pb, P['one_row'][0:1, 0:128], [P['one_row_b']], brow[0:1, nq * 512:(nq + 1) * 512], [browb],
                   False, True)
                kb.op('act', lambda e, pt=pt, vt=vt, nq=nq: e.activation(
                    out=vt[:, nq * 8:(nq + 1) * 8, 0:64], in_=pt.rearrange("p (h d) -> p h d", h=8), func=AF.Identity),
                    reads=pb, writes=[vtb])
            t0 = col0 + s * 128
            kb.dma('sp', T['vD'][t0:t0 + 128, :], vt.rearrange("p h d -> p (h d)"), reads=[vtb],
                   writes=xbufs(T, 'vD', t0, 128))


NA_NEG = -30000.0


def na_bias_table(rpb):
    rpb = np.asarray(rpb, dtype=np.float32)
    qc = np.arange(64)[None, :]
    kc = np.arange(64)[:, None]
    ws = np.clip(qc - 8, 0, 48)
    valid = (kc >= ws) & (kc < ws + 16)
    dcol = np.clip(kc - qc, -15, 15) + 15
    tab = np.full((2, 64, 16, 16, 64), NA_NEG, dtype=np.float32)

    def fill(half, pid, d):
        vals = rpb[:, d, :][:, dcol]
        vals = np.where(valid[None], vals, NA_NEG)
        tab[half, :, pid, :, :] = vals.transpose(1, 0, 2)
    for d in range(14):
        fill(0, d, d)
        fill(1, d, d + 1)
    fill(1, 14, 3)
    fill(0, 15, 10)
    return np.ascontiguousarray(tab.reshape(128, 16 * 16 * 64))


def phase_na_b(C, P, T, l, src, dst):
    kb = C.kb
    C.new_phase()
    Wo = C.tile([8, 1024], BF16)
    Wob = bufs(8)
    load_weight(C, Wo, Wob, T['na_w_o'][0], 8)
    bo = C.tile([8], F32)
    bob = Buf()
    kb.dma('sp', bo, T['na_b_oT'][:, :], writes=[bob])
    BT = C.tile([16, 16, 64], BF16)
    BTb = bufs(16)
    btv = T['na_bt'].rearrange("p (i x) -> p i x", i=16)
    BTf = BT.rearrange("p i h q -> p i (h q)")
    for i in range(16):
        kb.dma('pool', BTf[:, i, :], btv[:, i, :], writes=[BTb[i]])
    KcT = C.tile([8, 256], BF16)
    Vc = C.tile([2, 1040], BF16)
    Kcb, Vcb = Buf(), Buf()
    kvd = T['kT'].rearrange("(c p) t -> p c t", p=128)
    qvd = T['qT'].rearrange("(c p) t -> p c t", p=128)
    kb.dma('sp', KcT, kvd[:, :, NL:NT], reads=xbufs(T, 'kT', NL, NCX), writes=[Kcb])
    kb.dma('sp', Vc, T['vD'][NL:NT, :].rearrange("(j p) c -> p j c", p=128), reads=xbufs(T, 'vD', NL, NCX),
           writes=[Vcb])
    KT = [C.tile([8, 1024], BF16) for _ in range(2)]
    KTb = bufs(2)
    Vw = [C.tile([8, 1040], BF16) for _ in range(2)]
    Vwb = bufs(2)
    QT = [C.tile([8, 512], BF16) for _ in range(2)]
    QTb = bufs(2)
    PTs = Small(C, 3, [448], BF16)
    Ot = Small(C, 2, [1024], BF16)
    RD = Small(C, 3, [8], F32)
    OT = [C.tile([8, 256], BF16) for _ in range(2)]
    OTb = bufs(2)
    Yf = C.tile([8, 256], F32)
    Yfb = bufs(8)
    SQ = Small(C, 3, [512], BF16)
    RS = Small(C, 2, [256], F32)
    TMP = Small(C, 4, [256], F32)
    XR = Small(C, 4, [256], F32)
    S_BANKS = [0, 1, 2]
    PV_BANKS = [3, 4, 5]
    C.default_banks = [6, 7]
    ident = P['ident']
    psT_all = C.ps.bitcast(BF16)

    def attend_block(qt, qtb, qcol, lat_tiles, ot, otb):
        nt = len(lat_tiles)
        ntot = nt + 2
        hgroups = [list(range(0, 7)), list(range(7, 14)), list(range(14, 16))]
        pend = None

        def do_pv(pt, ptb, po, pvb, h):
            for t, (ktf, ktb, bid, vf, vb) in enumerate(lat_tiles):
                mm(C, po, pvb, pt[:, t * 64:(t + 1) * 64], [ptb], vf(h), [vb], t == 0, False)
            for c in range(2):
                mm(C, po, pvb, pt[:, (nt + c) * 64:(nt + c + 1) * 64], [ptb], Vc[:, c, h * 65:(h + 1) * 65], [Vcb],
                   (nt == 0 and c == 0), c == 1)

        def do_norm(pv, pvb, hg):
            nh = len(hg)
            rd, rdb = RD.get()
            pv3 = pv[0:64, :].rearrange("p (h d) -> p h d", d=65)
            kb.op('dve', lambda e: e.reciprocal(out=rd[0:64, 0:nh], in_=pv3[:, :, 64]), reads=pvb, writes=[rdb])
            h0 = hg[0]
            kb.op('dve', lambda e: e.tensor_tensor(
                out=ot[0:64, h0 * 64:(h0 + nh) * 64].rearrange("p (h d) -> p h d", d=64), in0=pv3[:, :, 0:64],
                in1=rd[0:64, 0:nh].unsqueeze(2).broadcast_to([64, nh, 64]), op=ALU.mult),
                reads=pvb + [rdb], writes=[otb], strict=True)

        for hg in hgroups:
            pv, pvb = C.psum(len(hg) * 65, banks=PV_BANKS)
            for hh, h in enumerate(hg):
                hp, ch = h % 2, h // 2
                q_ap = qt[hp * 64:(hp + 1) * 64, ch, qcol:qcol + 64]
                st, stb = C.psum(ntot * 64, banks=S_BANKS)
                for t, (ktf, ktb, bid, vf, vb) in enumerate(lat_tiles):
                    mm(C, st[:, t * 64:(t + 1) * 64], stb, ktf(h), [ktb], q_ap, [qtb], True, False)
                    mm(C, st[:, t * 64:(t + 1) * 64], stb, ident, [P['ident_b']], BT[:, bid, h, :], [BTb[bid]],
                       False, True)
                for c in range(2):
                    mm(C, st[:, (nt + c) * 64:(nt + c + 1) * 64], stb,
                       KcT[hp * 64:(hp + 1) * 64, ch, c * 128:(c + 1) * 128], [Kcb], q_ap, [qtb], True, True)
                pt, ptb = PTs.get()
                pt = pt[:, 0:ntot * 64]
                kb.op('act', lambda e, pt=pt, st=st: e.activation(out=pt, in_=st, func=AF.Exp), reads=stb, writes=[ptb])
                if pend is not None:
                    do_pv(*pend[0])
                    if pend[1] is not None:
                        do_norm(*pend[1])
                last_in_group = hh == len(hg) - 1
                pend = ((pt, ptb, pv[0:64, hh * 65:(hh + 1) * 65], pvb, h), (pv, pvb, hg) if last_in_group else None)
        do_pv(*pend[0])
        do_norm(*pend[1])

    def transpose_block(ot, otb, OTt, OTtb, slot):
        pt, pb = C.psum(256)
        bank = int(pb[0].name[2:])
        ptb16 = psT_all[:, bank * 1024:bank * 1024 + 512]
        for c in range(8):
            C.kb.op('pe', lambda e, c=c: e.transpose(ptb16[:, c * 64:(c + 1) * 64], ot[0:64, c * 128:(c + 1) * 128],
                                                     ident[0:64, 0:64]),
                    reads=[otb, P['ident_b']], writes=pb)
        kb.op('act', lambda e: e.activation(out=OTt[:, :, slot * 64:(slot + 1) * 64],
                                            in_=ptb16.rearrange("p (c t) -> p c t", c=8), func=AF.Identity),
              reads=pb, writes=[OTtb])

    def project(OTt, OTtb, n, w, col0):
        pr = PostRes(C, P, n, Yf, Yfb, SQ, RS, TMP, XR)
        for mp in range(4):
            pt, pb = C.psum(2 * n, banks=PV_BANKS + S_BANKS)
            for h in range(2):
                m = 2 * mp + h
                for k in range(8):
                    mm(C, pt[:, h * n:(h + 1) * n], pb, Wo[:, k, m * 128:(m + 1) * 128], [Wob[k]], OTt[:, k, 0:n],
                       [OTtb], k == 0, k == 7)
            pr.pair(2 * mp, pt, pb, bias=bo, bias_b=bob)
        pr.finish(l, 2, w, T[src], T[dst], col0, xbufs(T, src, col0, n), xbufs(T, dst, col0, n))

    ngroups = 16 if MAXG is None else MAXG
    oti = 0
    for g in range(ngroups):
        lo = max(0, 8 * g - 4)
        hi = min(128, 8 * g + 12)
        kt, ktb = KT[g % 2], KTb[g % 2]
        vw, vwb = Vw[g % 2], Vwb[g % 2]
        qt, qtb = QT[g % 2], QTb[g % 2]
        nw = (hi - lo) * 64
        kb.dma('sp', kt[:, :, 0:nw], kvd[:, :, lo * 64:hi * 64], reads=xbufs(T, 'kT', lo * 64, nw), writes=[ktb])
        kb.dma('sp', vw[:, 0:nw // 128, :], T['vD'][lo * 64:hi * 64, :].rearrange("(j p) c -> p j c", p=128),
               reads=xbufs(T, 'vD', lo * 64, nw), writes=[vwb])
        kb.dma('sp', qt, qvd[:, :, g * 512:(g + 1) * 512], reads=xbufs(T, 'qT', g * 512, 512), writes=[qtb])
        for rl in range(8):
            r = 8 * g + rl
            rs = min(max(r - 4, 0), 120)
            d0 = rs - r + 7
            tiles = []
            if rs % 2 == 0:
                for i4 in range(4):
                    tiles.append(((rs - lo) // 2 + i4, d0 + 2 * i4))
            else:
                assert d0 == 3
                j0 = (rs - 1 - lo) // 2
                tiles = [(j0, 14), (j0 + 1, 4), (j0 + 2, 6), (j0 + 3, 8), (j0 + 4, 15)]
            lat = []
            for (j, bid) in tiles:
                lat.append((lambda h, j=j: kt[(h % 2) * 64:(h % 2 + 1) * 64, h // 2, j * 128:(j + 1) * 128], ktb, bid,
                            lambda h, j=j: vw[:, j, h * 65:(h + 1) * 65], vwb))
            ot, otb = Ot.get()
            attend_block(qt, qtb, rl * 64, lat, ot, otb)
            OTt, OTtb = OT[oti % 2], OTb[oti % 2]
            transpose_block(ot, otb, OTt, OTtb, rl % 4)
            if rl % 4 == 3:
                project(OTt, OTtb, 256, 0, g * 512 + (rl // 4) * 256)
                oti += 1
    qt, qtb = QT[0], QTb[0]
    kb.dma('sp', qt[:, :, 0:256], qvd[:, :, NL:NT], reads=xbufs(T, 'qT', NL, NCX), writes=[qtb])
    OTt, OTtb = OT[oti % 2], OTb[oti % 2]
    for blk in range(4):
        ot, otb = Ot.get()
        attend_block(qt, qtb, blk * 64, [], ot, otb)
        transpose_block(ot, otb, OTt, OTtb, blk)
    project(OTt, OTtb, 256, 1, NL)


def ml_rope_tables():
    inv = 10000.0 ** (-np.arange(0, 32, 2, dtype=np.float64) / 32.0)
    p = np.arange(128)
    d = p % 64
    axis = d // 32
    i = d % 16
    half = (d % 32) // 16
    t = np.arange(NL)
    pos = np.where(axis[:, None] == 0, (t // 64)[None, :], (t % 64)[None, :]).astype(np.float64)
    ang = pos * inv[i][:, None]
    Cc = np.ones((128, NT), np.float64)
    Ss = np.zeros((128, NT), np.float64)
    Cc[:, :NL] = np.cos(ang)
    Ss[:, :NL] = np.sin(ang) * np.where(half[:, None] == 0, -1.0, 1.0)
    tabs = np.stack([Cc, Ss, Cc * 0.125, Ss * 0.125], axis=0).astype(np.float32)
    col = np.arange(1024)
    dd = col % 64
    perm = np.where((dd % 32) < 16, col + 16, col - 16)
    return np.ascontiguousarray(tabs), perm


def phase_ml_a(C, P, T, l, src):
    kb = C.kb
    C.new_phase()
    G = 256
    W = C.tile([8, 3072], BF16)
    Wb = bufs(8)
    load_weight(C, W, Wb, T['ml_w_in'][0], 8)
    Wsw = C.tile([8, 1024], BF16)
    Wswb = bufs(8)
    load_weight(C, Wsw, Wswb, T['ml_w_qk_sw'], 8)
    Wg = C.tile([8, 32], BF16)
    Wgb = bufs(8)
    load_weight(C, Wg, Wgb, T['ml_w_gate2'], 8)
    bg = C.tile([32], BF16)
    bgb = Buf()
    kb.dma('pool', bg[0:1, :], T['ml_b_gate2'][0:1, :], writes=[bgb])
    XL = [C.tile([8, G], F32) for _ in range(2)]
    XLb = bufs(2)
    H = C.tile([8, G], BF16)
    Hb = bufs(8)
    QK = [C.tile([8, G], BF16) for _ in range(2)]
    QKb = bufs(2)
    OT = [C.tile([8, G], BF16) for _ in range(2)]
    OTb = bufs(2)
    TB = [C.tile([4, G], F32) for _ in range(2)]
    TBb = bufs(2)
    Vt = [C.tile([8, 129], BF16) for _ in range(3)]
    Vtb = bufs(3)
    for i in range(3):
        kb.op('pool', lambda e, i=i: e.memset(Vt[i], 1.0), writes=[Vtb[i]])
    Kt = [C.tile([512], BF16) for _ in range(2)]
    Ktb = bufs(2)
    Gt = [C.tile([32], F32) for _ in range(2)]
    Gtb = bufs(2)
    GE = Small(C, 2, [32], F32)
    SQ = Small(C, 3, [G], BF16)
    RS = Small(C, 2, [G], F32)
    TMP = Small(C, 4, [G], F32)
    grp = groups(G, True, full=True)
    srcv = T[src].rearrange("(c p) t -> p c t", p=128)
    qkv = T['mqk'].rearrange("(c p) t -> p c t", p=128)
    ov = T['moT'].rearrange("(c p) t -> p c t", p=128)
    tabv = T['ml_rope'].rearrange("a p t -> p a t")
    psT_all = C.ps.bitcast(BF16)
    ident = P['ident']

    def load(i):
        col0, n, w = grp[i]
        kb.dma('sp', XL[i % 2][:, :, 0:n], srcv[:, :, col0:col0 + n], reads=xbufs(T, src, col0, n),
               writes=[XLb[i % 2]])
        kb.dma('sp', TB[i % 2][:, :, 0:n], tabv[:, :, col0:col0 + n], writes=[TBb[i % 2]])
    load(0)
    vi = 0
    for i, (col0, n, w) in enumerate(grp):
        if i + 1 < len(grp):
            load(i + 1)
        prenorm(C, P, XL[i % 2], XLb[i % 2], n, H, Hb, l, 0, 1, w, SQ, RS, TMP)
        qk, qkb = QK[i % 2], QKb[i % 2]
        tb, tbb = TB[i % 2], TBb[i % 2]
        for m in range(8):
            pt, pb = C.psum(2 * n)
            for k in range(8):
                mm(C, pt[:, 0:n], pb, W[:, k, m * 128:(m + 1) * 128], [Wb[k]], H[:, k, 0:n], [Hb[k]], k == 0, k == 7)
            for k in range(8):
                mm(C, pt[:, n:2 * n], pb, Wsw[:, k, m * 128:(m + 1) * 128], [Wswb[k]], H[:, k, 0:n], [Hb[k]],
                   k == 0, k == 7)
            ti = 0 if m < 4 else 2
            t1, t1b = TMP.get()
            t2, t2b = TMP.get()
            kb.op('dve', lambda e, t1=t1, pt=pt, ti=ti, tb=tb: e.tensor_tensor(out=t1[:, 0:n], in0=pt[:, 0:n],
                                                                             in1=tb[:, ti, 0:n], op=ALU.mult),
                  reads=pb + [tbb], writes=[t1b])
            kb.op('dve', lambda e, t2=t2, pt=pt, ti=ti, tb=tb: e.tensor_tensor(out=t2[:, 0:n], in0=pt[:, n:2 * n],
                                                                             in1=tb[:, ti + 1, 0:n], op=ALU.mult),
                  reads=pb + [tbb], writes=[t2b])
            kb.op('pool', lambda e, t1=t1, t2=t2, m=m, qk=qk: e.tensor_tensor(out=qk[:, m, 0:n], in0=t1[:, 0:n],
                                                                            in1=t2[:, 0:n], op=ALU.add),
                  reads=[t1b, t2b], writes=[qkb])
        kb.dma('sp', qkv[:, :, col0:col0 + n], qk[:, :, 0:n], reads=[qkb], writes=xbufs(T, 'mqk', col0, n))
        ot, otb = OT[i % 2], OTb[i % 2]
        for mp in range(4):
            pt, pb = C.psum(2 * n)
            for h in range(2):
                m = 2 * mp + h
                for k in range(8):
                    mm(C, pt[:, h * n:(h + 1) * n], pb, W[:, k, 2048 + m * 128:2048 + (m + 1) * 128], [Wb[k]],
                       H[:, k, 0:n], [Hb[k]], k == 0, k == 7)
            kb.op('act', lambda e, pt=pt, mp=mp, ot=ot: e.activation(
                out=ot[:, 2 * mp:2 * mp + 2, 0:n], in_=pt.rearrange("p (a t) -> p a t", a=2), func=AF.Sigmoid),
                reads=pb, writes=[otb])
        kb.dma('sp', ov[:, :, col0:col0 + n], ot[:, :, 0:n], reads=[otb], writes=xbufs(T, 'moT', col0, n))
        for s in range(n // 128):
            t0 = col0 + s * 128
            vt, vtb = Vt[vi % 3], Vtb[vi % 3]
            for nq in range(2):
                pt, pb = C.psum(512)
                for k in range(8):
                    mm(C, pt, pb, H[:, k, s * 128:(s + 1) * 128], [Hb[k]],
                       W[:, k, 1024 + nq * 512:1024 + (nq + 1) * 512], [Wb[k]], k == 0, k == 7)
                kb.op('act', lambda e, pt=pt, vt=vt, nq=nq: e.activation(
                    out=vt[:, nq * 4:(nq + 1) * 4, 0:128], in_=pt.rearrange("p (h d) -> p h d", h=4),
                    func=AF.Identity), reads=pb, writes=[vtb])
            kb.dma('sp', T['mvD'][t0:t0 + 128, :], vt.rearrange("p h d -> p (h d)"), reads=[vtb],
                   writes=xbufs(T, 'mvD', t0, 128))
            kt, ktb = Kt[vi % 2], Ktb[vi % 2]
            pt, pb = C.psum(256)
            bank = int(pb[0].name[2:])
            ptb16 = psT_all[:, bank * 1024:bank * 1024 + 512]
            for c in range(4):
                kb.op('pe', lambda e, c=c, ptb16=ptb16, qk=qk, s=s: e.transpose(
                    ptb16[:, c * 128:(c + 1) * 128], qk[:, 4 + c, s * 128:(s + 1) * 128], ident),
                    reads=[qkb, P['ident_b']], writes=pb)
            kb.op('act', lambda e, kt=kt, ptb16=ptb16: e.activation(out=kt, in_=ptb16, func=AF.Identity), reads=pb,
                  writes=[ktb])
            kb.dma('sp', T['mkD'][t0:t0 + 128, :], kt, reads=[ktb], writes=xbufs(T, 'mkD', t0, 128))
            gt, gtb = Gt[vi % 2], Gtb[vi % 2]
            pt, pb = C.psum(32)
            for k in range(8):
                mm(C, pt, pb, H[:, k, s * 128:(s + 1) * 128], [Hb[k]], Wg[:, k, :], [Wgb[k]], k == 0, False)
            mm(C, pt, pb, P['one_row'][0:1, 0:128], [P['one_row_b']], bg[0:1, :], [bgb], False, True)
            ge, geb = GE.get()
            p4 = pt.rearrange("p (a b h) -> p a b h", a=2, b=2)
            g4 = gt.rearrange("p (a b h) -> p a b h", a=2, b=2)
            e4 = ge.rearrange("p (a b h) -> p a b h", a=2, b=2)
            kb.op('act', lambda e, p4=p4, e4=e4: e.activation(out=e4[:, :, 1, :], in_=p4[:, :, 1, :], func=AF.Exp,
                                                             scale=-1.0), reads=pb, writes=[geb])
            kb.op('act', lambda e, e4=e4: e.activation(out=e4[:, :, 1, :], in_=e4[:, :, 1, :], func=AF.Ln, bias=1.0),
                  reads=[geb], writes=[geb], strict=True)
            kb.op('dve', lambda e, g4=g4, e4=e4: e.tensor_scalar(out=g4[:, :, 1, :], in0=e4[:, :, 1, :], scalar1=-1.0,
                                                                scalar2=None, op0=ALU.mult), reads=[geb], writes=[gtb])
            kb.op('dve', lambda e, g4=g4, p4=p4: e.tensor_copy(out=g4[:, :, 0, :], in_=p4[:, :, 0, :]), reads=pb,
                  writes=[gtb])
            kb.dma('sp', T['mgD'][t0:t0 + 128, :], gt, reads=[gtb], writes=xbufs(T, 'mgD', t0, 128))
            vi += 1


def phase_ml_b(C, P, T):
    kb = C.kb
    C.new_phase()
    NEG = -30000.0
    cA = [C.tile([128], F32) for _ in range(2)]
    cB = [C.tile([128], F32) for _ in range(2)]
    cM = [C.tile([128], F32) for _ in range(2)]
    cI = C.tile([128], F32)
    cO = C.tile([128], F32)
    onesb = C.tile([128], BF16)
    cb = Buf()
    for t_, val in ((cA[0], 1.0), (cA[1], 1.0), (cB[0], 1.0), (cB[1], 1.0), (cM[0], 0.0), (cM[1], 0.0), (cI, 0.0),
                    (cO, 1.0)):
        kb.op('pool', lambda e, t_=t_, val=val: e.memset(t_, val), writes=[cb])
    kb.op('pool', lambda e: e.memset(onesb, 1.0), writes=[cb])

    def asel(t_, cmp, fill, base=0, cm=1, pat=-1):
        kb.op('pool', lambda e: e.affine_select(out=t_, in_=t_, pattern=[[pat, 128]], compare_op=cmp, fill=fill,
                                                base=base, channel_multiplier=cm), reads=[cb], writes=[cb],
              strict=True)
    asel(cA[0], ALU.is_gt, 0.0, base=0, cm=1, pat=-1)
    asel(cA[1], ALU.is_gt, 0.0, base=0, cm=-1, pat=1)
    asel(cB[0], ALU.is_gt, 0.0, base=1, cm=-1, pat=1)
    asel(cB[1], ALU.is_gt, 0.0, base=1, cm=1, pat=-1)
    asel(cM[0], ALU.is_gt, NEG, base=1, cm=-1, pat=1)
    asel(cM[1], ALU.is_gt, NEG, base=1, cm=1, pat=-1)
    asel(cI, ALU.not_equal, 1.0)
    St = C.tile([8, 129], F32)
    Cbf = C.tile([8, 128], BF16)
    Nbc = C.tile([8, 128], BF16)
    Stb, Cbfb, Nbcb = Buf(), Buf(), Buf()
    NB = 2
    Q = [C.tile([8, 128], BF16) for _ in range(NB)]
    K_ = [C.tile([8, 128], BF16) for _ in range(NB)]
    KT = [C.tile([8, 64], BF16) for _ in range(NB)]
    V = [C.tile([8, 129], BF16) for _ in range(NB)]
    GA = [C.tile([32], F32) for _ in range(NB)]
    Qb, Kb_, KTb, Vb, GAb = bufs(NB), bufs(NB), bufs(NB), bufs(NB), bufs(NB)
    R = C.tile([8, 128], F32)
    R2 = C.tile([8, 128], F32)
    Rb, R2b = Buf(), Buf()
    E = C.tile([8, 128], F32)
    EB = C.tile([8, 128], F32)
    Eb, EBb = Buf(), Buf()
    AT = C.tile([8, 128], BF16)
    ATb = Buf()
    KW = C.tile([8, 64], BF16)
    KWb = Buf()
    SM = Small(C, 2, [32], F32)
    NUM = C.tile([8, 128], F32)
    DEN = C.tile([8, 128], F32)
    NUMb, DENb = Buf(), Buf()
    HO = [C.tile([8, 128], F32) for _ in range(2)]
    HOb = bufs(2)
    mq = T['mqk']
    hidx = 0
    it = 0
    ntl = 64 if MAXG is None else MAXG
    for dr in range(2):
        for t_ in (St, Cbf, Nbc):
            kb.op('pool', lambda e, t_=t_: e.memset(t_, 0.0), reads=[], writes=[Stb, Cbfb, Nbcb])
        lat = list(range(ntl)) if dr == 0 else list(range(ntl - 1, -1, -1))
        seq = ([64, 65] if dr == 0 else [65, 64]) + lat
        hD = T['mhf'] if dr == 0 else T['mhb']
        hkey = 'mhf' if dr == 0 else 'mhb'
        for ti in seq:
            t0 = ti * 128
            b = it % NB
            it += 1
            q, k_, kt, v, ga = Q[b], K_[b], KT[b], V[b], GA[b]
            kb.dma('sp', q[0:64], mq[0:512, t0:t0 + 128].rearrange("(h d) t -> d h t", d=64),
                   reads=xbufs(T, 'mqk', t0, 128), writes=[Qb[b]])
            kb.dma('sp', k_[0:64], mq[512:1024, t0:t0 + 128].rearrange("(h d) t -> d h t", d=64),
                   reads=xbufs(T, 'mqk', t0, 128), writes=[Kb_[b]])
            kb.dma('sp', kt, T['mkD'][t0:t0 + 128, :].rearrange("p (h d) -> p h d", h=8),
                   reads=xbufs(T, 'mkD', t0, 128), writes=[KTb[b]])
            kb.dma('sp', v, T['mvD'][t0:t0 + 128, :].rearrange("p (h d) -> p h d", h=8),
                   reads=xbufs(T, 'mvD', t0, 128), writes=[Vb[b]])
            kb.dma('sp', ga, T['mgD'][t0:t0 + 128, :], reads=xbufs(T, 'mgD', t0, 128), writes=[GAb[b]])
            ig = ga[:, dr * 16:dr * 16 + 8]
            lf = ga[:, dr * 16 + 8:dr * 16 + 16]
            kb.op('dve', lambda e, lf=lf, dr=dr: e.tensor_tensor(out=R, in0=cB[dr].unsqueeze(1).broadcast_to([128, 8, 128]),
                                                          in1=lf.unsqueeze(2).broadcast_to([128, 8, 128]),
                                                          op=ALU.mult), reads=[cb, GAb[b]], writes=[Rb])
            kb.op('pool', lambda e, ig=ig, dr=dr: e.tensor_tensor(out=R2, in0=cM[dr].unsqueeze(1).broadcast_to([128, 8, 128]),
                                                           in1=ig.unsqueeze(2).broadcast_to([128, 8, 128]),
                                                           op=ALU.add), reads=[cb, GAb[b]], writes=[R2b])
            psm, psmb = C.psum(16)
            mm(C, psm[:, 0:8], psmb, cA[dr], [cb], lf, [GAb[b]], True, True)
            mm(C, psm[:, 8:16], psmb, cO, [cb], lf, [GAb[b]], True, True)
            sm, smb = SM.get()
            kb.op('dve', lambda e, sm=sm, psm=psm, ig=ig: e.tensor_tensor(out=sm[:, 0:8], in0=psm[:, 0:8], in1=ig,
                                                                         op=ALU.add), reads=psmb + [GAb[b]],
                  writes=[smb])
            kb.op('act', lambda e, sm=sm: e.activation(out=sm[:, 0:8], in_=sm[:, 0:8], func=AF.Exp), reads=[smb],
                  writes=[smb])
            kb.op('act', lambda e, sm=sm, psm=psm: e.activation(out=sm[:, 8:16], in_=psm[:, 8:16], func=AF.Exp),
                  reads=psmb, writes=[smb], strict=True)
            pE, pB, pS = [], [], []
            for hf in range(2):
                pe_, peb = C.psum(512)
                Rv = R[:, hf * 4:(hf + 1) * 4, :].rearrange("p h t -> p (h t)")
                R2v = R2[:, hf * 4:(hf + 1) * 4, :].rearrange("p h t -> p (h t)")
                mm(C, pe_, peb, cA[dr], [cb], Rv, [Rb], True, False)
                mm(C, pe_, peb, cI, [cb], R2v, [R2b], False, True)
                kb.op('act', lambda e, pe_=pe_, hf=hf: e.activation(
                    out=E[:, hf * 4:(hf + 1) * 4, :].rearrange("p h t -> p (h t)"), in_=pe_, func=AF.Exp),
                    reads=peb, writes=[Eb])
                pb_, pbb = C.psum(512)
                mm(C, pb_, pbb, cO, [cb], Rv, [Rb], True, True)
                kb.op('act', lambda e, pb_=pb_, hf=hf: e.activation(
                    out=EB[:, hf * 4:(hf + 1) * 4, :].rearrange("p h t -> p (h t)"), in_=pb_, func=AF.Exp),
                    reads=pbb, writes=[EBb])
            for hf in range(2):
                ps_, psb_ = C.psum(512)
                for hh in range(4):
                    h = hf * 4 + hh
                    mm(C, ps_[:, hh * 128:(hh + 1) * 128], psb_, k_[0:64, h, :], [Kb_[b]], q[0:64, h, :], [Qb[b]],
                       True, True)
                kb.op('dve', lambda e, ps_=ps_, hf=hf: e.tensor_tensor(
                    out=AT[:, hf * 4:(hf + 1) * 4, :].rearrange("p h t -> p (h t)"), in0=ps_,
                    in1=E[:, hf * 4:(hf + 1) * 4, :].rearrange("p h t -> p (h t)"), op=ALU.mult),
                    reads=psb_ + [Eb], writes=[ATb])
            ho, hob = HO[hidx % 2], HOb[hidx % 2]
            hidx += 1
            for hf in range(2):
                hs = slice(hf * 4, (hf + 1) * 4)
                pn, pnb = C.psum(512)
                for hh in range(4):
                    h = hf * 4 + hh
                    mm(C, pn[:, hh * 128:(hh + 1) * 128], pnb, v[:, h, 0:128], [Vb[b]], AT[:, h, :], [ATb], True, True)
                pd_, pdb = C.psum(512)
                mm(C, pd_, pdb, onesb, [cb], AT[:, hs, :].rearrange("p h t -> p (h t)"), [ATb], True, True)
                pi, pib = C.psum(512)
                for hh in range(4):
                    h = hf * 4 + hh
                    mm(C, pi[:, hh * 128:(hh + 1) * 128], pib, Cbf[0:64, h, :], [Cbfb], q[0:64, h, :], [Qb[b]],
                       True, True)
                pq, pqb = C.psum(512)
                for hh in range(4):
                    h = hf * 4 + hh
                    mm(C, pq[:, hh * 128:(hh + 1) * 128], pqb, Nbc[0:64, h, :], [Nbcb], q[0:64, h, :], [Qb[b]],
                       True, True)
                ebv = EB[:, hs, :].rearrange("p h t -> p (h t)")
                numv = NUM[:, hs, :].rearrange("p h t -> p (h t)")
                denv = DEN[:, hs, :].rearrange("p h t -> p (h t)")
                hov = ho[:, hs, :].rearrange("p h t -> p (h t)")
                kb.op('dve', lambda e, pi=pi, ebv=ebv, numv=numv: e.tensor_tensor(out=numv, in0=pi, in1=ebv,
                                                                                 op=ALU.mult),
                      reads=pib + [EBb], writes=[NUMb])
                kb.op('dve', lambda e, pn=pn, numv=numv: e.tensor_tensor(out=numv, in0=pn, in1=numv, op=ALU.add),
                      reads=pnb + [NUMb], writes=[NUMb])
                kb.op('dve', lambda e, pq=pq, ebv=ebv, denv=denv: e.tensor_tensor(out=denv, in0=pq, in1=ebv,
                                                                                 op=ALU.mult),
                      reads=pqb + [EBb], writes=[DENb])
                kb.op('dve', lambda e, pd_=pd_, denv=denv: e.tensor_tensor(out=denv, in0=pd_, in1=denv, op=ALU.add),
                      reads=pdb + [DENb], writes=[DENb])
                kb.op('act', lambda e, denv=denv: e.activation(out=denv, in_=denv, func=AF.Abs), reads=[DENb],
                      writes=[DENb])
                kb.op('dve', lambda e, denv=denv: e.tensor_scalar(out=denv, in0=denv, scalar1=1.0, scalar2=None,
                                                                  op0=ALU.max), reads=[DENb], writes=[DENb])
                kb.op('dve', lambda e, denv=denv: e.reciprocal(out=denv, in_=denv), reads=[DENb], writes=[DENb])
                kb.op('pool', lambda e, denv=denv, numv=numv, hov=hov: e.tensor_tensor(out=hov, in0=numv, in1=denv,
                                                                                      op=ALU.mult),
                      reads=[DENb, NUMb], writes=[hob])
            kb.dma('sp', hD[:, t0:t0 + 128].rearrange("(h p) t -> p h t", p=128), ho, reads=[hob],
                   writes=xbufs(T, hkey, t0, 128))
            sm_w = sm[:, 0:8]
            kb.op('dve', lambda e, sm_w=sm_w, kt=kt: e.tensor_tensor(
                out=KW, in0=kt, in1=sm_w.unsqueeze(2).broadcast_to([128, 8, 64]), op=ALU.mult),
                reads=[KTb[b], smb], writes=[KWb])
            for (h0, nh) in ((0, 3), (3, 3), (6, 2)):
                pu, pub = C.psum(nh * 129)
                for hh in range(nh):
                    h = h0 + hh
                    mm(C, pu[0:64, hh * 129:(hh + 1) * 129], pub, KW[:, h, :], [KWb], v[:, h, :], [Vb[b]], True, True)
                stv = St[0:64, h0:h0 + nh, :]
                kb.op('dve', lambda e, stv=stv, sm=sm, h0=h0, nh=nh: e.tensor_tensor(
                    out=stv, in0=stv, in1=sm[0:64, 8 + h0:8 + h0 + nh].unsqueeze(2).broadcast_to([64, nh, 129]),
                    op=ALU.mult), reads=[Stb, smb], writes=[Stb], strict=True)
                kb.op('dve', lambda e, stv=stv, pu=pu, nh=nh: e.tensor_tensor(
                    out=stv, in0=pu[0:64, :].rearrange("p (h d) -> p h d", h=nh), in1=stv, op=ALU.add),
                    reads=pub + [Stb], writes=[Stb], strict=True)
            kb.op('act', lambda e: e.activation(out=Cbf[0:64], in_=St[0:64, :, 0:128], func=AF.Identity),
                  reads=[Stb], writes=[Cbfb])
            kb.op('pool', lambda e: e.tensor_copy(out=Nbc[0:64], in_=St[0:64, :, 128:129].broadcast_to([64, 8, 128])),
                  reads=[Stb], writes=[Nbcb])


def phase_ml_c(C, P, T, l, src, dst):
    kb = C.kb
    C.new_phase()
    G = 256
    Wo = C.tile([8, 1024], BF16)
    Wob = bufs(8)
    load_weight(C, Wo, Wob, T['ml_w_out'][0], 8)
    ng = C.tile([8], F32)
    ngb = Buf()
    kb.dma('sp', ng, T['ml_norm_gT'][:, :], writes=[ngb])
    ones128 = C.tile([128], BF16)
    o1b = Buf()
    kb.op('pool', lambda e: e.memset(ones128, 1.0 / 128), writes=[o1b])
    HF = [C.tile([8, G], F32) for _ in range(2)]
    HB = [C.tile([8, G], F32) for _ in range(2)]
    OG = [C.tile([8, G], BF16) for _ in range(2)]
    HFb, HBb, OGb = bufs(2), bufs(2), bufs(2)
    Hm = C.tile([8, G], BF16)
    Hmb = bufs(8)
    Yf = C.tile([8, G], F32)
    Yfb = bufs(8)
    SQ = Small(C, 3, [2 * G], BF16)
    RS = Small(C, 3, [G], F32)
    TMP = Small(C, 4, [G], F32)
    XR = Small(C, 4, [G], F32)
    grp = groups(G, True)
    hfv = T['mhf'].rearrange("(c p) t -> p c t", p=128)
    hbv = T['mhb'].rearrange("(c p) t -> p c t", p=128)
    ov = T['moT'].rearrange("(c p) t -> p c t", p=128)

    def load(i):
        col0, n, w = grp[i]
        kb.dma('sp', HF[i % 2][:, :, 0:n], hfv[:, :, col0:col0 + n], reads=xbufs(T, 'mhf', col0, n),
               writes=[HFb[i % 2]])
        kb.dma('sp', HB[i % 2][:, :, 0:n], hbv[:, :, col0:col0 + n], reads=xbufs(T, 'mhb', col0, n),
               writes=[HBb[i % 2]])
        kb.dma('sp', OG[i % 2][:, :, 0:n], ov[:, :, col0:col0 + n], reads=xbufs(T, 'moT', col0, n),
               writes=[OGb[i % 2]])
    load(0)
    for i, (col0, n, w) in enumerate(grp):
        if i + 1 < len(grp):
            load(i + 1)
        hf, hb, og = HF[i % 2], HB[i % 2], OG[i % 2]
        kb.op('pool', lambda e, hf=hf, hb=hb: e.tensor_tensor(out=hf[:, :, 0:n], in0=hf[:, :, 0:n], in1=hb[:, :, 0:n],
                                                             op=ALU.add), reads=[HFb[i % 2], HBb[i % 2]],
              writes=[HFb[i % 2]])
        for c in range(8):
            s_, sb_ = SQ.get()
            kb.op('act', lambda e, s_=s_, c=c, hf=hf: e.activation(out=s_[:, 0:n], in_=hf[:, c, 0:n], func=AF.Square),
                  reads=[HFb[i % 2]], writes=[sb_])
            pst, pb = C.psum(n)
            mm(C, pst, pb, ones128, [o1b], s_[:, 0:n], [sb_], True, True)
            rs, rb = rstd_from_psum(C, P, pst, pb, RS)
            t, tb = TMP.get()
            kb.op('dve', lambda e, t=t, c=c, hf=hf, rs=rs: e.scalar_tensor_tensor(
                out=t[:, 0:n], in0=hf[:, c, 0:n], scalar=ng[:, c:c + 1], in1=rs, op0=ALU.mult, op1=ALU.mult),
                reads=[HFb[i % 2], rb, ngb], writes=[tb])
            kb.op('pool', lambda e, t=t, c=c, og=og: e.tensor_tensor(out=Hm[:, c, 0:n], in0=t[:, 0:n],
                                                                    in1=og[:, c, 0:n], op=ALU.mult),
                  reads=[tb, OGb[i % 2]], writes=[Hmb[c]])
        pr = PostRes(C, P, n, Yf, Yfb, SQ, RS, TMP, XR)
        for mp in range(4):
            pt, pb = C.psum(2 * n)
            for h in range(2):
                m = 2 * mp + h
                for k in range(8):
                    mm(C, pt[:, h * n:(h + 1) * n], pb, Wo[:, k, m * 128:(m + 1) * 128], [Wob[k]], Hm[:, k, 0:n],
                       [Hmb[k]], k == 0, k == 7)
            pr.pair(2 * mp, pt, pb)
        pr.finish(l, 2, w, T[src], T[dst], col0, xbufs(T, src, col0, n), xbufs(T, dst, col0, n))


def build_program(plan, dbg=False):
    nc = bass.Bass("TRN2", target_bir_lowering=False)
    es = ExitStack()
    T = {}

    def dram(name, shape, dt, kind):
        T[name] = nc.dram_tensor(name, shape, dt, kind=kind).ap()
        return T[name]

    dram('xin', [D, NT], F32, "ExternalInput")
    dram('cvec', [128, 8, 2], F32, "ExternalInput")
    dram('ada_w', [4, D, 6 * D], F32, "ExternalInput")
    dram('ada_bT', [128, 4, 48], F32, "ExternalInput")
    dram('norm_gT', [128, 4, 4, 8], F32, "ExternalInput")
    dram('ffn_w1', [4, D, 4 * D], F32, "ExternalInput")
    dram('ffn_w2', [4, 4 * D, D], F32, "ExternalInput")
    dram('gm_w_in', [2, D, 6 * D], F32, "ExternalInput")
    dram('gm_b_in', [2, 6 * D], F32, "ExternalInput")
    dram('gm_ln_gT', [2, 128, 24], F32, "ExternalInput")
    dram('gm_wsT', [2, 128, 8 * 128], F32, "ExternalInput")
    dram('gm_bs_bc', [2, 128, 8 * 128], F32, "ExternalInput")
    dram('gm_w_out', [2, 3 * D, D], F32, "ExternalInput")
    dram('na_w_qkv', [1, D, 3 * D], F32, "ExternalInput")
    dram('na_b_qkv', [1, 3 * D], F32, "ExternalInput")
    dram('na_bqkvT', [128, 24], F32, "ExternalInput")
    dram('na_bt', [128, 16 * 16 * 64], F32, "ExternalInput")
    dram('na_w_o', [1, D, D], F32, "ExternalInput")
    dram('na_b_oT', [128, 8], F32, "ExternalInput")
    dram('qT', [D, NT], BF16, "Internal")
    dram('kT', [D, NT], BF16, "Internal")
    dram('vD', [NT, 1040], BF16, "Internal")
    dram('ml_w_in', [1, D, 3 * D], F32, "ExternalInput")
    dram('ml_w_qk_sw', [D, D], F32, "ExternalInput")
    dram('ml_w_gate2', [D, 32], F32, "ExternalInput")
    dram('ml_b_gate2', [1, 32], F32, "ExternalInput")
    dram('ml_rope', [4, 128, NT], F32, "ExternalInput")
    dram('ml_norm_gT', [128, 8], F32, "ExternalInput")
    dram('ml_w_out', [1, D, D], F32, "ExternalInput")
    dram('mqk', [D, NT], BF16, "Internal")
    dram('moT', [D, NT], BF16, "Internal")
    dram('mvD', [NT, 8 * 129], BF16, "Internal")
    dram('mkD', [NT, 512], BF16, "Internal")
    dram('mgD', [NT, 32], F32, "Internal")
    dram('mhf', [D, NT], F32, "ExternalOutput" if dbg else "Internal")
    dram('mhb', [D, NT], F32, "ExternalOutput" if dbg else "Internal")
    dram('yout', [D, NL], F32, "ExternalOutput")
    dram('xs', [D, NT], F32, "ExternalOutput" if dbg else "Internal")
    dram('pd', [3 * D, NT], BF16, "ExternalOutput" if dbg else "Internal")
    if dbg:
        dram('dbg_vh', [128, 3072], BF16, "ExternalOutput")
        dram('dbg_mv', [128, 8], F32, "ExternalOutput")
        dram('dbg_zg', [128, 3072], F32, "ExternalOutput")
    for k in ['xin', 'xs', 'pd', 'yout', 'qT', 'kT', 'vD', 'mqk', 'moT', 'mvD', 'mkD', 'mgD', 'mhf', 'mhb']:
        T[k + '_b'] = bufs(NT // 128, k)
    C = Ctx(nc, es)
    P = phase_consts(C, T)
    for ph in plan:
        kind = ph[0]
        if kind == 'ffn':
            _, l, src, dst, with_ctx, final = ph
            phase_ffn(C, P, T, l, src, dst, with_ctx, final)
        elif kind == 'gmlp':
            _, l, j, src, dst, with_ctx = ph
            phase_gmlp_a(C, P, T, l, j, src, with_ctx)
            phase_gmlp_b(C, P, T, l, j, src, dst, with_ctx)
        elif kind == 'na':
            _, l, src, dst = ph
            if NA_PARTS & 1:
                phase_na_a(C, P, T, l, src)
            if NA_PARTS & 2:
                phase_na_b(C, P, T, l, src, dst)
        elif kind == 'ml':
            _, l, src, dst = ph
            phase_ml_a(C, P, T, l, src)
            phase_ml_b(C, P, T)
            phase_ml_c(C, P, T, l, src, dst)
        else:
            raise ValueError(kind)
    C.kb.barrier()
    C.kb.emit(es)
    es.close()
    return nc, C


def host_shared(inp):
    f = np.float32
    m = {}
    m['ada_w'] = inp['ada_w']
    m['ada_bT'] = np.ascontiguousarray(inp['ada_b'].reshape(4, 48, 128).transpose(2, 0, 1), dtype=f)
    m['norm_gT'] = np.ascontiguousarray(inp['norm_g'].reshape(4, 4, 8, 128).transpose(3, 0, 1, 2), dtype=f)
    m['ffn_w1'] = inp['ffn_w1']
    m['ffn_w2'] = inp['ffn_w2']
    m['gm_w_in'] = inp['gm_w_in']
    m['gm_b_in'] = inp['gm_b_in']
    m['gm_w_out'] = inp['gm_w_out']
    m['gm_ln_gT'] = np.ascontiguousarray(inp['gm_ln_g'].reshape(2, 24, 128).transpose(0, 2, 1), dtype=f)
    m['gm_wsT'] = np.ascontiguousarray(inp['gm_ws'].transpose(0, 3, 1, 2).reshape(2, 128, 1024), dtype=f)
    m['na_w_qkv'] = inp['na_w_qkv']
    m['na_b_qkv'] = inp['na_b_qkv']
    m['na_bqkvT'] = np.ascontiguousarray(inp['na_b_qkv'][0].reshape(24, 128).T, dtype=f)
    m['na_bt'] = na_bias_table(inp['na_rpb'][0])
    m['na_w_o'] = inp['na_w_o']
    m['na_b_oT'] = np.ascontiguousarray(inp['na_b_o'][0].reshape(8, 128).T, dtype=f)
    tabs, perm = ml_rope_tables()
    m['ml_w_in'] = inp['ml_w_in']
    m['ml_w_qk_sw'] = np.ascontiguousarray(inp['ml_w_in'][0][:, :1024][:, perm], dtype=f)
    wg = inp['ml_w_gate'][0]
    m['ml_w_gate2'] = np.ascontiguousarray(np.concatenate([wg[0], wg[1]], axis=1), dtype=f)
    m['ml_b_gate2'] = np.ascontiguousarray(inp['ml_b_gate'][0].reshape(1, 32), dtype=f)
    m['ml_rope'] = tabs
    m['ml_norm_gT'] = np.ascontiguousarray(inp['ml_norm_g'][0].reshape(8, 128).T, dtype=f)
    m['ml_w_out'] = inp['ml_w_out']
    m['gm_bs_bc'] = np.ascontiguousarray(np.broadcast_to(inp['gm_bs'].reshape(2, 1, 1024), (2, 128, 1024)), dtype=f)
    return m


def host_inputs(inp, b, shared=None):
    f = np.float32
    m = dict(shared if shared is not None else host_shared(inp))
    m['xin'] = np.ascontiguousarray(np.concatenate([inp['x'][b].T, inp['ctx'][b].T], axis=1), dtype=f)
    cv = np.stack([inp['c'][b], inp['c_ctx']], axis=-1)
    m['cvec'] = np.ascontiguousarray(cv.reshape(8, 128, 2).transpose(1, 0, 2), dtype=f)
    return m


FULL_PLAN = [
    ('gmlp', 0, 0, 'xin', 'xs', True), ('ffn', 0, 'xs', 'xs', True, False),
    ('na', 1, 'xs', 'xs'), ('ffn', 1, 'xs', 'xs', True, False),
    ('ml', 2, 'xs', 'xs'), ('ffn', 2, 'xs', 'xs', True, False),
    ('gmlp', 3, 1, 'xs', 'xs', False), ('ffn', 3, 'xs', 'xs', False, True),
]


def kernel(**inputs):
    inp = {k: np.asarray(v) for k, v in inputs.items()}
    nc, C = build_program(FULL_PLAN)
    shared = host_shared(inp)
    in_maps = [host_inputs(inp, b, shared) for b in range(8)]
    res = run_bass_kernel_spmd(nc, in_maps, core_ids=list(range(8)))
    out = np.stack([np.ascontiguousarray(np.asarray(res.results[b]['yout']).T) for b in range(8)], axis=0)
    return out.astype(np.float32)
```

```python
import math
import numpy as np
import concourse.bass as bass
import concourse.mybir as mybir
from concourse.bass_utils import run_bass_kernel_spmd
from contextlib import ExitStack

F32 = mybir.dt.float32
BF16 = mybir.dt.bfloat16
U8 = mybir.dt.uint8
AF = mybir.ActivationFunctionType
ALU = mybir.AluOpType

ENGS = ['pe', 'act', 'dve', 'pool', 'sp']
NSLOT = 12
SAME_ENGINE_SYNC = False

D = 1024
KC = 8
NL = 8192
NCX = 256
NT = NL + NCX
EPS = 1e-6
ARENA_BYTES = 204 * 1024


class Buf:
    __slots__ = ('name', 'lw', 'rd')

    def __init__(self, name=''):
        self.name = name
        self.lw = None
        self.rd = {}


def bufs(n, name=''):
    return [Buf(name + str(i)) for i in range(n)]


class KB:
    def __init__(self, nc):
        self.nc = nc
        self.streams = {e: [] for e in ENGS}
        self.seen = {e: {} for e in ENGS}
        self.dseen = {e: set() for e in ENGS}
        self.slot_rr = {e: 0 for e in ENGS}
        self.slot_cnt = {e: [0] * NSLOT for e in ENGS}
        self.slot_last = {e: [None] * NSLOT for e in ENGS}
        self.n_ops = 0

    def _collect(self, eng, reads, writes, strict=False):
        deps_e = {}
        deps_d = set()

        def addtok(t):
            if t is None:
                return
            if t[0] == 'e':
                if t[2] > deps_e.get(t[1], -1):
                    deps_e[t[1]] = t[2]
            else:
                deps_d.add(t)

        for b in reads:
            addtok(b.lw)
        for b in writes:
            addtok(b.lw)
            for k, v in b.rd.items():
                if isinstance(k, tuple):
                    deps_d.add(k)
                else:
                    addtok(('e', k, v))
        waits = []
        for E, idx in deps_e.items():
            if E == eng and (eng == 'pe' or not (SAME_ENGINE_SYNC or strict)):
                continue
            if self.seen[eng].get(E, -1) >= idx:
                continue
            self.seen[eng][E] = idx
            self.streams[E][idx]['inc'] = True
            waits.append(('e', E, idx))
        for t in deps_d:
            if t in self.dseen[eng]:
                continue
            self.dseen[eng].add(t)
            waits.append(t)
        return waits

    def _commit(self, tok, reads, writes):
        for b in writes:
            b.lw = tok
            b.rd = {}
        for b in reads:
            if b in writes:
                continue
            if tok[0] == 'e':
                if b.rd.get(tok[1], -1) < tok[2]:
                    b.rd[tok[1]] = tok[2]
            else:
                b.rd[tok] = 1

    def op(self, eng, fn, reads=(), writes=(), strict=False):
        waits = self._collect(eng, reads, writes, strict)
        idx = len(self.streams[eng])
        self.streams[eng].append(dict(waits=waits, fn=fn, inc=False, dma=None))
        self._commit(('e', eng, idx), reads, writes)
        self.n_ops += 1

    def dma(self, q, out, in_, reads=(), writes=()):
        waits = self._collect(q, reads, writes)
        slot = self.slot_rr[q]
        self.slot_rr[q] = (slot + 1) % NSLOT
        prev = self.slot_last[q][slot]
        if prev is not None and prev not in self.dseen[q]:
            self.dseen[q].add(prev)
            waits.append(prev)
        self.slot_cnt[q][slot] += 1
        tok = ('d', q, slot, 16 * self.slot_cnt[q][slot])
        self.slot_last[q][slot] = tok
        self.streams[q].append(dict(waits=waits, fn=lambda e, o=out, i=in_: e.dma_start(out=o, in_=i),
                                    inc=False, dma=(q, slot)))
        self._commit(tok, reads, writes)
        self.n_ops += 1

    def barrier(self):
        lasts = {}
        for E in ENGS:
            for idx in range(len(self.streams[E]) - 1, -1, -1):
                if self.streams[E][idx]['fn'] is not None and self.streams[E][idx]['dma'] is None:
                    lasts[E] = idx
                    break
        dtoks = [t for q in ENGS for t in self.slot_last[q] if t is not None]
        for W in ENGS:
            waits = []
            for E, idx in lasts.items():
                if E == W:
                    continue
                if self.seen[W].get(E, -1) >= idx:
                    continue
                self.seen[W][E] = idx
                self.streams[E][idx]['inc'] = True
                waits.append(('e', E, idx))
            for t in dtoks:
                if t in self.dseen[W]:
                    continue
                self.dseen[W].add(t)
                waits.append(t)
            if waits:
                self.streams[W].append(dict(waits=waits, fn=None, inc=False, dma=None))

    def _pref(self):
        pref = {}
        for e in ENGS:
            c = 0
            arr = []
            for ent in self.streams[e]:
                if ent['inc']:
                    assert ent['dma'] is None and ent['fn'] is not None
                    c += 1
                arr.append(c)
            pref[e] = arr
        return pref

    def check(self):
        pref = self._pref()
        esem = {e: 0 for e in ENGS}
        dsem = {(q, i): 0 for q in ENGS for i in range(NSLOT)}
        pc = {e: 0 for e in ENGS}
        progress = True
        while progress:
            progress = False
            for e in ENGS:
                st = self.streams[e]
                while pc[e] < len(st):
                    ent = st[pc[e]]
                    ok = True
                    for w in ent['waits']:
                        if w[0] == 'e':
                            if esem[w[1]] < pref[w[1]][w[2]]:
                                ok = False
                        elif dsem[(w[1], w[2])] < w[3]:
                            ok = False
                    if not ok:
                        break
                    if ent['dma'] is not None:
                        dsem[ent['dma']] += 16
                    elif ent['inc']:
                        esem[e] += 1
                    pc[e] += 1
                    progress = True
        for e in ENGS:
            if pc[e] < len(self.streams[e]):
                raise RuntimeError(f"DEADLOCK: {e} stuck at {pc[e]}/{len(self.streams[e])}")

    def emit(self, es):
        self.check()
        nc = self.nc
        esem = {e: es.enter_context(nc.semaphore("s_" + e)) for e in ENGS}
        dsem = {q: [es.enter_context(nc.semaphore(f"d_{q}{i}")) for i in range(NSLOT)]
                for q in ENGS if any(self.slot_cnt[q])}
        pref = self._pref()
        block = es.enter_context(nc.Block())
        handles = {'pe': block.tensor, 'act': block.scalar, 'dve': block.vector, 'pool': block.gpsimd,
                   'sp': block.sync}
        for e in ENGS:
            stream = self.streams[e]
            if not stream:
                continue

            def body(h, stream=stream, e=e):
                for ent in stream:
                    for w in ent['waits']:
                        if w[0] == 'e':
                            h.wait_ge(esem[w[1]], pref[w[1]][w[2]])
                        else:
                            h.wait_ge(dsem[w[1]][w[2]], w[3])
                    if ent['fn'] is None:
                        continue
                    ins = ent['fn'](h)
                    if ent['dma'] is not None:
                        q, slot = ent['dma']
                        ins.then_inc(dsem[q][slot], 16)
                    elif ent['inc']:
                        ins.then_inc(esem[e], 1)
            handles[e](body)


class Ctx:
    def __init__(self, nc, es):
        self.nc = nc
        self.kb = KB(nc)
        self.arena = es.enter_context(nc.sbuf_tensor("arena", [128, ARENA_BYTES], U8))
        self.ps = es.enter_context(nc.psum_tensor("ps", [128, 4096], F32))
        self.off = 0
        self.persist_end = 0
        self.psb = bufs(8, 'ps')
        self.ps_rr = 0
        self.pool_rr = {}
        self.default_banks = list(range(8))
        self.tmp_rr = 0
        self.dram = {}

    def tile(self, free_shape, dt):
        n = int(np.prod(free_shape))
        esz = 4 if dt == F32 else 2
        nbytes = (n * esz + 31) // 32 * 32
        assert self.off + nbytes <= ARENA_BYTES, (self.off, nbytes)
        v = self.arena[:, self.off:self.off + n * esz].bitcast(dt)
        self.off += nbytes
        if len(free_shape) == 2:
            v = v.rearrange("p (a b) -> p a b", a=free_shape[0])
        elif len(free_shape) == 3:
            v = v.rearrange("p (a b c) -> p a b c", a=free_shape[0], b=free_shape[1])
        elif len(free_shape) == 4:
            v = v.rearrange("p (a b c d) -> p a b c d", a=free_shape[0], b=free_shape[1], c=free_shape[2])
        return v

    def end_persist(self):
        self.persist_end = self.off

    def new_phase(self):
        self.kb.barrier()
        self.off = self.persist_end
        self.psb = bufs(8, 'ps')
        self.default_banks = list(range(8))

    def psum(self, ncols, banks=None):
        assert ncols <= 512
        if banks is None:
            banks = self.default_banks
        key = tuple(banks)
        j = self.pool_rr.get(key, 0)
        self.pool_rr[key] = (j + 1) % len(banks)
        i = banks[j]
        return self.ps[:, i * 512:i * 512 + ncols], [self.psb[i]]


def mm(C, out, ob, lhsT, lb, rhs, rb, start, stop):
    C.kb.op('pe', lambda e: e.matmul(out, lhsT, rhs, start=start, stop=stop), reads=lb + rb, writes=ob)


def load_weight(C, dst, dbufs, src, kc_n, queue='pool'):
    sv = src.rearrange("(k p) n -> p k n", p=128)
    for k in range(kc_n):
        C.kb.dma(queue, dst[:, k, :], sv[:, k, :], writes=[dbufs[k]])


def phase_consts(C, T):
    kb = C.kb
    P = {}
    P['ones_bf'] = C.tile([128], BF16)
    P['ones_bf_b'] = Buf()
    P['one_row'] = C.tile([512], BF16)
    P['one_row_b'] = Buf()
    P['eps'] = C.tile([1], F32)
    P['eps_b'] = Buf()
    P['ident'] = C.tile([128], BF16)
    P['ident_b'] = Buf()
    kb.op('pool', lambda e: e.memset(P['ones_bf'], 1.0 / 1024), writes=[P['ones_bf_b']])
    kb.op('pool', lambda e: e.memset(P['one_row'], 1.0), writes=[P['one_row_b']])
    kb.op('pool', lambda e: e.memset(P['eps'], EPS), writes=[P['eps_b']])
    kb.op('pool', lambda e: e.memset(P['ident'], 0.0), writes=[P['ident_b']])
    kb.op('pool', lambda e: e.affine_select(out=P['ident'], in_=P['ident'], pattern=[[-1, 128]],
                                            compare_op=ALU.not_equal, fill=1.0, base=0, channel_multiplier=1),
          reads=[P['ident_b']], writes=[P['ident_b']])
    cv = C.tile([8, 2], F32)
    ca = C.tile([8, 2], F32)
    MOD = C.tile([4, 48, 2], F32)
    SC = C.tile([4, 6, 8, 2], F32)
    NG = C.tile([4, 4, 8], F32)
    AB = C.tile([4, 48], F32)
    P['SC'] = SC
    P['SC_b'] = Buf()
    C.end_persist()
    b_cv, b_ca, b_mod, b_ng, b_ab = Buf(), Buf(), Buf(), Buf(), Buf()
    kb.dma('sp', cv, T['cvec'][:, :, :], writes=[b_cv])
    kb.dma('sp', NG, T['norm_gT'][:, :, :, :], writes=[b_ng])
    kb.dma('sp', AB, T['ada_bT'][:, :, :], writes=[b_ab])
    kb.op('act', lambda e: e.activation(out=ca, in_=cv, func=AF.Silu), reads=[b_cv], writes=[b_ca])
    Wt = [C.tile([8, 768], F32) for _ in range(2)]
    Wb = bufs(2)
    it = 0
    for l in range(4):
        pst, pb = C.psum(96)
        for nb in range(8):
            w = Wt[it % 2]
            wb = Wb[it % 2]
            it += 1
            src = T['ada_w'][l].rearrange("(k p) n -> p k n", p=128)[:, :, nb * 768:(nb + 1) * 768]
            kb.dma('sp', w, src, writes=[wb])
            for j in range(6):
                n = nb * 6 + j
                for k in range(8):
                    mm(C, pst[:, 2 * n:2 * n + 2], pb, w[:, k, j * 128:(j + 1) * 128], [wb], ca[:, k, :], [b_ca],
                       k == 0, k == 7)
        pv = pst.rearrange("p (n w) -> p n w", w=2)
        kb.op('dve', lambda e, l=l, pv=pv: e.tensor_tensor(out=MOD[:, l], in0=pv,
                                                            in1=AB[:, l].unsqueeze(2).broadcast_to([128, 48, 2]),
                                                            op=ALU.add),
              reads=pb + [b_ab], writes=[b_mod], strict=True)

        def m(j6, l=l):
            return MOD[:, l, j6 * 8:(j6 + 1) * 8, :]

        def g(j, l=l):
            return NG[:, l, j, :].unsqueeze(2).broadcast_to([128, 8, 2])
        rd = [b_mod, b_ng]
        wr = [P['SC_b']]
        kb.op('dve', lambda e, l=l, m=m, g=g: e.scalar_tensor_tensor(out=SC[:, l, 0], in0=m(1), scalar=1.0, in1=g(0),
                                                                    op0=ALU.add, op1=ALU.mult), reads=rd, writes=wr, strict=True)
        kb.op('dve', lambda e, l=l, m=m: e.tensor_copy(out=SC[:, l, 1], in_=m(0)), reads=rd, writes=wr, strict=True)
        kb.op('dve', lambda e, l=l, m=m, g=g: e.tensor_tensor(out=SC[:, l, 2], in0=m(2), in1=g(1), op=ALU.mult),
              reads=rd, writes=wr, strict=True)
        kb.op('dve', lambda e, l=l, m=m, g=g: e.scalar_tensor_tensor(out=SC[:, l, 3], in0=m(4), scalar=1.0, in1=g(2),
                                                                    op0=ALU.add, op1=ALU.mult), reads=rd, writes=wr, strict=True)
        kb.op('dve', lambda e, l=l, m=m: e.tensor_copy(out=SC[:, l, 4], in_=m(3)), reads=rd, writes=wr, strict=True)
        kb.op('dve', lambda e, l=l, m=m, g=g: e.tensor_tensor(out=SC[:, l, 5], in0=m(5), in1=g(3), op=ALU.mult),
              reads=rd, writes=wr, strict=True)
    return P


def sc(P, l, kind, c, w):
    return P['SC'][:, l, kind, c, w:w + 1]


class Small:
    def __init__(self, C, n, free, dt):
        self.t = [C.tile(free, dt) for _ in range(n)]
        self.b = bufs(n)
        self.i = 0

    def get(self):
        i = self.i
        self.i = (i + 1) % len(self.t)
        return self.t[i], self.b[i]


def rstd_from_psum(C, P, pst, pb, RS):
    rs, rb = RS.get()
    n = pst.shape[-1]
    rs = rs[:, 0:n]
    C.kb.op('act', lambda e: e.activation(out=rs, in_=pst, func=AF.Sqrt, bias=P['eps'][:, 0:1]),
            reads=pb + [P['eps_b']], writes=[rb])
    C.kb.op('dve', lambda e: e.reciprocal(out=rs, in_=rs), reads=[rb], writes=[rb])
    return rs, rb


def prenorm(C, P, XL, XLb, n, H, Hb, l, kA, kB, w, SQ, RS, TMP):
    kb = C.kb
    pst, pb = C.psum(n)
    sq = []
    for c in range(8):
        s, sb_ = SQ.get()
        s = s[:, 0:n]
        kb.op('act', lambda e, s=s, c=c: e.activation(out=s, in_=XL[:, c, 0:n], func=AF.Square), reads=[XLb],
              writes=[sb_])
        sq.append((s, sb_))
        if c >= 1:
            s0, sb0 = sq[c - 1]
            mm(C, pst, pb, P['ones_bf'], [P['ones_bf_b']], s0, [sb0], c - 1 == 0, False)
    s0, sb0 = sq[7]
    mm(C, pst, pb, P['ones_bf'], [P['ones_bf_b']], s0, [sb0], False, True)
    rs, rb = rstd_from_psum(C, P, pst, pb, RS)
    for c in range(8):
        t, tb = TMP.get()
        t = t[:, 0:n]
        kb.op('dve', lambda e, t=t, c=c: e.scalar_tensor_tensor(out=t, in0=XL[:, c, 0:n], scalar=sc(P, l, kA, c, w),
                                                               in1=rs, op0=ALU.mult, op1=ALU.mult),
              reads=[XLb, rb, P['SC_b']], writes=[tb])
        kb.op('act', lambda e, t=t, c=c: e.activation(out=H[:, c, 0:n], in_=t, func=AF.Identity,
                                                      bias=sc(P, l, kB, c, w)),
              reads=[tb, P['SC_b']], writes=[Hb[c]])


class PostRes:
    def __init__(self, C, P, n, Yf, Yfb, SQ, RS, TMP, XR):
        self.C, self.P, self.n = C, P, n
        self.Yf, self.Yfb, self.SQ, self.RS, self.TMP, self.XR = Yf, Yfb, SQ, RS, TMP, XR
        self.pst, self.pb = C.psum(n)
        self.pending = None
        self.cnt = 0

    def _flush(self, last):
        if self.pending is not None:
            s0, sb0 = self.pending
            n = self.n
            for h in range(2):
                mm(self.C, self.pst, self.pb, self.P['ones_bf'], [self.P['ones_bf_b']], s0[:, h * n:(h + 1) * n], [sb0],
                   self.cnt == 0, last and h == 1)
                self.cnt += 1
            self.pending = None

    def pair(self, m, yp, ypb, bias=None, bias_b=None):
        C, n = self.C, self.n
        self._flush(False)
        s, sb_ = self.SQ.get()
        s = s[:, 0:2 * n]
        if bias is None:
            yv = yp.rearrange("p (a t) -> p a t", a=2)
            C.kb.op('act', lambda e: e.activation(out=self.Yf[:, m:m + 2, 0:n], in_=yv, func=AF.Identity), reads=ypb,
                    writes=[self.Yfb[m], self.Yfb[m + 1]])
            C.kb.op('act', lambda e: e.activation(out=s, in_=yp, func=AF.Square), reads=ypb, writes=[sb_])
        else:
            for h in range(2):
                C.kb.op('act', lambda e, h=h: e.activation(out=self.Yf[:, m + h, 0:n], in_=yp[:, h * n:(h + 1) * n],
                                                          func=AF.Identity, bias=bias[:, m + h:m + h + 1]),
                        reads=ypb + [bias_b], writes=[self.Yfb[m + h]])
                C.kb.op('act', lambda e, h=h: e.activation(out=s[:, h * n:(h + 1) * n], in_=yp[:, h * n:(h + 1) * n],
                                                          func=AF.Square, bias=bias[:, m + h:m + h + 1]),
                        reads=ypb + [bias_b], writes=[sb_])
        self.pending = (s, sb_)

    def finish(self, l, kG, w, src, dst, col0, srcb, dstb):
        C, P, n = self.C, self.P, self.n
        self._flush(True)
        rs, rb = rstd_from_psum(C, P, self.pst, self.pb, self.RS)
        for c in range(8):
            xr, xrb = self.XR.get()
            xr = xr[:, 0:n]
            C.kb.dma('sp', xr, src[c * 128:(c + 1) * 128, col0:col0 + n], reads=srcb, writes=[xrb])
            t, tb = self.TMP.get()
            t = t[:, 0:n]
            C.kb.op('dve', lambda e, t=t, c=c: e.scalar_tensor_tensor(out=t, in0=self.Yf[:, c, 0:n],
                                                                   scalar=sc(P, l, kG, c, w), in1=rs,
                                                                   op0=ALU.mult, op1=ALU.mult),
                    reads=[self.Yfb[c], rb, P['SC_b']], writes=[tb])
            C.kb.op('dve', lambda e, t=t, xr=xr: e.tensor_tensor(out=xr, in0=xr, in1=t, op=ALU.add),
                    reads=[tb, xrb], writes=[xrb])
            C.kb.dma('sp', dst[c * 128:(c + 1) * 128, col0:col0 + n], xr, reads=[xrb], writes=dstb)


MAXG = None
NA_PARTS = 3
STOP = 99
FULLBANK = False
SUB = 99


def groups(G, with_ctx=True, full=False):
    if MAXG is not None and not full:
        return [(g * G, G, 0) for g in range(MAXG)] + ([(NL, min(G, NCX), 1)] if with_ctx else [])
    out = []
    for g in range(NL // G):
        out.append((g * G, G, 0))
    if with_ctx:
        for g in range(NCX // min(G, NCX)):
            n = min(G, NCX)
            out.append((NL + g * n, n, 1))
    return out


def xbufs(T, key, col0, n):
    return T[key + '_b'][col0 // 128:(col0 + n) // 128]


def phase_ffn(C, P, T, l, src, dst, with_ctx, final=False):
    kb = C.kb
    C.new_phase()
    G = 256
    W1 = C.tile([8, 4096], BF16)
    W2 = C.tile([32, 1024], BF16)
    W1b, W2b = bufs(8), bufs(32)
    load_weight(C, W1, W1b, T['ffn_w1'][l], 8)
    load_weight(C, W2, W2b, T['ffn_w2'][l], 32)
    XL = [C.tile([8, G], F32) for _ in range(2)]
    XLb = bufs(2)
    H = C.tile([8, G], BF16)
    Hb = bufs(8)
    H1 = C.tile([32, G], BF16)
    H1b = bufs(32)
    Yf = C.tile([8, G], F32)
    Yfb = bufs(8)
    SQ = Small(C, 3, [2 * G], BF16)
    RS = Small(C, 2, [G], F32)
    TMP = Small(C, 4, [G], F32)
    XR = Small(C, 4, [G], F32)
    RL = Small(C, 3, [2 * G], F32)
    grp = groups(G, with_ctx)
    srcv = T[src].rearrange("(c p) t -> p c t", p=128)

    def load(i):
        col0, n, w = grp[i]
        kb.dma('sp', XL[i % 2][:, :, 0:n], srcv[:, :, col0:col0 + n], reads=xbufs(T, src, col0, n),
               writes=[XLb[i % 2]])
    load(0)
    prenorm(C, P, XL[0], XLb[0], grp[0][1], H, Hb, l, 3, 4, grp[0][2], SQ, RS, TMP)
    for i, (col0, n, w) in enumerate(grp):
        if i + 1 < len(grp):
            load(i + 1)
        for mp in range(16):
            pt, pb = C.psum(2 * n)
            for h in range(2):
                m = 2 * mp + h
                for k in range(8):
                    mm(C, pt[:, h * n:(h + 1) * n], pb, W1[:, k, m * 128:(m + 1) * 128], [W1b[k]], H[:, k, 0:n],
                       [Hb[k]], k == 0, k == 7)
            r, rb = RL.get()
            r = r[:, 0:2 * n]
            kb.op('act', lambda e, r=r, pt=pt: e.activation(out=r, in_=pt, func=AF.Relu), reads=pb, writes=[rb])
            rv = r.rearrange("p (a t) -> p a t", a=2)
            kb.op('dve', lambda e, rv=rv, mp=mp: e.tensor_tensor(out=H1[:, 2 * mp:2 * mp + 2, 0:n], in0=rv, in1=rv,
                                                                 op=ALU.mult),
                  reads=[rb], writes=[H1b[2 * mp], H1b[2 * mp + 1]])
        if i + 1 < len(grp):
            prenorm(C, P, XL[(i + 1) % 2], XLb[(i + 1) % 2], grp[i + 1][1], H, Hb, l, 3, 4, grp[i + 1][2], SQ, RS, TMP)
        pr = PostRes(C, P, n, Yf, Yfb, SQ, RS, TMP, XR)
        for mp in range(4):
            pt, pb = C.psum(2 * n)
            for h in range(2):
                m = 2 * mp + h
                for k in range(32):
                    mm(C, pt[:, h * n:(h + 1) * n], pb, W2[:, k, m * 128:(m + 1) * 128], [W2b[k]], H1[:, k, 0:n],
                       [H1b[k]], k == 0, k == 31)
            pr.pair(2 * mp, pt, pb)
        if final and w == 0:
            pr.finish(l, 5, w, T[src], T['yout'], col0, xbufs(T, src, col0, n), xbufs(T, 'yout', col0, n))
        else:
            pr.finish(l, 5, w, T[src], T[dst], col0, xbufs(T, src, col0, n), xbufs(T, dst, col0, n))


GELU_A = math.sqrt(0.044715)
GELU_B = 2.0 * math.sqrt(2.0 / math.pi)


def gelu_psum(C, zp, zpb, out, outb, GT, a=1):
    kb = C.kb
    n = zp.shape[-1]
    t, tb = GT.get()
    t = t[:, 0:n]
    if a > 1:
        t = t.rearrange("p (a t) -> p a t", a=a)
        zp = zp.rearrange("p (a t) -> p a t", a=a)
    kb.op('act', lambda e: e.activation(out=t, in_=zp, func=AF.Square, scale=GELU_A), reads=zpb, writes=[tb])
    kb.op('dve', lambda e: e.scalar_tensor_tensor(out=t, in0=t, scalar=1.0, in1=zp, op0=ALU.add, op1=ALU.mult),
          reads=zpb + [tb], writes=[tb])
    kb.op('act', lambda e: e.activation(out=t, in_=t, func=AF.Sigmoid, scale=GELU_B), reads=[tb], writes=[tb])
    kb.op('dve', lambda e: e.tensor_tensor(out=out, in0=t, in1=zp, op=ALU.mult), reads=zpb + [tb], writes=outb)


def phase_gmlp_a(C, P, T, l, j, src, with_ctx):
    kb = C.kb
    C.new_phase()
    G = 256
    Win = C.tile([8, 6144], BF16)
    Winb = bufs(8)
    load_weight(C, Win, Winb, T['gm_w_in'][j], 8)
    brow = C.tile([6144], BF16)
    browb = Buf()
    kb.dma('pool', brow[0:1, :], T['gm_b_in'][j:j + 1, :], writes=[browb])
    wsT = C.tile([8, 128], BF16)
    wsTb = Buf()
    kb.dma('pool', wsT, T['gm_wsT'][j].rearrange("q (g p) -> q g p", g=8), writes=[wsTb])
    bsbc = C.tile([8, 128], F32)
    bsbcb = Buf()
    kb.dma('sp', bsbc, T['gm_bs'][j:j + 1, :].partition_broadcast(128).rearrange("q (g p) -> q g p", g=8)
           if False else T['gm_bs_bc'][j].rearrange("q (g p) -> q g p", g=8), writes=[bsbcb])
    lng = C.tile([24], F32)
    lngb = Buf()
    kb.dma('sp', lng, T['gm_ln_gT'][j], writes=[lngb])
    XL = [C.tile([8, G], F32) for _ in range(1)]
    XLb = bufs(1)
    H = C.tile([8, G], BF16)
    Hb = bufs(8)
    U = [C.tile([24, G], BF16) for _ in range(2)]
    Ub = [bufs(24), bufs(24)]
    ZG = [C.tile([3072], F32) for _ in range(1)]
    ZGb = [bufs(6) for _ in range(1)]
    VH = [C.tile([3072], BF16) for _ in range(2)]
    VHb = bufs(2)
    MV = Small(C, 2, [8], F32)
    SQ = Small(C, 3, [G], BF16)
    RS = Small(C, 2, [G], F32)
    TMP = Small(C, 3, [G], F32)
    GT = Small(C, 3, [512], F32)
    grp = groups(G, with_ctx)
    srcv = T[src].rearrange("(c p) t -> p c t", p=128)
    pdv = T['pd'].rearrange("(c p) t -> p c t", p=128)
    vit = 0
    for i, (col0, n, w) in enumerate(grp):
        kb.dma('sp', XL[0][:, :, 0:n], srcv[:, :, col0:col0 + n], reads=xbufs(T, src, col0, n), writes=[XLb[0]])
        prenorm(C, P, XL[0], XLb[0], n, H, Hb, l, 0, 1, w, SQ, RS, TMP)
        Ui, Ubi = U[i % 2], Ub[i % 2]
        vhs = []
        for s in range(n // 128):
            zg, zgb = ZG[0], ZGb[0]
            for q in range(6):
                pt, pb = C.psum(512)
                for k in range(8):
                    mm(C, pt, pb, H[:, k, s * 128:(s + 1) * 128], [Hb[k]],
                       Win[:, k, 3072 + q * 512:3072 + (q + 1) * 512], [Winb[k]], k == 0, False)
                mm(C, pt, pb, P['one_row'][0:1, 0:128], [P['one_row_b']],
                   brow[0:1, 3072 + q * 512:3072 + (q + 1) * 512], [browb], False, True)
                gelu_psum(C, pt, pb, zg[:, q * 512:(q + 1) * 512], [zgb[q]], GT)
            mv, mvb = MV.get()
            vh, vhb = VH[vit % 2], VHb[vit % 2]
            vit += 1
            kb.op('dve', lambda e, mv=mv, zg=zg: e.reduce_sum(out=mv[:, 0:1], in_=zg, axis=mybir.AxisListType.X),
                  reads=zgb, writes=[mvb], strict=True)
            kb.op('act', lambda e, mv=mv, zg=zg, vh=vh: e.activation(out=vh, in_=zg, func=AF.Square,
                                                                   accum_out=mv[:, 1:2]),
                  reads=zgb + [mvb], writes=[mvb, vhb], strict=True)
            kb.op('dve', lambda e, mv=mv: e.tensor_scalar(out=mv[:, 2:3], in0=mv[:, 0:1], scalar1=1.0 / 3072,
                                                          scalar2=None, op0=ALU.mult), reads=[mvb], writes=[mvb], strict=True)
            kb.op('dve', lambda e, mv=mv: e.scalar_tensor_tensor(out=mv[:, 3:4], in0=mv[:, 2:3], scalar=-1.0,
                                                                 in1=mv[:, 2:3], op0=ALU.mult, op1=ALU.mult),
                  reads=[mvb], writes=[mvb], strict=True)
            kb.op('dve', lambda e, mv=mv: e.scalar_tensor_tensor(out=mv[:, 4:5], in0=mv[:, 1:2], scalar=1.0 / 3072,
                                                                 in1=mv[:, 3:4], op0=ALU.mult, op1=ALU.add),
                  reads=[mvb], writes=[mvb], strict=True)
            kb.op('act', lambda e, mv=mv: e.activation(out=mv[:, 5:6], in_=mv[:, 4:5], func=AF.Sqrt,
                                                       bias=P['eps'][:, 0:1]), reads=[mvb, P['eps_b']], writes=[mvb], strict=True)
            kb.op('dve', lambda e, mv=mv: e.reciprocal(out=mv[:, 5:6], in_=mv[:, 5:6]), reads=[mvb], writes=[mvb], strict=True)
            kb.op('dve', lambda e, mv=mv: e.scalar_tensor_tensor(out=mv[:, 6:7], in0=mv[:, 2:3], scalar=-1.0,
                                                                 in1=mv[:, 5:6], op0=ALU.mult, op1=ALU.mult),
                  reads=[mvb], writes=[mvb], strict=True)
            kb.op('act', lambda e, vh=vh, zg=zg, mv=mv: e.activation(out=vh, in_=zg, func=AF.Identity,
                                                                   scale=mv[:, 5:6], bias=mv[:, 6:7]),
                  reads=zgb + [mvb], writes=[vhb])
            vhs.append((vh, vhb))
            if 'dbg_vh' in T and w == 1 and s == 1:
                kb.dma('sp', T['dbg_vh'][:, :], vh, reads=[vhb], writes=[Buf()])
                kb.dma('sp', T['dbg_mv'][:, :], mv, reads=[mvb], writes=[Buf()])
                kb.dma('sp', T['dbg_zg'][:, :], zg, reads=zgb, writes=[Buf()])
        for mp in range(12):
            pt, pb = C.psum(2 * n)
            for h in range(2):
                m = 2 * mp + h
                po = pt[:, h * n:(h + 1) * n]
                for k in range(8):
                    mm(C, po, pb, Win[:, k, m * 128:(m + 1) * 128], [Winb[k]], H[:, k, 0:n], [Hb[k]], k == 0, False)
                mm(C, po, pb, brow[0:1, m * 128:(m + 1) * 128], [browb], P['one_row'][0:1, 0:n], [P['one_row_b']],
                   False, True)
            gelu_psum(C, pt, pb, Ui[:, 2 * mp:2 * mp + 2, 0:n], [Ubi[2 * mp], Ubi[2 * mp + 1]], GT, a=2)
        for s in range(n // 128):
            vh, vhb = vhs[s]
            for c4 in range(6):
                pt, pb = C.psum(512)
                for jj in range(4):
                    cc = c4 * 4 + jj
                    mm(C, pt[:, jj * 128:(jj + 1) * 128], pb, vh[:, cc * 128:(cc + 1) * 128], [vhb],
                       wsT[:, cc // 3, :], [wsTb], True, True)
                t, tb = GT.get()
                for jj in range(4):
                    cc = c4 * 4 + jj
                    kb.op('dve', lambda e, t=t, pt=pt, cc=cc, jj=jj: e.scalar_tensor_tensor(
                        out=t[:, jj * 128:(jj + 1) * 128], in0=pt[:, jj * 128:(jj + 1) * 128],
                        scalar=lng[:, cc:cc + 1], in1=bsbc[:, cc // 3, :], op0=ALU.mult, op1=ALU.add),
                        reads=pb + [lngb, bsbcb], writes=[tb])
                tv = t.rearrange("p (a t) -> p a t", a=4)
                kb.op('pool', lambda e, tv=tv, c4=c4, s=s, Ui=Ui: e.tensor_tensor(
                    out=Ui[:, c4 * 4:c4 * 4 + 4, s * 128:(s + 1) * 128],
                    in0=Ui[:, c4 * 4:c4 * 4 + 4, s * 128:(s + 1) * 128], in1=tv, op=ALU.mult),
                    reads=[tb] + Ubi[c4 * 4:c4 * 4 + 4], writes=Ubi[c4 * 4:c4 * 4 + 4])
        kb.dma('sp', pdv[:, :, col0:col0 + n], Ui[:, :, 0:n], reads=Ubi, writes=xbufs(T, 'pd', col0, n))


def phase_gmlp_b(C, P, T, l, j, src, dst, with_ctx):
    kb = C.kb
    C.new_phase()
    G = 256
    Wo = C.tile([24, 1024], BF16)
    Wob = bufs(24)
    load_weight(C, Wo, Wob, T['gm_w_out'][j], 24)
    PT = [C.tile([24, G], BF16) for _ in range(2)]
    PTb = bufs(2)
    Yf = C.tile([8, G], F32)
    Yfb = bufs(8)
    SQ = Small(C, 3, [2 * G], BF16)
    RS = Small(C, 2, [G], F32)
    TMP = Small(C, 4, [G], F32)
    XR = Small(C, 4, [G], F32)
    grp = groups(G, with_ctx)
    pdv = T['pd'].rearrange("(c p) t -> p c t", p=128)

    def load(i):
        col0, n, w = grp[i]
        kb.dma('sp', PT[i % 2][:, :, 0:n], pdv[:, :, col0:col0 + n], reads=xbufs(T, 'pd', col0, n),
               writes=[PTb[i % 2]])
    load(0)
    for i, (col0, n, w) in enumerate(grp):
        if i + 1 < len(grp):
            load(i + 1)
        pr = PostRes(C, P, n, Yf, Yfb, SQ, RS, TMP, XR)
        for mp in range(4):
            pt, pb = C.psum(2 * n)
            for h in range(2):
                m = 2 * mp + h
                for k in range(24):
                    mm(C, pt[:, h * n:(h + 1) * n], pb, Wo[:, k, m * 128:(m + 1) * 128], [Wob[k]],
                       PT[i % 2][:, k, 0:n], [PTb[i % 2]], k == 0, k == 23)
            pr.pair(2 * mp, pt, pb)
        pr.finish(l, 2, w, T[src], T[dst], col0, xbufs(T, src, col0, n), xbufs(T, dst, col0, n))


def phase_na_a(C, P, T, l, src):
    kb = C.kb
    C.new_phase()
    G = 256
    W = C.tile([8, 3072], BF16)
    Wb = bufs(8)
    load_weight(C, W, Wb, T['na_w_qkv'][0], 8)
    bqk = C.tile([24], F32)
    bqkb = Buf()
    kb.dma('sp', bqk, T['na_bqkvT'][:, :], writes=[bqkb])
    bq8 = C.tile([8], F32)
    kb.op('dve', lambda e: e.tensor_scalar(out=bq8, in0=bqk[:, 0:8], scalar1=0.125, scalar2=None, op0=ALU.mult),
          reads=[bqkb], writes=[bqkb], strict=True)
    brow = C.tile([1024], BF16)
    browb = Buf()
    kb.dma('pool', brow[0:1, :], T['na_b_qkv'][0:1, 2048:3072], writes=[browb])
    XL = [C.tile([8, G], F32) for _ in range(2)]
    XLb = bufs(2)
    H = C.tile([8, G], BF16)
    Hb = bufs(8)
    QT = [C.tile([8, G], BF16) for _ in range(2)]
    QTb = bufs(2)
    KT = [C.tile([8, G], BF16) for _ in range(2)]
    KTb = bufs(2)
    Vt = [C.tile([16, 65], BF16) for _ in range(3)]
    Vtb = bufs(3)
    for i in range(3):
        kb.op('pool', lambda e, i=i: e.memset(Vt[i], 1.0), writes=[Vtb[i]])
    SQ = Small(C, 3, [G], BF16)
    RS = Small(C, 2, [G], F32)
    TMP = Small(C, 3, [G], F32)
    grp = groups(G, True, full=True)
    srcv = T[src].rearrange("(c p) t -> p c t", p=128)
    qv = T['qT'].rearrange("(c p) t -> p c t", p=128)
    kv = T['kT'].rearrange("(c p) t -> p c t", p=128)

    def load(i):
        col0, n, w = grp[i]
        kb.dma('sp', XL[i % 2][:, :, 0:n], srcv[:, :, col0:col0 + n], reads=xbufs(T, src, col0, n),
               writes=[XLb[i % 2]])
    load(0)
    vi = 0
    for i, (col0, n, w) in enumerate(grp):
        if i + 1 < len(grp):
            load(i + 1)
        prenorm(C, P, XL[i % 2], XLb[i % 2], n, H, Hb, l, 0, 1, w, SQ, RS, TMP)
        for which, dstT, dstb in ((0, QT[i % 2], QTb[i % 2]), (1, KT[i % 2], KTb[i % 2])):
            for mp in range(4):
                pt, pb = C.psum(2 * n)
                for h in range(2):
                    m = 2 * mp + h
                    mcol = which * 1024 + m * 128
                    for k in range(8):
                        mm(C, pt[:, h * n:(h + 1) * n], pb, W[:, k, mcol:mcol + 128], [Wb[k]], H[:, k, 0:n], [Hb[k]],
                           k == 0, k == 7)
                for h in range(2):
                    m = 2 * mp + h
                    if which == 0:
                        kb.op('act', lambda e, pt=pt, h=h, m=m, dstT=dstT: e.activation(
                            out=dstT[:, m, 0:n], in_=pt[:, h * n:(h + 1) * n], func=AF.Identity, scale=0.125,
                            bias=bq8[:, m:m + 1]), reads=pb + [bqkb], writes=[dstb])
                    else:
                        kb.op('act', lambda e, pt=pt, h=h, m=m, dstT=dstT: e.activation(
                            out=dstT[:, m, 0:n], in_=pt[:, h * n:(h + 1) * n], func=AF.Identity,
                            bias=bqk[:, 8 + m:9 + m]), reads=pb + [bqkb], writes=[dstb])
        kb.dma('sp', qv[:, :, col0:col0 + n], QT[i % 2][:, :, 0:n], reads=[QTb[i % 2]], writes=xbufs(T, 'qT', col0, n))
        kb.dma('sp', kv[:, :, col0:col0 + n], KT[i % 2][:, :, 0:n], reads=[KTb[i % 2]], writes=xbufs(T, 'kT', col0, n))
        for s in range(n // 128):
            vt, vtb = Vt[vi % 3], Vtb[vi % 3]
            vi += 1
            for nq in range(2):
                pt, pb = C.psum(512)
                for k in range(8):
                    mm(C, pt, pb, H[:, k, s * 128:(s + 1) * 128], [Hb[k]],
                       W[:, k, 2048 + nq * 512:2048 + (nq + 1) * 512], [Wb[k]], k == 0, False)
                mm(C, pt, pb, P['one_row'][0:1, 0:128], [P['one_row_b']], brow[0:1, nq * 512:(nq + 1) * 512], [browb],
                   False, True)
                kb.op('act', lambda e, pt=pt, vt=vt, nq=nq: e.activation(
                    out=vt[:, nq * 8:(nq + 1) * 8, 0:64], in_=pt.rearrange("p (h d) -> p h d", h=8), func=AF.Identity),
                    reads=pb, writes=[vtb])
            t0 = col0 + s * 128
            kb.dma('sp', T['vD'][t0:t0 + 128, :], vt.rearrange("p h d -> p (h d)"), reads=[vtb],
                   writes=xbufs(T, 'vD', t0, 128))


NA_NEG = -30000.0


def na_bias_table(rpb):
    rpb = np.asarray(rpb, dtype=np.float32)
    qc = np.arange(64)[None, :]
    kc = np.arange(64)[:, None]
    ws = np.clip(qc - 8, 0, 48)
    valid = (kc >= ws) & (kc < ws + 16)
    dcol = np.clip(kc - qc, -15, 15) + 15
    tab = np.full((2, 64, 16, 16, 64), NA_NEG, dtype=np.float32)

    def fill(half, pid, d):
        vals = rpb[:, d, :][:, dcol]
        vals = np.where(valid[None], vals, NA_NEG)
        tab[half, :, pid, :, :] = vals.transpose(1, 0, 2)
    for d in range(14):
        fill(0, d, d)
        fill(1, d, d + 1)
    fill(1, 14, 3)
    fill(0, 15, 10)
    return np.ascontiguousarray(tab.reshape(128, 16 * 16 * 64))


def phase_na_b(C, P, T, l, src, dst):
    kb = C.kb
    C.new_phase()
    Wo = C.tile([8, 1024], BF16)
    Wob = bufs(8)
    load_weight(C, Wo, Wob, T['na_w_o'][0], 8)
    bo = C.tile([8], F32)
    bob = Buf()
    kb.dma('sp', bo, T['na_b_oT'][:, :], writes=[bob])
    BT = C.tile([16, 16, 64], BF16)
    BTb = bufs(16)
    btv = T['na_bt'].rearrange("p (i x) -> p i x", i=16)
    BTf = BT.rearrange("p i h q -> p i (h q)")
    for i in range(16):
        kb.dma('pool', BTf[:, i, :], btv[:, i, :], writes=[BTb[i]])
    KcT = C.tile([8, 256], BF16)
    Vc = C.tile([2, 1040], BF16)
    Kcb, Vcb = Buf(), Buf()
    kvd = T['kT'].rearrange("(c p) t -> p c t", p=128)
    qvd = T['qT'].rearrange("(c p) t -> p c t", p=128)
    kb.dma('sp', KcT, kvd[:, :, NL:NT], reads=xbufs(T, 'kT', NL, NCX), writes=[Kcb])
    kb.dma('sp', Vc, T['vD'][NL:NT, :].rearrange("(j p) c -> p j c", p=128), reads=xbufs(T, 'vD', NL, NCX),
           writes=[Vcb])
    KT = [C.tile([8, 1024], BF16) for _ in range(2)]
    KTb = bufs(2)
    Vw = [C.tile([8, 1040], BF16) for _ in range(2)]
    Vwb = bufs(2)
    QT = [C.tile([8, 512], BF16) for _ in range(2)]
    QTb = bufs(2)
    PTs = Small(C, 3, [448], BF16)
    Ot = Small(C, 2, [1024], BF16)
    RD = Small(C, 3, [8], F32)
    OT = [C.tile([8, 256], BF16) for _ in range(2)]
    OTb = bufs(2)
    Yf = C.tile([8, 256], F32)
    Yfb = bufs(8)
    SQ = Small(C, 3, [512], BF16)
    RS = Small(C, 2, [256], F32)
    TMP = Small(C, 4, [256], F32)
    XR = Small(C, 4, [256], F32)
    S_BANKS = [0, 1, 2]
    PV_BANKS = [3, 4, 5]
    C.default_banks = [6, 7]
    ident = P['ident']
    psT_all = C.ps.bitcast(BF16)

    def attend_block(qt, qtb, qcol, lat_tiles, ot, otb):
        nt = len(lat_tiles)
        ntot = nt + 2
        hgroups = [list(range(0, 7)), list(range(7, 14)), list(range(14, 16))]
        pend = None

        def do_pv(pt, ptb, po, pvb, h):
            for t, (ktf, ktb, bid, vf, vb) in enumerate(lat_tiles):
                mm(C, po, pvb, pt[:, t * 64:(t + 1) * 64], [ptb], vf(h), [vb], t == 0, False)
            for c in range(2):
                mm(C, po, pvb, pt[:, (nt + c) * 64:(nt + c + 1) * 64], [ptb], Vc[:, c, h * 65:(h + 1) * 65], [Vcb],
                   (nt == 0 and c == 0), c == 1)

        def do_norm(pv, pvb, hg):
            nh = len(hg)
            rd, rdb = RD.get()
            pv3 = pv[0:64, :].rearrange("p (h d) -> p h d", d=65)
            kb.op('dve', lambda e: e.reciprocal(out=rd[0:64, 0:nh], in_=pv3[:, :, 64]), reads=pvb, writes=[rdb])
            h0 = hg[0]
            kb.op('dve', lambda e: e.tensor_tensor(
                out=ot[0:64, h0 * 64:(h0 + nh) * 64].rearrange("p (h d) -> p h d", d=64), in0=pv3[:, :, 0:64],
                in1=rd[0:64, 0:nh].unsqueeze(2).broadcast_to([64, nh, 64]), op=ALU.mult),
                reads=pvb + [rdb], writes=[otb], strict=True)

        for hg in hgroups:
            pv, pvb = C.psum(len(hg) * 65, banks=PV_BANKS)
            for hh, h in enumerate(hg):
                hp, ch = h % 2, h // 2
                q_ap = qt[hp * 64:(hp + 1) * 64, ch, qcol:qcol + 64]
                st, stb = C.psum(ntot * 64, banks=S_BANKS)
                for t, (ktf, ktb, bid, vf, vb) in enumerate(lat_tiles):
                    mm(C, st[:, t * 64:(t + 1) * 64], stb, ktf(h), [ktb], q_ap, [qtb], True, False)
                    mm(C, st[:, t * 64:(t + 1) * 64], stb, ident, [P['ident_b']], BT[:, bid, h, :], [BTb[bid]],
                       False, True)
                for c in range(2):
                    mm(C, st[:, (nt + c) * 64:(nt + c + 1) * 64], stb,
                       KcT[hp * 64:(hp + 1) * 64, ch, c * 128:(c + 1) * 128], [Kcb], q_ap, [qtb], True, True)
                pt, ptb = PTs.get()
                pt = pt[:, 0:ntot * 64]
                kb.op('act', lambda e, pt=pt, st=st: e.activation(out=pt, in_=st, func=AF.Exp), reads=stb, writes=[ptb])
                if pend is not None:
                    do_pv(*pend[0])
                    if pend[1] is not None:
                        do_norm(*pend[1])
                last_in_group = hh == len(hg) - 1
                pend = ((pt, ptb, pv[0:64, hh * 65:(hh + 1) * 65], pvb, h), (pv, pvb, hg) if last_in_group else None)
        do_pv(*pend[0])
        do_norm(*pend[1])

    def transpose_block(ot, otb, OTt, OTtb, slot):
        pt, pb = C.psum(256)
        bank = int(pb[0].name[2:])
        ptb16 = psT_all[:, bank * 1024:bank * 1024 + 512]
        for c in range(8):
            C.kb.op('pe', lambda e, c=c: e.transpose(ptb16[:, c * 64:(c + 1) * 64], ot[0:64, c * 128:(c + 1) * 128],
                                                     ident[0:64, 0:64]),
                    reads=[otb, P['ident_b']], writes=pb)
        kb.op('act', lambda e: e.activation(out=OTt[:, :, slot * 64:(slot + 1) * 64],
                                            in_=ptb16.rearrange("p (c t) -> p c t", c=8), func=AF.Identity),
              reads=pb, writes=[OTtb])

    def project(OTt, OTtb, n, w, col0):
        pr = PostRes(C, P, n, Yf, Yfb, SQ, RS, TMP, XR)
        for mp in range(4):
            pt, pb = C.psum(2 * n, banks=PV_BANKS + S_BANKS)
            for h in range(2):
                m = 2 * mp + h
                for k in range(8):
                    mm(C, pt[:, h * n:(h + 1) * n], pb, Wo[:, k, m * 128:(m + 1) * 128], [Wob[k]], OTt[:, k, 0:n],
                       [OTtb], k == 0, k == 7)
            pr.pair(2 * mp, pt, pb, bias=bo, bias_b=bob)
        pr.finish(l, 2, w, T[src], T[dst], col0, xbufs(T, src, col0, n), xbufs(T, dst, col0, n))

    ngroups = 16 if MAXG is None else MAXG
    oti = 0
    for g in range(ngroups):
        lo = max(0, 8 * g - 4)
        hi = min(128, 8 * g + 12)
        kt, ktb = KT[g % 2], KTb[g % 2]
        vw, vwb = Vw[g % 2], Vwb[g % 2]
        qt, qtb = QT[g % 2], QTb[g % 2]
        nw = (hi - lo) * 64
        kb.dma('sp', kt[:, :, 0:nw], kvd[:, :, lo * 64:hi * 64], reads=xbufs(T, 'kT', lo * 64, nw), writes=[ktb])
        kb.dma('sp', vw[:, 0:nw // 128, :], T['vD'][lo * 64:hi * 64, :].rearrange("(j p) c -> p j c", p=128),
               reads=xbufs(T, 'vD', lo * 64, nw), writes=[vwb])
        kb.dma('sp', qt, qvd[:, :, g * 512:(g + 1) * 512], reads=xbufs(T, 'qT', g * 512, 512), writes=[qtb])
        for rl in range(8):
            r = 8 * g + rl
            rs = min(max(r - 4, 0), 120)
            d0 = rs - r + 7
            tiles = []
            if rs % 2 == 0:
                for i4 in range(4):
                    tiles.append(((rs - lo) // 2 + i4, d0 + 2 * i4))
            else:
                assert d0 == 3
                j0 = (rs - 1 - lo) // 2
                tiles = [(j0, 14), (j0 + 1, 4), (j0 + 2, 6), (j0 + 3, 8), (j0 + 4, 15)]
            lat = []
            for (j, bid) in tiles:
                lat.append((lambda h, j=j: kt[(h % 2) * 64:(h % 2 + 1) * 64, h // 2, j * 128:(j + 1) * 128], ktb, bid,
                            lambda h, j=j: vw[:, j, h * 65:(h + 1) * 65], vwb))
            ot, otb = Ot.get()
            attend_block(qt, qtb, rl * 64, lat, ot, otb)
            OTt, OTtb = OT[oti % 2], OTb[oti % 2]
            transpose_block(ot, otb, OTt, OTtb, rl % 4)
            if rl % 4 == 3:
                project(OTt, OTtb, 256, 0, g * 512 + (rl // 4) * 256)
                oti += 1
    qt, qtb = QT[0], QTb[0]
    kb.dma('sp', qt[:, :, 0:256], qvd[:, :, NL:NT], reads=xbufs(T, 'qT', NL, NCX), writes=[qtb])
    OTt, OTtb = OT[oti % 2], OTb[oti % 2]
    for blk in range(4):
        ot, otb = Ot.get()
        attend_block(qt, qtb, blk * 64, [], ot, otb)
        transpose_block(ot, otb, OTt, OTtb, blk)
    project(OTt, OTtb, 256, 1, NL)


def ml_rope_tables():
    inv = 10000.0 ** (-np.arange(0, 32, 2, dtype=np.float64) / 32.0)
    p = np.arange(128)
    d = p % 64
    axis = d // 32
    i = d % 16
    half = (d % 32) // 16
    t = np.arange(NL)
    pos = np.where(axis[:, None] == 0, (t // 64)[None, :], (t % 64)[None, :]).astype(np.float64)
    ang = pos * inv[i][:, None]
    Cc = np.ones((128, NT), np.float64)
    Ss = np.zeros((128, NT), np.float64)
    Cc[:, :NL] = np.cos(ang)
    Ss[:, :NL] = np.sin(ang) * np.where(half[:, None] == 0, -1.0, 1.0)
    tabs = np.stack([Cc, Ss, Cc * 0.125, Ss * 0.125], axis=0).astype(np.float32)
    col = np.arange(1024)
    dd = col % 64
    perm = np.where((dd % 32) < 16, col + 16, col - 16)
    return np.ascontiguousarray(tabs), perm


def phase_ml_a(C, P, T, l, src):
    kb = C.kb
    C.new_phase()
    G = 256
    W = C.tile([8, 3072], BF16)
    Wb = bufs(8)
    load_weight(C, W, Wb, T['ml_w_in'][0], 8)
    Wsw = C.tile([8, 1024], BF16)
    Wswb = bufs(8)
    load_weight(C, Wsw, Wswb, T['ml_w_qk_sw'], 8)
    Wg = C.tile([8, 32], BF16)
    Wgb = bufs(8)
    load_weight(C, Wg, Wgb, T['ml_w_gate2'], 8)
    bg = C.tile([32], BF16)
    bgb = Buf()
    kb.dma('pool', bg[0:1, :], T['ml_b_gate2'][0:1, :], writes=[bgb])
    XL = [C.tile([8, G], F32) for _ in range(2)]
    XLb = bufs(2)
    H = C.tile([8, G], BF16)
    Hb = bufs(8)
    QK = [C.tile([8, G], BF16) for _ in range(2)]
    QKb = bufs(2)
    OT = [C.tile([8, G], BF16) for _ in range(2)]
    OTb = bufs(2)
    TB = [C.tile([4, G], F32) for _ in range(2)]
    TBb = bufs(2)
    Vt = [C.tile([8, 129], BF16) for _ in range(3)]
    Vtb = bufs(3)
    for i in range(3):
        kb.op('pool', lambda e, i=i: e.memset(Vt[i], 1.0), writes=[Vtb[i]])
    Kt = [C.tile([512], BF16) for _ in range(2)]
    Ktb = bufs(2)
    Gt = [C.tile([32], F32) for _ in range(2)]
    Gtb = bufs(2)
    GE = Small(C, 2, [32], F32)
    SQ = Small(C, 3, [G], BF16)
    RS = Small(C, 2, [G], F32)
    TMP = Small(C, 4, [G], F32)
    grp = groups(G, True, full=True)
    srcv = T[src].rearrange("(c p) t -> p c t", p=128)
    qkv = T['mqk'].rearrange("(c p) t -> p c t", p=128)
    ov = T['moT'].rearrange("(c p) t -> p c t", p=128)
    tabv = T['ml_rope'].rearrange("a p t -> p a t")
    psT_all = C.ps.bitcast(BF16)
    ident = P['ident']

    def load(i):
        col0, n, w = grp[i]
        kb.dma('sp', XL[i % 2][:, :, 0:n], srcv[:, :, col0:col0 + n], reads=xbufs(T, src, col0, n),
               writes=[XLb[i % 2]])
        kb.dma('sp', TB[i % 2][:, :, 0:n], tabv[:, :, col0:col0 + n], writes=[TBb[i % 2]])
    load(0)
    vi = 0
    for i, (col0, n, w) in enumerate(grp):
        if i + 1 < len(grp):
            load(i + 1)
        prenorm(C, P, XL[i % 2], XLb[i % 2], n, H, Hb, l, 0, 1, w, SQ, RS, TMP)
        qk, qkb = QK[i % 2], QKb[i % 2]
        tb, tbb = TB[i % 2], TBb[i % 2]
        for m in range(8):
            pt, pb = C.psum(2 * n)
            for k in range(8):
                mm(C, pt[:, 0:n], pb, W[:, k, m * 128:(m + 1) * 128], [Wb[k]], H[:, k, 0:n], [Hb[k]], k == 0, k == 7)
            for k in range(8):
                mm(C, pt[:, n:2 * n], pb, Wsw[:, k, m * 128:(m + 1) * 128], [Wswb[k]], H[:, k, 0:n], [Hb[k]],
                   k == 0, k == 7)
            ti = 0 if m < 4 else 2
            t1, t1b = TMP.get()
            t2, t2b = TMP.get()
            kb.op('dve', lambda e, t1=t1, pt=pt, ti=ti, tb=tb: e.tensor_tensor(out=t1[:, 0:n], in0=pt[:, 0:n],
                                                                             in1=tb[:, ti, 0:n], op=ALU.mult),
                  reads=pb + [tbb], writes=[t1b])
            kb.op('dve', lambda e, t2=t2, pt=pt, ti=ti, tb=tb: e.tensor_tensor(out=t2[:, 0:n], in0=pt[:, n:2 * n],
                                                                             in1=tb[:, ti + 1, 0:n], op=ALU.mult),
                  reads=pb + [tbb], writes=[t2b])
            kb.op('pool', lambda e, t1=t1, t2=t2, m=m, qk=qk: e.tensor_tensor(out=qk[:, m, 0:n], in0=t1[:, 0:n],
                                                                            in1=t2[:, 0:n], op=ALU.add),
                  reads=[t1b, t2b], writes=[qkb])
        kb.dma('sp', qkv[:, :, col0:col0 + n], qk[:, :, 0:n], reads=[qkb], writes=xbufs(T, 'mqk', col0, n))
        ot, otb = OT[i % 2], OTb[i % 2]
        for mp in range(4):
            pt, pb = C.psum(2 * n)
            for h in range(2):
                m = 2 * mp + h
                for k in range(8):
                    mm(C, pt[:, h * n:(h + 1) * n], pb, W[:, k, 2048 + m * 128:2048 + (m + 1) * 128], [Wb[k]],
                       H[:, k, 0:n], [Hb[k]], k == 0, k == 7)
            kb.op('act', lambda e, pt=pt, mp=mp, ot=ot: e.activation(
                out=ot[:, 2 * mp:2 * mp + 2, 0:n], in_=pt.rearrange("p (a t) -> p a t", a=2), func=AF.Sigmoid),
                reads=pb, writes=[otb])
        kb.dma('sp', ov[:, :, col0:col0 + n], ot[:, :, 0:n], reads=[otb], writes=xbufs(T, 'moT', col0, n))
        for s in range(n // 128):
            t0 = col0 + s * 128
            vt, vtb = Vt[vi % 3], Vtb[vi % 3]
            for nq in range(2):
                pt, pb = C.psum(512)
                for k in range(8):
                    mm(C, pt, pb, H[:, k, s * 128:(s + 1) * 128], [Hb[k]],
                       W[:, k, 1024 + nq * 512:1024 + (nq + 1) * 512], [Wb[k]], k == 0, k == 7)
                kb.op('act', lambda e, pt=pt, vt=vt, nq=nq: e.activation(
                    out=vt[:, nq * 4:(nq + 1) * 4, 0:128], in_=pt.rearrange("p (h d) -> p h d", h=4),
                    func=AF.Identity), reads=pb, writes=[vtb])
            kb.dma('sp', T['mvD'][t0:t0 + 128, :], vt.rearrange("p h d -> p (h d)"), reads=[vtb],
                   writes=xbufs(T, 'mvD', t0, 128))
            kt, ktb = Kt[vi % 2], Ktb[vi % 2]
            pt, pb = C.psum(256)
            bank = int(pb[0].name[2:])
            ptb16 = psT_all[:, bank * 1024:bank * 1024 + 512]
            for c in range(4):
                kb.op('pe', lambda e, c=c, ptb16=ptb16, qk=qk, s=s: e.transpose(
                    ptb16[:, c * 128:(c + 1) * 128], qk[:, 4 + c, s * 128:(s + 1) * 128], ident),
                    reads=[qkb, P['ident_b']], writes=pb)
            kb.op('act', lambda e, kt=kt, ptb16=ptb16: e.activation(out=kt, in_=ptb16, func=AF.Identity), reads=pb,
                  writes=[ktb])
            kb.dma('sp', T['mkD'][t0:t0 + 128, :], kt, reads=[ktb], writes=xbufs(T, 'mkD', t0, 128))
            gt, gtb = Gt[vi % 2], Gtb[vi % 2]
            pt, pb = C.psum(32)
            for k in range(8):
                mm(C, pt, pb, H[:, k, s * 128:(s + 1) * 128], [Hb[k]], Wg[:, k, :], [Wgb[k]], k == 0, False)
            mm(C, pt, pb, P['one_row'][0:1, 0:128], [P['one_row_b']], bg[0:1, :], [bgb], False, True)
            ge, geb = GE.get()
            p4 = pt.rearrange("p (a b h) -> p a b h", a=2, b=2)
            g4 = gt.rearrange("p (a b h) -> p a b h", a=2, b=2)
            e4 = ge.rearrange("p (a b h) -> p a b h", a=2, b=2)
            kb.op('act', lambda e, p4=p4, e4=e4: e.activation(out=e4[:, :, 1, :], in_=p4[:, :, 1, :], func=AF.Exp,
                                                             scale=-1.0), reads=pb, writes=[geb])
            kb.op('act', lambda e, e4=e4: e.activation(out=e4[:, :, 1, :], in_=e4[:, :, 1, :], func=AF.Ln, bias=1.0),
                  reads=[geb], writes=[geb], strict=True)
            kb.op('dve', lambda e, g4=g4, e4=e4: e.tensor_scalar(out=g4[:, :, 1, :], in0=e4[:, :, 1, :], scalar1=-1.0,
                                                                scalar2=None, op0=ALU.mult), reads=[geb], writes=[gtb])
            kb.op('dve', lambda e, g4=g4, p4=p4: e.tensor_copy(out=g4[:, :, 0, :], in_=p4[:, :, 0, :]), reads=pb,
                  writes=[gtb])
            kb.dma('sp', T['mgD'][t0:t0 + 128, :], gt, reads=[gtb], writes=xbufs(T, 'mgD', t0, 128))
            vi += 1


def phase_ml_b(C, P, T):
    kb = C.kb
    C.new_phase()
    NEG = -30000.0
    cA = [C.tile([128], F32) for _ in range(2)]
    cB = [C.tile([128], F32) for _ in range(2)]
    cM = [C.tile([128], F32) for _ in range(2)]
    cI = C.tile([128], F32)
    cO = C.tile([128], F32)
    onesb = C.tile([128], BF16)
    cb = Buf()
    for t_, val in ((cA[0], 1.0), (cA[1], 1.0), (cB[0], 1.0), (cB[1], 1.0), (cM[0], 0.0), (cM[1], 0.0), (cI, 0.0),
                    (cO, 1.0)):
        kb.op('pool', lambda e, t_=t_, val=val: e.memset(t_, val), writes=[cb])
    kb.op('pool', lambda e: e.memset(onesb, 1.0), writes=[cb])

    def asel(t_, cmp, fill, base=0, cm=1, pat=-1):
        kb.op('pool', lambda e: e.affine_select(out=t_, in_=t_, pattern=[[pat, 128]], compare_op=cmp, fill=fill,
                                                base=base, channel_multiplier=cm), reads=[cb], writes=[cb],
              strict=True)
    asel(cA[0], ALU.is_gt, 0.0, base=0, cm=1, pat=-1)
    asel(cA[1], ALU.is_gt, 0.0, base=0, cm=-1, pat=1)
    asel(cB[0], ALU.is_gt, 0.0, base=1, cm=-1, pat=1)
    asel(cB[1], ALU.is_gt, 0.0, base=1, cm=1, pat=-1)
    asel(cM[0], ALU.is_gt, NEG, base=1, cm=-1, pat=1)
    asel(cM[1], ALU.is_gt, NEG, base=1, cm=1, pat=-1)
    asel(cI, ALU.not_equal, 1.0)
    NB = 2

    def alloc_dir():
        St = C.tile([8, 129], F32)
        Cbf = C.tile([8, 128], BF16)
        Nbc = C.tile([8, 128], BF16)
        Stb, Cbfb, Nbcb = Buf(), Buf(), Buf()
        NB = 2
        Q = [C.tile([8, 128], BF16) for _ in range(NB)]
        K_ = [C.tile([8, 128], BF16) for _ in range(NB)]
        KT = [C.tile([8, 64], BF16) for _ in range(NB)]
        V = [C.tile([8, 129], BF16) for _ in range(NB)]
        GA = [C.tile([32], F32) for _ in range(NB)]
        Qb, Kb_, KTb, Vb, GAb = bufs(NB), bufs(NB), bufs(NB), bufs(NB), bufs(NB)
        R = C.tile([8, 128], F32)
        R2 = C.tile([8, 128], F32)
        Rb, R2b = Buf(), Buf()
        E = C.tile([8, 128], F32)
        EB = C.tile([8, 128], F32)
        Eb, EBb = Buf(), Buf()
        AT = C.tile([8, 128], BF16)
        ATb = Buf()
        KW = C.tile([8, 64], BF16)
        KWb = Buf()
        SM = Small(C, 2, [32], F32)
        NUM = C.tile([8, 128], F32)
        DEN = C.tile([8, 128], F32)
        NUMb, DENb = Buf(), Buf()
        HO = [C.tile([8, 128], F32) for _ in range(2)]
        HOb = bufs(2)
        return dict(St=St, Cbf=Cbf, Nbc=Nbc, Stb=Stb, Cbfb=Cbfb, Nbcb=Nbcb, Q=Q, K_=K_, KT=KT, V=V, GA=GA, Qb=Qb, Kb_=Kb_, KTb=KTb, Vb=Vb, GAb=GAb, R=R, R2=R2, Rb=Rb, R2b=R2b, E=E, EB=EB, Eb=Eb, EBb=EBb, AT=AT, ATb=ATb, KW=KW, KWb=KWb, SM=SM, NUM=NUM, DEN=DEN, NUMb=NUMb, DENb=DENb, HO=HO, HOb=HOb, it=0, hidx=0)
    SD = [alloc_dir(), alloc_dir()]
    mq = T['mqk']
    ntl = 64 if MAXG is None else MAXG
    seqs = [[64, 65] + list(range(ntl)), [65, 64] + list(range(ntl - 1, -1, -1))]
    for dr in range(2):
        sd = SD[dr]
        for t_ in (sd['St'], sd['Cbf'], sd['Nbc']):
            kb.op('pool', lambda e, t_=t_: e.memset(t_, 0.0), reads=[], writes=[sd['Stb'], sd['Cbfb'], sd['Nbcb']])

    def tile_step(dr, ti):
        sd = SD[dr]
        St = sd['St']
        Cbf = sd['Cbf']
        Nbc = sd['Nbc']
        Stb = sd['Stb']
        Cbfb = sd['Cbfb']
        Nbcb = sd['Nbcb']
        Q = sd['Q']
        K_ = sd['K_']
        KT = sd['KT']
        V = sd['V']
        GA = sd['GA']
        Qb = sd['Qb']
        Kb_ = sd['Kb_']
        KTb = sd['KTb']
        Vb = sd['Vb']
        GAb = sd['GAb']
        R = sd['R']
        R2 = sd['R2']
        Rb = sd['Rb']
        R2b = sd['R2b']
        E = sd['E']
        EB = sd['EB']
        Eb = sd['Eb']
        EBb = sd['EBb']
        AT = sd['AT']
        ATb = sd['ATb']
        KW = sd['KW']
        KWb = sd['KWb']
        SM = sd['SM']
        NUM = sd['NUM']
        DEN = sd['DEN']
        NUMb = sd['NUMb']
        DENb = sd['DENb']
        HO = sd['HO']
        HOb = sd['HOb']
        hD = T['mhf'] if dr == 0 else T['mhb']
        hkey = 'mhf' if dr == 0 else 'mhb'
        t0 = ti * 128
        b = sd['it'] % NB
        sd['it'] += 1
        q, k_, kt, v, ga = Q[b], K_[b], KT[b], V[b], GA[b]
        kb.dma('sp', q[0:64], mq[0:512, t0:t0 + 128].rearrange("(h d) t -> d h t", d=64),
               reads=xbufs(T, 'mqk', t0, 128), writes=[Qb[b]])
        kb.dma('sp', k_[0:64], mq[512:1024, t0:t0 + 128].rearrange("(h d) t -> d h t", d=64),
               reads=xbufs(T, 'mqk', t0, 128), writes=[Kb_[b]])
        kb.dma('sp', kt, T['mkD'][t0:t0 + 128, :].rearrange("p (h d) -> p h d", h=8),
               reads=xbufs(T, 'mkD', t0, 128), writes=[KTb[b]])
        kb.dma('sp', v, T['mvD'][t0:t0 + 128, :].rearrange("p (h d) -> p h d", h=8),
               reads=xbufs(T, 'mvD', t0, 128), writes=[Vb[b]])
        kb.dma('sp', ga, T['mgD'][t0:t0 + 128, :], reads=xbufs(T, 'mgD', t0, 128), writes=[GAb[b]])
        ig = ga[:, dr * 16:dr * 16 + 8]
        lf = ga[:, dr * 16 + 8:dr * 16 + 16]
        kb.op('dve', lambda e, lf=lf, dr=dr: e.tensor_tensor(out=R, in0=cB[dr].unsqueeze(1).broadcast_to([128, 8, 128]),
                                                      in1=lf.unsqueeze(2).broadcast_to([128, 8, 128]),
                                                      op=ALU.mult), reads=[cb, GAb[b]], writes=[Rb])
        kb.op('pool', lambda e, ig=ig, dr=dr: e.tensor_tensor(out=R2, in0=cM[dr].unsqueeze(1).broadcast_to([128, 8, 128]),
                                                       in1=ig.unsqueeze(2).broadcast_to([128, 8, 128]),
                                                       op=ALU.add), reads=[cb, GAb[b]], writes=[R2b])
        psm, psmb = C.psum(16)
        mm(C, psm[:, 0:8], psmb, cA[dr], [cb], lf, [GAb[b]], True, True)
        mm(C, psm[:, 8:16], psmb, cO, [cb], lf, [GAb[b]], True, True)
        sm, smb = SM.get()
        kb.op('dve', lambda e, sm=sm, psm=psm, ig=ig: e.tensor_tensor(out=sm[:, 0:8], in0=psm[:, 0:8], in1=ig,
                                                                     op=ALU.add), reads=psmb + [GAb[b]],
              writes=[smb])
        kb.op('act', lambda e, sm=sm: e.activation(out=sm[:, 0:8], in_=sm[:, 0:8], func=AF.Exp), reads=[smb],
              writes=[smb])
        kb.op('act', lambda e, sm=sm, psm=psm: e.activation(out=sm[:, 8:16], in_=psm[:, 8:16], func=AF.Exp),
              reads=psmb, writes=[smb], strict=True)
        pE, pB, pS = [], [], []
        for hf in range(2):
            pe_, peb = C.psum(512)
            Rv = R[:, hf * 4:(hf + 1) * 4, :].rearrange("p h t -> p (h t)")
            R2v = R2[:, hf * 4:(hf + 1) * 4, :].rearrange("p h t -> p (h t)")
            mm(C, pe_, peb, cA[dr], [cb], Rv, [Rb], True, False)
            mm(C, pe_, peb, cI, [cb], R2v, [R2b], False, True)
            kb.op('act', lambda e, pe_=pe_, hf=hf: e.activation(
                out=E[:, hf * 4:(hf + 1) * 4, :].rearrange("p h t -> p (h t)"), in_=pe_, func=AF.Exp),
                reads=peb, writes=[Eb])
            pb_, pbb = C.psum(512)
            mm(C, pb_, pbb, cO, [cb], Rv, [Rb], True, True)
            kb.op('act', lambda e, pb_=pb_, hf=hf: e.activation(
                out=EB[:, hf * 4:(hf + 1) * 4, :].rearrange("p h t -> p (h t)"), in_=pb_, func=AF.Exp),
                reads=pbb, writes=[EBb])
        for hf in range(2):
            ps_, psb_ = C.psum(512)
            for hh in range(4):
                h = hf * 4 + hh
                mm(C, ps_[:, hh * 128:(hh + 1) * 128], psb_, k_[0:64, h, :], [Kb_[b]], q[0:64, h, :], [Qb[b]],
                   True, True)
            kb.op('dve', lambda e, ps_=ps_, hf=hf: e.tensor_tensor(
                out=AT[:, hf * 4:(hf + 1) * 4, :].rearrange("p h t -> p (h t)"), in0=ps_,
                in1=E[:, hf * 4:(hf + 1) * 4, :].rearrange("p h t -> p (h t)"), op=ALU.mult),
                reads=psb_ + [Eb], writes=[ATb])
        ho, hob = HO[sd['hidx'] % 2], HOb[sd['hidx'] % 2]
        sd['hidx'] += 1
        for hf in range(2):
            hs = slice(hf * 4, (hf + 1) * 4)
            pn, pnb = C.psum(512)
            for hh in range(4):
                h = hf * 4 + hh
                mm(C, pn[:, hh * 128:(hh + 1) * 128], pnb, v[:, h, 0:128], [Vb[b]], AT[:, h, :], [ATb], True, True)
            pd_, pdb = C.psum(512)
            mm(C, pd_, pdb, onesb, [cb], AT[:, hs, :].rearrange("p h t -> p (h t)"), [ATb], True, True)
            pi, pib = C.psum(512)
            for hh in range(4):
                h = hf * 4 + hh
                mm(C, pi[:, hh * 128:(hh + 1) * 128], pib, Cbf[0:64, h, :], [Cbfb], q[0:64, h, :], [Qb[b]],
                   True, True)
            pq, pqb = C.psum(512)
            for hh in range(4):
                h = hf * 4 + hh
                mm(C, pq[:, hh * 128:(hh + 1) * 128], pqb, Nbc[0:64, h, :], [Nbcb], q[0:64, h, :], [Qb[b]],
                   True, True)
            ebv = EB[:, hs, :].rearrange("p h t -> p (h t)")
            numv = NUM[:, hs, :].rearrange("p h t -> p (h t)")
            denv = DEN[:, hs, :].rearrange("p h t -> p (h t)")
            hov = ho[:, hs, :].rearrange("p h t -> p (h t)")
            kb.op('dve', lambda e, pi=pi, ebv=ebv, numv=numv: e.tensor_tensor(out=numv, in0=pi, in1=ebv,
                                                                             op=ALU.mult),
                  reads=pib + [EBb], writes=[NUMb])
            kb.op('dve', lambda e, pn=pn, numv=numv: e.tensor_tensor(out=numv, in0=pn, in1=numv, op=ALU.add),
                  reads=pnb + [NUMb], writes=[NUMb])
            kb.op('dve', lambda e, pq=pq, ebv=ebv, denv=denv: e.tensor_tensor(out=denv, in0=pq, in1=ebv,
                                                                             op=ALU.mult),
                  reads=pqb + [EBb], writes=[DENb])
            kb.op('dve', lambda e, pd_=pd_, denv=denv: e.tensor_tensor(out=denv, in0=pd_, in1=denv, op=ALU.add),
                  reads=pdb + [DENb], writes=[DENb])
            kb.op('act', lambda e, denv=denv: e.activation(out=denv, in_=denv, func=AF.Abs), reads=[DENb],
                  writes=[DENb])
            kb.op('dve', lambda e, denv=denv: e.tensor_scalar(out=denv, in0=denv, scalar1=1.0, scalar2=None,
                                                              op0=ALU.max), reads=[DENb], writes=[DENb])
            kb.op('dve', lambda e, denv=denv: e.reciprocal(out=denv, in_=denv), reads=[DENb], writes=[DENb])
            kb.op('pool', lambda e, denv=denv, numv=numv, hov=hov: e.tensor_tensor(out=hov, in0=numv, in1=denv,
                                                                                  op=ALU.mult),
                  reads=[DENb, NUMb], writes=[hob])
        kb.dma('sp', hD[:, t0:t0 + 128].rearrange("(h p) t -> p h t", p=128), ho, reads=[hob],
               writes=xbufs(T, hkey, t0, 128))
        sm_w = sm[:, 0:8]
        kb.op('dve', lambda e, sm_w=sm_w, kt=kt: e.tensor_tensor(
            out=KW, in0=kt, in1=sm_w.unsqueeze(2).broadcast_to([128, 8, 64]), op=ALU.mult),
            reads=[KTb[b], smb], writes=[KWb])
        for (h0, nh) in ((0, 3), (3, 3), (6, 2)):
            pu, pub = C.psum(nh * 129)
            for hh in range(nh):
                h = h0 + hh
                mm(C, pu[0:64, hh * 129:(hh + 1) * 129], pub, KW[:, h, :], [KWb], v[:, h, :], [Vb[b]], True, True)
            stv = St[0:64, h0:h0 + nh, :]
            kb.op('dve', lambda e, stv=stv, sm=sm, h0=h0, nh=nh: e.tensor_tensor(
                out=stv, in0=stv, in1=sm[0:64, 8 + h0:8 + h0 + nh].unsqueeze(2).broadcast_to([64, nh, 129]),
                op=ALU.mult), reads=[Stb, smb], writes=[Stb], strict=True)
            kb.op('dve', lambda e, stv=stv, pu=pu, nh=nh: e.tensor_tensor(
                out=stv, in0=pu[0:64, :].rearrange("p (h d) -> p h d", h=nh), in1=stv, op=ALU.add),
                reads=pub + [Stb], writes=[Stb], strict=True)
        kb.op('act', lambda e: e.activation(out=Cbf[0:64], in_=St[0:64, :, 0:128], func=AF.Identity),
              reads=[Stb], writes=[Cbfb])
        kb.op('pool', lambda e: e.tensor_copy(out=Nbc[0:64], in_=St[0:64, :, 128:129].broadcast_to([64, 8, 128])),
              reads=[Stb], writes=[Nbcb])

    for step in range(len(seqs[0])):
        for dr in range(2):
            tile_step(dr, seqs[dr][step])


def phase_ml_c(C, P, T, l, src, dst):
    kb = C.kb
    C.new_phase()
    G = 256
    Wo = C.tile([8, 1024], BF16)
    Wob = bufs(8)
    load_weight(C, Wo, Wob, T['ml_w_out'][0], 8)
    ng = C.tile([8], F32)
    ngb = Buf()
    kb.dma('sp', ng, T['ml_norm_gT'][:, :], writes=[ngb])
    ones128 = C.tile([128], BF16)
    o1b = Buf()
    kb.op('pool', lambda e: e.memset(ones128, 1.0 / 128), writes=[o1b])
    HF = [C.tile([8, G], F32) for _ in range(2)]
    HB = [C.tile([8, G], F32) for _ in range(2)]
    OG = [C.tile([8, G], BF16) for _ in range(2)]
    HFb, HBb, OGb = bufs(2), bufs(2), bufs(2)
    Hm = C.tile([8, G], BF16)
    Hmb = bufs(8)
    Yf = C.tile([8, G], F32)
    Yfb = bufs(8)
    SQ = Small(C, 3, [2 * G], BF16)
    RS = Small(C, 3, [G], F32)
    TMP = Small(C, 4, [G], F32)
    XR = Small(C, 4, [G], F32)
    grp = groups(G, True)
    hfv = T['mhf'].rearrange("(c p) t -> p c t", p=128)
    hbv = T['mhb'].rearrange("(c p) t -> p c t", p=128)
    ov = T['moT'].rearrange("(c p) t -> p c t", p=128)

    def load(i):
        col0, n, w = grp[i]
        kb.dma('sp', HF[i % 2][:, :, 0:n], hfv[:, :, col0:col0 + n], reads=xbufs(T, 'mhf', col0, n),
               writes=[HFb[i % 2]])
        kb.dma('sp', HB[i % 2][:, :, 0:n], hbv[:, :, col0:col0 + n], reads=xbufs(T, 'mhb', col0, n),
               writes=[HBb[i % 2]])
        kb.dma('sp', OG[i % 2][:, :, 0:n], ov[:, :, col0:col0 + n], reads=xbufs(T, 'moT', col0, n),
               writes=[OGb[i % 2]])
    load(0)
    for i, (col0, n, w) in enumerate(grp):
        if i + 1 < len(grp):
            load(i + 1)
        hf, hb, og = HF[i % 2], HB[i % 2], OG[i % 2]
        kb.op('pool', lambda e, hf=hf, hb=hb: e.tensor_tensor(out=hf[:, :, 0:n], in0=hf[:, :, 0:n], in1=hb[:, :, 0:n],
                                                             op=ALU.add), reads=[HFb[i % 2], HBb[i % 2]],
              writes=[HFb[i % 2]])
        for c in range(8):
            s_, sb_ = SQ.get()
            kb.op('act', lambda e, s_=s_, c=c, hf=hf: e.activation(out=s_[:, 0:n], in_=hf[:, c, 0:n], func=AF.Square),
                  reads=[HFb[i % 2]], writes=[sb_])
            pst, pb = C.psum(n)
            mm(C, pst, pb, ones128, [o1b], s_[:, 0:n], [sb_], True, True)
            rs, rb = rstd_from_psum(C, P, pst, pb, RS)
            t, tb = TMP.get()
            kb.op('dve', lambda e, t=t, c=c, hf=hf, rs=rs: e.scalar_tensor_tensor(
                out=t[:, 0:n], in0=hf[:, c, 0:n], scalar=ng[:, c:c + 1], in1=rs, op0=ALU.mult, op1=ALU.mult),
                reads=[HFb[i % 2], rb, ngb], writes=[tb])
            kb.op('pool', lambda e, t=t, c=c, og=og: e.tensor_tensor(out=Hm[:, c, 0:n], in0=t[:, 0:n],
                                                                    in1=og[:, c, 0:n], op=ALU.mult),
                  reads=[tb, OGb[i % 2]], writes=[Hmb[c]])
        pr = PostRes(C, P, n, Yf, Yfb, SQ, RS, TMP, XR)
        for mp in range(4):
            pt, pb = C.psum(2 * n)
            for h in range(2):
                m = 2 * mp + h
                for k in range(8):
                    mm(C, pt[:, h * n:(h + 1) * n], pb, Wo[:, k, m * 128:(m + 1) * 128], [Wob[k]], Hm[:, k, 0:n],
                       [Hmb[k]], k == 0, k == 7)
            pr.pair(2 * mp, pt, pb)
        pr.finish(l, 2, w, T[src], T[dst], col0, xbufs(T, src, col0, n), xbufs(T, dst, col0, n))


def build_program(plan, dbg=False):
    nc = bass.Bass("TRN2", target_bir_lowering=False)
    es = ExitStack()
    T = {}

    def dram(name, shape, dt, kind):
        T[name] = nc.dram_tensor(name, shape, dt, kind=kind).ap()
        return T[name]

    dram('xin', [D, NT], F32, "ExternalInput")
    dram('cvec', [128, 8, 2], F32, "ExternalInput")
    dram('ada_w', [4, D, 6 * D], F32, "ExternalInput")
    dram('ada_bT', [128, 4, 48], F32, "ExternalInput")
    dram('norm_gT', [128, 4, 4, 8], F32, "ExternalInput")
    dram('ffn_w1', [4, D, 4 * D], F32, "ExternalInput")
    dram('ffn_w2', [4, 4 * D, D], F32, "ExternalInput")
    dram('gm_w_in', [2, D, 6 * D], F32, "ExternalInput")
    dram('gm_b_in', [2, 6 * D], F32, "ExternalInput")
    dram('gm_ln_gT', [2, 128, 24], F32, "ExternalInput")
    dram('gm_wsT', [2, 128, 8 * 128], F32, "ExternalInput")
    dram('gm_bs_bc', [2, 128, 8 * 128], F32, "ExternalInput")
    dram('gm_w_out', [2, 3 * D, D], F32, "ExternalInput")
    dram('na_w_qkv', [1, D, 3 * D], F32, "ExternalInput")
    dram('na_b_qkv', [1, 3 * D], F32, "ExternalInput")
    dram('na_bqkvT', [128, 24], F32, "ExternalInput")
    dram('na_bt', [128, 16 * 16 * 64], F32, "ExternalInput")
    dram('na_w_o', [1, D, D], F32, "ExternalInput")
    dram('na_b_oT', [128, 8], F32, "ExternalInput")
    dram('qT', [D, NT], BF16, "Internal")
    dram('kT', [D, NT], BF16, "Internal")
    dram('vD', [NT, 1040], BF16, "Internal")
    dram('ml_w_in', [1, D, 3 * D], F32, "ExternalInput")
    dram('ml_w_qk_sw', [D, D], F32, "ExternalInput")
    dram('ml_w_gate2', [D, 32], F32, "ExternalInput")
    dram('ml_b_gate2', [1, 32], F32, "ExternalInput")
    dram('ml_rope', [4, 128, NT], F32, "ExternalInput")
    dram('ml_norm_gT', [128, 8], F32, "ExternalInput")
    dram('ml_w_out', [1, D, D], F32, "ExternalInput")
    dram('mqk', [D, NT], BF16, "Internal")
    dram('moT', [D, NT], BF16, "Internal")
    dram('mvD', [NT, 8 * 129], BF16, "Internal")
    dram('mkD', [NT, 512], BF16, "Internal")
    dram('mgD', [NT, 32], F32, "Internal")
    dram('mhf', [D, NT], F32, "ExternalOutput" if dbg else "Internal")
    dram('mhb', [D, NT], F32, "ExternalOutput" if dbg else "Internal")
    dram('yout', [D, NL], F32, "ExternalOutput")
    dram('xs', [D, NT], F32, "ExternalOutput" if dbg else "Internal")
    dram('pd', [3 * D, NT], BF16, "ExternalOutput" if dbg else "Internal")
    if dbg:
        dram('dbg_vh', [128, 3072], BF16, "ExternalOutput")
        dram('dbg_mv', [128, 8], F32, "ExternalOutput")
        dram('dbg_zg', [128, 3072], F32, "ExternalOutput")
    for k in ['xin', 'xs', 'pd', 'yout', 'qT', 'kT', 'vD', 'mqk', 'moT', 'mvD', 'mkD', 'mgD', 'mhf', 'mhb']:
        T[k + '_b'] = bufs(NT // 128, k)
    C = Ctx(nc, es)
    P = phase_consts(C, T)
    for ph in plan:
        kind = ph[0]
        if kind == 'ffn':
            _, l, src, dst, with_ctx, final = ph
            phase_ffn(C, P, T, l, src, dst, with_ctx, final)
        elif kind == 'gmlp':
            _, l, j, src, dst, with_ctx = ph
            phase_gmlp_a(C, P, T, l, j, src, with_ctx)
            phase_gmlp_b(C, P, T, l, j, src, dst, with_ctx)
        elif kind == 'na':
            _, l, src, dst = ph
            if NA_PARTS & 1:
                phase_na_a(C, P, T, l, src)
            if NA_PARTS & 2:
                phase_na_b(C, P, T, l, src, dst)
        elif kind == 'ml':
            _, l, src, dst = ph
            phase_ml_a(C, P, T, l, src)
            phase_ml_b(C, P, T)
            phase_ml_c(C, P, T, l, src, dst)
        else:
            raise ValueError(kind)
    C.kb.barrier()
    C.kb.emit(es)
    es.close()
    return nc, C


def host_shared(inp):
    f = np.float32
    m = {}
    m['ada_w'] = inp['ada_w']
    m['ada_bT'] = np.ascontiguousarray(inp['ada_b'].reshape(4, 48, 128).transpose(2, 0, 1), dtype=f)
    m['norm_gT'] = np.ascontiguousarray(inp['norm_g'].reshape(4, 4, 8, 128).transpose(3, 0, 1, 2), dtype=f)
    m['ffn_w1'] = inp['ffn_w1']
    m['ffn_w2'] = inp['ffn_w2']
    m['gm_w_in'] = inp['gm_w_in']
    m['gm_b_in'] = inp['gm_b_in']
    m['gm_w_out'] = inp['gm_w_out']
    m['gm_ln_gT'] = np.ascontiguousarray(inp['gm_ln_g'].reshape(2, 24, 128).transpose(0, 2, 1), dtype=f)
    m['gm_wsT'] = np.ascontiguousarray(inp['gm_ws'].transpose(0, 3, 1, 2).reshape(2, 128, 1024), dtype=f)
    m['na_w_qkv'] = inp['na_w_qkv']
    m['na_b_qkv'] = inp['na_b_qkv']
    m['na_bqkvT'] = np.ascontiguousarray(inp['na_b_qkv'][0].reshape(24, 128).T, dtype=f)
    m['na_bt'] = na_bias_table(inp['na_rpb'][0])
    m['na_w_o'] = inp['na_w_o']
    m['na_b_oT'] = np.ascontiguousarray(inp['na_b_o'][0].reshape(8, 128).T, dtype=f)
    tabs, perm = ml_rope_tables()
    m['ml_w_in'] = inp['ml_w_in']
    m['ml_w_qk_sw'] = np.ascontiguousarray(inp['ml_w_in'][0][:, :1024][:, perm], dtype=f)
    wg = inp['ml_w_gate'][0]
    m['ml_w_gate2'] = np.ascontiguousarray(np.concatenate([wg[0], wg[1]], axis=1), dtype=f)
    m['ml_b_gate2'] = np.ascontiguousarray(inp['ml_b_gate'][0].reshape(1, 32), dtype=f)
    m['ml_rope'] = tabs
    m['ml_norm_gT'] = np.ascontiguousarray(inp['ml_norm_g'][0].reshape(8, 128).T, dtype=f)
    m['ml_w_out'] = inp['ml_w_out']
    m['gm_bs_bc'] = np.ascontiguousarray(np.broadcast_to(inp['gm_bs'].reshape(2, 1, 1024), (2, 128, 1024)), dtype=f)
    return m


def host_inputs(inp, b, shared=None):
    f = np.float32
    m = dict(shared if shared is not None else host_shared(inp))
    m['xin'] = np.ascontiguousarray(np.concatenate([inp['x'][b].T, inp['ctx'][b].T], axis=1), dtype=f)
    cv = np.stack([inp['c'][b], inp['c_ctx']], axis=-1)
    m['cvec'] = np.ascontiguousarray(cv.reshape(8, 128, 2).transpose(1, 0, 2), dtype=f)
    return m


FULL_PLAN = [
    ('gmlp', 0, 0, 'xin', 'xs', True), ('ffn', 0, 'xs', 'xs', True, False),
    ('na', 1, 'xs', 'xs'), ('ffn', 1, 'xs', 'xs', True, False),
    ('ml', 2, 'xs', 'xs'), ('ffn', 2, 'xs', 'xs', True, False),
    ('gmlp', 3, 1, 'xs', 'xs', False), ('ffn', 3, 'xs', 'xs', False, True),
]


def kernel(**inputs):
    inp = {k: np.asarray(v) for k, v in inputs.items()}
    nc, C = build_program(FULL_PLAN)
    shared = host_shared(inp)
    in_maps = [host_inputs(inp, b, shared) for b in range(8)]
    res = run_bass_kernel_spmd(nc, in_maps, core_ids=list(range(8)))
    out = np.stack([np.ascontiguousarray(np.asarray(res.results[b]['yout']).T) for b in range(8)], axis=0)
    return out.astype(np.float32)
```

```python
import math
import numpy as np
import concourse.bass as bass
import concourse.mybir as mybir
from concourse.bass_utils import run_bass_kernel_spmd
from contextlib import ExitStack

F32 = mybir.dt.float32
BF16 = mybir.dt.bfloat16
U8 = mybir.dt.uint8
AF = mybir.ActivationFunctionType
ALU = mybir.AluOpType

ENGS = ['pe', 'act', 'dve', 'pool', 'sp']
NSLOT = 12
SAME_ENGINE_SYNC = False

D = 1024
KC = 8
NL = 8192
NCX = 256
NT = NL + NCX
EPS = 1e-6
ARENA_BYTES = 204 * 1024


class Buf:
    __slots__ = ('name', 'lw', 'rd')

    def __init__(self, name=''):
        self.name = name
        self.lw = None
        self.rd = {}


def bufs(n, name=''):
    return [Buf(name + str(i)) for i in range(n)]


class KB:
    def __init__(self, nc):
        self.nc = nc
        self.streams = {e: [] for e in ENGS}
        self.seen = {e: {} for e in ENGS}
        self.dseen = {e: set() for e in ENGS}
        self.slot_rr = {e: 0 for e in ENGS}
        self.slot_cnt = {e: [0] * NSLOT for e in ENGS}
        self.slot_last = {e: [None] * NSLOT for e in ENGS}
        self.n_ops = 0

    def _collect(self, eng, reads, writes, strict=False):
        deps_e = {}
        deps_d = set()

        def addtok(t):
            if t is None:
                return
            if t[0] == 'e':
                if t[2] > deps_e.get(t[1], -1):
                    deps_e[t[1]] = t[2]
            else:
                deps_d.add(t)

        for b in reads:
            addtok(b.lw)
        for b in writes:
            addtok(b.lw)
            for k, v in b.rd.items():
                if isinstance(k, tuple):
                    deps_d.add(k)
                else:
                    addtok(('e', k, v))
        waits = []
        for E, idx in deps_e.items():
            if E == eng and (eng == 'pe' or not (SAME_ENGINE_SYNC or strict)):
                continue
            if self.seen[eng].get(E, -1) >= idx:
                continue
            self.seen[eng][E] = idx
            self.streams[E][idx]['inc'] = True
            waits.append(('e', E, idx))
        for t in deps_d:
            if t in self.dseen[eng]:
                continue
            self.dseen[eng].add(t)
            waits.append(t)
        return waits

    def _commit(self, tok, reads, writes):
        for b in writes:
            b.lw = tok
            b.rd = {}
        for b in reads:
            if b in writes:
                continue
            if tok[0] == 'e':
                if b.rd.get(tok[1], -1) < tok[2]:
                    b.rd[tok[1]] = tok[2]
            else:
                b.rd[tok] = 1

    def op(self, eng, fn, reads=(), writes=(), strict=False):
        waits = self._collect(eng, reads, writes, strict)
        idx = len(self.streams[eng])
        self.streams[eng].append(dict(waits=waits, fn=fn, inc=False, dma=None))
        self._commit(('e', eng, idx), reads, writes)
        self.n_ops += 1

    def dma(self, q, out, in_, reads=(), writes=()):
        waits = self._collect(q, reads, writes)
        slot = self.slot_rr[q]
        self.slot_rr[q] = (slot + 1) % NSLOT
        prev = self.slot_last[q][slot]
        if prev is not None and prev not in self.dseen[q]:
            self.dseen[q].add(prev)
            waits.append(prev)
        self.slot_cnt[q][slot] += 1
        tok = ('d', q, slot, 16 * self.slot_cnt[q][slot])
        self.slot_last[q][slot] = tok
        self.streams[q].append(dict(waits=waits, fn=lambda e, o=out, i=in_: e.dma_start(out=o, in_=i),
                                    inc=False, dma=(q, slot)))
        self._commit(tok, reads, writes)
        self.n_ops += 1

    def barrier(self):
        lasts = {}
        for E in ENGS:
            for idx in range(len(self.streams[E]) - 1, -1, -1):
                if self.streams[E][idx]['fn'] is not None and self.streams[E][idx]['dma'] is None:
                    lasts[E] = idx
                    break
        dtoks = [t for q in ENGS for t in self.slot_last[q] if t is not None]
        for W in ENGS:
            waits = []
            for E, idx in lasts.items():
                if E == W:
                    continue
                if self.seen[W].get(E, -1) >= idx:
                    continue
                self.seen[W][E] = idx
                self.streams[E][idx]['inc'] = True
                waits.append(('e', E, idx))
            for t in dtoks:
                if t in self.dseen[W]:
                    continue
                self.dseen[W].add(t)
                waits.append(t)
            if waits:
                self.streams[W].append(dict(waits=waits, fn=None, inc=False, dma=None))

    def _pref(self):
        pref = {}
        for e in ENGS:
            c = 0
            arr = []
            for ent in self.streams[e]:
                if ent['inc']:
                    assert ent['dma'] is None and ent['fn'] is not None
                    c += 1
                arr.append(c)
            pref[e] = arr
        return pref

    def check(self):
        pref = self._pref()
        esem = {e: 0 for e in ENGS}
        dsem = {(q, i): 0 for q in ENGS for i in range(NSLOT)}
        pc = {e: 0 for e in ENGS}
        progress = True
        while progress:
            progress = False
            for e in ENGS:
                st = self.streams[e]
                while pc[e] < len(st):
                    ent = st[pc[e]]
                    ok = True
                    for w in ent['waits']:
                        if w[0] == 'e':
                            if esem[w[1]] < pref[w[1]][w[2]]:
                                ok = False
                        elif dsem[(w[1], w[2])] < w[3]:
                            ok = False
                    if not ok:
                        break
                    if ent['dma'] is not None:
                        dsem[ent['dma']] += 16
                    elif ent['inc']:
                        esem[e] += 1
                    pc[e] += 1
                    progress = True
        for e in ENGS:
            if pc[e] < len(self.streams[e]):
                raise RuntimeError(f"DEADLOCK: {e} stuck at {pc[e]}/{len(self.streams[e])}")

    def emit(self, es):
        self.check()
        nc = self.nc
        esem = {e: es.enter_context(nc.semaphore("s_" + e)) for e in ENGS}
        dsem = {q: [es.enter_context(nc.semaphore(f"d_{q}{i}")) for i in range(NSLOT)]
                for q in ENGS if any(self.slot_cnt[q])}
        pref = self._pref()
        block = es.enter_context(nc.Block())
        handles = {'pe': block.tensor, 'act': block.scalar, 'dve': block.vector, 'pool': block.gpsimd,
                   'sp': block.sync}
        for e in ENGS:
            stream = self.streams[e]
            if not stream:
                continue

            def body(h, stream=stream, e=e):
                for ent in stream:
                    for w in ent['waits']:
                        if w[0] == 'e':
                            h.wait_ge(esem[w[1]], pref[w[1]][w[2]])
                        else:
                            h.wait_ge(dsem[w[1]][w[2]], w[3])
                    if ent['fn'] is None:
                        continue
                    ins = ent['fn'](h)
                    if ent['dma'] is not None:
                        q, slot = ent['dma']
                        ins.then_inc(dsem[q][slot], 16)
                    elif ent['inc']:
                        ins.then_inc(esem[e], 1)
            handles[e](body)


class Ctx:
    def __init__(self, nc, es):
        self.nc = nc
        self.kb = KB(nc)
        self.arena = es.enter_context(nc.sbuf_tensor("arena", [128, ARENA_BYTES], U8))
        self.ps = es.enter_context(nc.psum_tensor("ps", [128, 4096], F32))
        self.off = 0
        self.persist_end = 0
        self.psb = bufs(8, 'ps')
        self.ps_rr = 0
        self.pool_rr = {}
        self.default_banks = list(range(8))
        self.tmp_rr = 0
        self.dram = {}

    def tile(self, free_shape, dt):
        n = int(np.prod(free_shape))
        esz = 4 if dt == F32 else 2
        nbytes = (n * esz + 31) // 32 * 32
        assert self.off + nbytes <= ARENA_BYTES, (self.off, nbytes)
        v = self.arena[:, self.off:self.off + n * esz].bitcast(dt)
        self.off += nbytes
        if len(free_shape) == 2:
            v = v.rearrange("p (a b) -> p a b", a=free_shape[0])
        elif len(free_shape) == 3:
            v = v.rearrange("p (a b c) -> p a b c", a=free_shape[0], b=free_shape[1])
        elif len(free_shape) == 4:
            v = v.rearrange("p (a b c d) -> p a b c d", a=free_shape[0], b=free_shape[1], c=free_shape[2])
        return v

    def end_persist(self):
        self.persist_end = self.off

    def new_phase(self):
        self.kb.barrier()
        self.off = self.persist_end
        self.psb = bufs(8, 'ps')
        self.default_banks = list(range(8))

    def psum(self, ncols, banks=None):
        assert ncols <= 512
        if banks is None:
            banks = self.default_banks
        key = tuple(banks)
        j = self.pool_rr.get(key, 0)
        self.pool_rr[key] = (j + 1) % len(banks)
        i = banks[j]
        return self.ps[:, i * 512:i * 512 + ncols], [self.psb[i]]


def mm(C, out, ob, lhsT, lb, rhs, rb, start, stop):
    C.kb.op('pe', lambda e: e.matmul(out, lhsT, rhs, start=start, stop=stop), reads=lb + rb, writes=ob)


def load_weight(C, dst, dbufs, src, kc_n, queue='pool'):
    sv = src.rearrange("(k p) n -> p k n", p=128)
    for k in range(kc_n):
        C.kb.dma(queue, dst[:, k, :], sv[:, k, :], writes=[dbufs[k]])


def phase_consts(C, T):
    kb = C.kb
    P = {}
    P['ones_bf'] = C.tile([128], BF16)
    P['ones_bf_b'] = Buf()
    P['one_row'] = C.tile([512], BF16)
    P['one_row_b'] = Buf()
    P['eps'] = C.tile([1], F32)
    P['eps_b'] = Buf()
    P['ident'] = C.tile([128], BF16)
    P['ident_b'] = Buf()
    kb.op('pool', lambda e: e.memset(P['ones_bf'], 1.0 / 1024), writes=[P['ones_bf_b']])
    kb.op('pool', lambda e: e.memset(P['one_row'], 1.0), writes=[P['one_row_b']])
    kb.op('pool', lambda e: e.memset(P['eps'], EPS), writes=[P['eps_b']])
    kb.op('pool', lambda e: e.memset(P['ident'], 0.0), writes=[P['ident_b']])
    kb.op('pool', lambda e: e.affine_select(out=P['ident'], in_=P['ident'], pattern=[[-1, 128]],
                                            compare_op=ALU.not_equal, fill=1.0, base=0, channel_multiplier=1),
          reads=[P['ident_b']], writes=[P['ident_b']])
    cv = C.tile([8, 2], F32)
    ca = C.tile([8, 2], F32)
    MOD = C.tile([4, 48, 2], F32)
    SC = C.tile([4, 6, 8, 2], F32)
    NG = C.tile([4, 4, 8], F32)
    AB = C.tile([4, 48], F32)
    P['SC'] = SC
    P['SC_b'] = Buf()
    C.end_persist()
    b_cv, b_ca, b_mod, b_ng, b_ab = Buf(), Buf(), Buf(), Buf(), Buf()
    kb.dma('sp', cv, T['cvec'][:, :, :], writes=[b_cv])
    kb.dma('sp', NG, T['norm_gT'][:, :, :, :], writes=[b_ng])
    kb.dma('sp', AB, T['ada_bT'][:, :, :], writes=[b_ab])
    kb.op('act', lambda e: e.activation(out=ca, in_=cv, func=AF.Silu), reads=[b_cv], writes=[b_ca])
    Wt = [C.tile([8, 768], F32) for _ in range(2)]
    Wb = bufs(2)
    it = 0
    for l in range(4):
        pst, pb = C.psum(96)
        for nb in range(8):
            w = Wt[it % 2]
            wb = Wb[it % 2]
            it += 1
            src = T['ada_w'][l].rearrange("(k p) n -> p k n", p=128)[:, :, nb * 768:(nb + 1) * 768]
            kb.dma('sp', w, src, writes=[wb])
            for j in range(6):
                n = nb * 6 + j
                for k in range(8):
                    mm(C, pst[:, 2 * n:2 * n + 2], pb, w[:, k, j * 128:(j + 1) * 128], [wb], ca[:, k, :], [b_ca],
                       k == 0, k == 7)
        pv = pst.rearrange("p (n w) -> p n w", w=2)
        kb.op('dve', lambda e, l=l, pv=pv: e.tensor_tensor(out=MOD[:, l], in0=pv,
                                                            in1=AB[:, l].unsqueeze(2).broadcast_to([128, 48, 2]),
                                                            op=ALU.add),
              reads=pb + [b_ab], writes=[b_mod], strict=True)

        def m(j6, l=l):
            return MOD[:, l, j6 * 8:(j6 + 1) * 8, :]

        def g(j, l=l):
            return NG[:, l, j, :].unsqueeze(2).broadcast_to([128, 8, 2])
        rd = [b_mod, b_ng]
        wr = [P['SC_b']]
        kb.op('dve', lambda e, l=l, m=m, g=g: e.scalar_tensor_tensor(out=SC[:, l, 0], in0=m(1), scalar=1.0, in1=g(0),
                                                                    op0=ALU.add, op1=ALU.mult), reads=rd, writes=wr, strict=True)
        kb.op('dve', lambda e, l=l, m=m: e.tensor_copy(out=SC[:, l, 1], in_=m(0)), reads=rd, writes=wr, strict=True)
        kb.op('dve', lambda e, l=l, m=m, g=g: e.tensor_tensor(out=SC[:, l, 2], in0=m(2), in1=g(1), op=ALU.mult),
              reads=rd, writes=wr, strict=True)
        kb.op('dve', lambda e, l=l, m=m, g=g: e.scalar_tensor_tensor(out=SC[:, l, 3], in0=m(4), scalar=1.0, in1=g(2),
                                                                    op0=ALU.add, op1=ALU.mult), reads=rd, writes=wr, strict=True)
        kb.op('dve', lambda e, l=l, m=m: e.tensor_copy(out=SC[:, l, 4], in_=m(3)), reads=rd, writes=wr, strict=True)
        kb.op('dve', lambda e, l=l, m=m, g=g: e.tensor_tensor(out=SC[:, l, 5], in0=m(5), in1=g(3), op=ALU.mult),
              reads=rd, writes=wr, strict=True)
    return P


def sc(P, l, kind, c, w):
    return P['SC'][:, l, kind, c, w:w + 1]


class Small:
    def __init__(self, C, n, free, dt):
        self.t = [C.tile(free, dt) for _ in range(n)]
        self.b = bufs(n)
        self.i = 0

    def get(self):
        i = self.i
        self.i = (i + 1) % len(self.t)
        return self.t[i], self.b[i]


def rstd_from_psum(C, P, pst, pb, RS):
    rs, rb = RS.get()
    n = pst.shape[-1]
    rs = rs[:, 0:n]
    C.kb.op('act', lambda e: e.activation(out=rs, in_=pst, func=AF.Sqrt, bias=P['eps'][:, 0:1]),
            reads=pb + [P['eps_b']], writes=[rb])
    C.kb.op('dve', lambda e: e.reciprocal(out=rs, in_=rs), reads=[rb], writes=[rb])
    return rs, rb


def prenorm(C, P, XL, XLb, n, H, Hb, l, kA, kB, w, SQ, RS, TMP):
    kb = C.kb
    pst, pb = C.psum(n)
    sq = []
    for c in range(8):
        s, sb_ = SQ.get()
        s = s[:, 0:n]
        kb.op('act', lambda e, s=s, c=c: e.activation(out=s, in_=XL[:, c, 0:n], func=AF.Square), reads=[XLb],
              writes=[sb_])
        sq.append((s, sb_))
        if c >= 1:
            s0, sb0 = sq[c - 1]
            mm(C, pst, pb, P['ones_bf'], [P['ones_bf_b']], s0, [sb0], c - 1 == 0, False)
    s0, sb0 = sq[7]
    mm(C, pst, pb, P['ones_bf'], [P['ones_bf_b']], s0, [sb0], False, True)
    rs, rb = rstd_from_psum(C, P, pst, pb, RS)
    for c in range(8):
        t, tb = TMP.get()
        t = t[:, 0:n]
        kb.op('dve', lambda e, t=t, c=c: e.scalar_tensor_tensor(out=t, in0=XL[:, c, 0:n], scalar=sc(P, l, kA, c, w),
                                                               in1=rs, op0=ALU.mult, op1=ALU.mult),
              reads=[XLb, rb, P['SC_b']], writes=[tb])
        kb.op('act', lambda e, t=t, c=c: e.activation(out=H[:, c, 0:n], in_=t, func=AF.Identity,
                                                      bias=sc(P, l, kB, c, w)),
              reads=[tb, P['SC_b']], writes=[Hb[c]])


class PostRes:
    def __init__(self, C, P, n, Yf, Yfb, SQ, RS, TMP, XR):
        self.C, self.P, self.n = C, P, n
        self.Yf, self.Yfb, self.SQ, self.RS, self.TMP, self.XR = Yf, Yfb, SQ, RS, TMP, XR
        self.pst, self.pb = C.psum(n)
        self.pending = None
        self.cnt = 0

    def _flush(self, last):
        if self.pending is not None:
            s0, sb0 = self.pending
            n = self.n
            for h in range(2):
                mm(self.C, self.pst, self.pb, self.P['ones_bf'], [self.P['ones_bf_b']], s0[:, h * n:(h + 1) * n], [sb0],
                   self.cnt == 0, last and h == 1)
                self.cnt += 1
            self.pending = None

    def pair(self, m, yp, ypb, bias=None, bias_b=None):
        C, n = self.C, self.n
        self._flush(False)
        s, sb_ = self.SQ.get()
        s = s[:, 0:2 * n]
        if bias is None:
            yv = yp.rearrange("p (a t) -> p a t", a=2)
            C.kb.op('act', lambda e: e.activation(out=self.Yf[:, m:m + 2, 0:n], in_=yv, func=AF.Identity), reads=ypb,
                    writes=[self.Yfb[m], self.Yfb[m + 1]])
            C.kb.op('act', lambda e: e.activation(out=s, in_=yp, func=AF.Square), reads=ypb, writes=[sb_])
        else:
            for h in range(2):
                C.kb.op('act', lambda e, h=h: e.activation(out=self.Yf[:, m + h, 0:n], in_=yp[:, h * n:(h + 1) * n],
                                                          func=AF.Identity, bias=bias[:, m + h:m + h + 1]),
                        reads=ypb + [bias_b], writes=[self.Yfb[m + h]])
                C.kb.op('act', lambda e, h=h: e.activation(out=s[:, h * n:(h + 1) * n], in_=yp[:, h * n:(h + 1) * n],
                                                          func=AF.Square, bias=bias[:, m + h:m + h + 1]),
                        reads=ypb + [bias_b], writes=[sb_])
        self.pending = (s, sb_)

    def finish(self, l, kG, w, src, dst, col0, srcb, dstb):
        C, P, n = self.C, self.P, self.n
        self._flush(True)
        rs, rb = rstd_from_psum(C, P, self.pst, self.pb, self.RS)
        for c in range(8):
            xr, xrb = self.XR.get()
            xr = xr[:, 0:n]
            C.kb.dma('sp', xr, src[c * 128:(c + 1) * 128, col0:col0 + n], reads=srcb, writes=[xrb])
            t, tb = self.TMP.get()
            t = t[:, 0:n]
            C.kb.op('dve', lambda e, t=t, c=c: e.scalar_tensor_tensor(out=t, in0=self.Yf[:, c, 0:n],
                                                                   scalar=sc(P, l, kG, c, w), in1=rs,
                                                                   op0=ALU.mult, op1=ALU.mult),
                    reads=[self.Yfb[c], rb, P['SC_b']], writes=[tb])
            C.kb.op('dve', lambda e, t=t, xr=xr: e.tensor_tensor(out=xr, in0=xr, in1=t, op=ALU.add),
                    reads=[tb, xrb], writes=[xrb])
            C.kb.dma('sp', dst[c * 128:(c + 1) * 128, col0:col0 + n], xr, reads=[xrb], writes=dstb)


MAXG = None
NA_PARTS = 3
STOP = 99
FULLBANK = False
SUB = 99


def groups(G, with_ctx=True, full=False):
    if MAXG is not None and not full:
        return [(g * G, G, 0) for g in range(MAXG)] + ([(NL, min(G, NCX), 1)] if with_ctx else [])
    out = []
    for g in range(NL // G):
        out.append((g * G, G, 0))
    if with_ctx:
        for g in range(NCX // min(G, NCX)):
            n = min(G, NCX)
            out.append((NL + g * n, n, 1))
    return out


def xbufs(T, key, col0, n):
    return T[key + '_b'][col0 // 128:(col0 + n) // 128]


def phase_ffn(C, P, T, l, src, dst, with_ctx, final=False):
    kb = C.kb
    C.new_phase()
    G = 256
    W1 = C.tile([8, 4096], BF16)
    W2 = C.tile([32, 1024], BF16)
    W1b, W2b = bufs(8), bufs(32)
    load_weight(C, W1, W1b, T['ffn_w1'][l], 8)
    load_weight(C, W2, W2b, T['ffn_w2'][l], 32)
    XL = [C.tile([8, G], F32) for _ in range(2)]
    XLb = bufs(2)
    H = C.tile([8, G], BF16)
    Hb = bufs(8)
    H1 = C.tile([32, G], BF16)
    H1b = bufs(32)
    Yf = C.tile([8, G], F32)
    Yfb = bufs(8)
    SQ = Small(C, 3, [2 * G], BF16)
    RS = Small(C, 2, [G], F32)
    TMP = Small(C, 4, [G], F32)
    XR = Small(C, 4, [G], F32)
    RL = Small(C, 3, [2 * G], F32)
    grp = groups(G, with_ctx)
    srcv = T[src].rearrange("(c p) t -> p c t", p=128)

    def load(i):
        col0, n, w = grp[i]
        kb.dma('sp', XL[i % 2][:, :, 0:n], srcv[:, :, col0:col0 + n], reads=xbufs(T, src, col0, n),
               writes=[XLb[i % 2]])
    load(0)
    prenorm(C, P, XL[0], XLb[0], grp[0][1], H, Hb, l, 3, 4, grp[0][2], SQ, RS, TMP)
    for i, (col0, n, w) in enumerate(grp):
        if i + 1 < len(grp):
            load(i + 1)
        for mp in range(16):
            pt, pb = C.psum(2 * n)
            for h in range(2):
                m = 2 * mp + h
                for k in range(8):
                    mm(C, pt[:, h * n:(h + 1) * n], pb, W1[:, k, m * 128:(m + 1) * 128], [W1b[k]], H[:, k, 0:n],
                       [Hb[k]], k == 0, k == 7)
            r, rb = RL.get()
            r = r[:, 0:2 * n]
            kb.op('act', lambda e, r=r, pt=pt: e.activation(out=r, in_=pt, func=AF.Relu), reads=pb, writes=[rb])
            rv = r.rearrange("p (a t) -> p a t", a=2)
            kb.op('dve', lambda e, rv=rv, mp=mp: e.tensor_tensor(out=H1[:, 2 * mp:2 * mp + 2, 0:n], in0=rv, in1=rv,
                                                                 op=ALU.mult),
                  reads=[rb], writes=[H1b[2 * mp], H1b[2 * mp + 1]])
        if i + 1 < len(grp):
            prenorm(C, P, XL[(i + 1) % 2], XLb[(i + 1) % 2], grp[i + 1][1], H, Hb, l, 3, 4, grp[i + 1][2], SQ, RS, TMP)
        pr = PostRes(C, P, n, Yf, Yfb, SQ, RS, TMP, XR)
        for mp in range(4):
            pt, pb = C.psum(2 * n)
            for h in range(2):
                m = 2 * mp + h
                for k in range(32):
                    mm(C, pt[:, h * n:(h + 1) * n], pb, W2[:, k, m * 128:(m + 1) * 128], [W2b[k]], H1[:, k, 0:n],
                       [H1b[k]], k == 0, k == 31)
            pr.pair(2 * mp, pt, pb)
        if final and w == 0:
            pr.finish(l, 5, w, T[src], T['yout'], col0, xbufs(T, src, col0, n), xbufs(T, 'yout', col0, n))
        else:
            pr.finish(l, 5, w, T[src], T[dst], col0, xbufs(T, src, col0, n), xbufs(T, dst, col0, n))


GELU_A = math.sqrt(0.044715)
GELU_B = 2.0 * math.sqrt(2.0 / math.pi)


def gelu_psum(C, zp, zpb, out, outb, GT, a=1):
    kb = C.kb
    n = zp.shape[-1]
    t, tb = GT.get()
    t = t[:, 0:n]
    if a > 1:
        t = t.rearrange("p (a t) -> p a t", a=a)
        zp = zp.rearrange("p (a t) -> p a t", a=a)
    kb.op('act', lambda e: e.activation(out=t, in_=zp, func=AF.Square, scale=GELU_A), reads=zpb, writes=[tb])
    kb.op('dve', lambda e: e.scalar_tensor_tensor(out=t, in0=t, scalar=1.0, in1=zp, op0=ALU.add, op1=ALU.mult),
          reads=zpb + [tb], writes=[tb])
    kb.op('act', lambda e: e.activation(out=t, in_=t, func=AF.Sigmoid, scale=GELU_B), reads=[tb], writes=[tb])
    kb.op('dve', lambda e: e.tensor_tensor(out=out, in0=t, in1=zp, op=ALU.mult), reads=zpb + [tb], writes=outb)


def phase_gmlp_a(C, P, T, l, j, src, with_ctx):
    kb = C.kb
    C.new_phase()
    G = 256
    Win = C.tile([8, 6144], BF16)
    Winb = bufs(8)
    load_weight(C, Win, Winb, T['gm_w_in'][j], 8)
    brow = C.tile([6144], BF16)
    browb = Buf()
    kb.dma('pool', brow[0:1, :], T['gm_b_in'][j:j + 1, :], writes=[browb])
    wsT = C.tile([8, 128], BF16)
    wsTb = Buf()
    kb.dma('pool', wsT, T['gm_wsT'][j].rearrange("q (g p) -> q g p", g=8), writes=[wsTb])
    bsbc = C.tile([8, 128], F32)
    bsbcb = Buf()
    kb.dma('sp', bsbc, T['gm_bs'][j:j + 1, :].partition_broadcast(128).rearrange("q (g p) -> q g p", g=8)
           if False else T['gm_bs_bc'][j].rearrange("q (g p) -> q g p", g=8), writes=[bsbcb])
    lng = C.tile([24], F32)
    lngb = Buf()
    kb.dma('sp', lng, T['gm_ln_gT'][j], writes=[lngb])
    XL = [C.tile([8, G], F32) for _ in range(1)]
    XLb = bufs(1)
    H = C.tile([8, G], BF16)
    Hb = bufs(8)
    U = [C.tile([24, G], BF16) for _ in range(2)]
    Ub = [bufs(24), bufs(24)]
    ZG = [C.tile([3072], F32) for _ in range(1)]
    ZGb = [bufs(6) for _ in range(1)]
    VH = [C.tile([3072], BF16) for _ in range(2)]
    VHb = bufs(2)
    MV = Small(C, 2, [8], F32)
    SQ = Small(C, 3, [G], BF16)
    RS = Small(C, 2, [G], F32)
    TMP = Small(C, 3, [G], F32)
    GT = Small(C, 3, [512], F32)
    grp = groups(G, with_ctx)
    srcv = T[src].rearrange("(c p) t -> p c t", p=128)
    pdv = T['pd'].rearrange("(c p) t -> p c t", p=128)
    vit = 0
    for i, (col0, n, w) in enumerate(grp):
        kb.dma('sp', XL[0][:, :, 0:n], srcv[:, :, col0:col0 + n], reads=xbufs(T, src, col0, n), writes=[XLb[0]])
        prenorm(C, P, XL[0], XLb[0], n, H, Hb, l, 0, 1, w, SQ, RS, TMP)
        Ui, Ubi = U[i % 2], Ub[i % 2]
        vhs = []
        for s in range(n // 128):
            zg, zgb = ZG[0], ZGb[0]
            for q in range(6):
                pt, pb = C.psum(512)
                for k in range(8):
                    mm(C, pt, pb, H[:, k, s * 128:(s + 1) * 128], [Hb[k]],
                       Win[:, k, 3072 + q * 512:3072 + (q + 1) * 512], [Winb[k]], k == 0, False)
                mm(C, pt, pb, P['one_row'][0:1, 0:128], [P['one_row_b']],
                   brow[0:1, 3072 + q * 512:3072 + (q + 1) * 512], [browb], False, True)
                gelu_psum(C, pt, pb, zg[:, q * 512:(q + 1) * 512], [zgb[q]], GT)
            mv, mvb = MV.get()
            vh, vhb = VH[vit % 2], VHb[vit % 2]
            vit += 1
            kb.op('dve', lambda e, mv=mv, zg=zg: e.reduce_sum(out=mv[:, 0:1], in_=zg, axis=mybir.AxisListType.X),
                  reads=zgb, writes=[mvb], strict=True)
            kb.op('act', lambda e, mv=mv, zg=zg, vh=vh: e.activation(out=vh, in_=zg, func=AF.Square,
                                                                   accum_out=mv[:, 1:2]),
                  reads=zgb + [mvb], writes=[mvb, vhb], strict=True)
            kb.op('dve', lambda e, mv=mv: e.tensor_scalar(out=mv[:, 2:3], in0=mv[:, 0:1], scalar1=1.0 / 3072,
                                                          scalar2=None, op0=ALU.mult), reads=[mvb], writes=[mvb], strict=True)
            kb.op('dve', lambda e, mv=mv: e.scalar_tensor_tensor(out=mv[:, 3:4], in0=mv[:, 2:3], scalar=-1.0,
                                                                 in1=mv[:, 2:3], op0=ALU.mult, op1=ALU.mult),
                  reads=[mvb], writes=[mvb], strict=True)
            kb.op('dve', lambda e, mv=mv: e.scalar_tensor_tensor(out=mv[:, 4:5], in0=mv[:, 1:2], scalar=1.0 / 3072,
                                                                 in1=mv[:, 3:4], op0=ALU.mult, op1=ALU.add),
                  reads=[mvb], writes=[mvb], strict=True)
            kb.op('act', lambda e, mv=mv: e.activation(out=mv[:, 5:6], in_=mv[:, 4:5], func=AF.Sqrt,
                                                       bias=P['eps'][:, 0:1]), reads=[mvb, P['eps_b']], writes=[mvb], strict=True)
            kb.op('dve', lambda e, mv=mv: e.reciprocal(out=mv[:, 5:6], in_=mv[:, 5:6]), reads=[mvb], writes=[mvb], strict=True)
            kb.op('dve', lambda e, mv=mv: e.scalar_tensor_tensor(out=mv[:, 6:7], in0=mv[:, 2:3], scalar=-1.0,
                                                                 in1=mv[:, 5:6], op0=ALU.mult, op1=ALU.mult),
                  reads=[mvb], writes=[mvb], strict=True)
            kb.op('act', lambda e, vh=vh, zg=zg, mv=mv: e.activation(out=vh, in_=zg, func=AF.Identity,
                                                                   scale=mv[:, 5:6], bias=mv[:, 6:7]),
                  reads=zgb + [mvb], writes=[vhb])
            vhs.append((vh, vhb))
            if 'dbg_vh' in T and w == 1 and s == 1:
                kb.dma('sp', T['dbg_vh'][:, :], vh, reads=[vhb], writes=[Buf()])
                kb.dma('sp', T['dbg_mv'][:, :], mv, reads=[mvb], writes=[Buf()])
                kb.dma('sp', T['dbg_zg'][:, :], zg, reads=zgb, writes=[Buf()])
        for mp in range(12):
            pt, pb = C.psum(2 * n)
            for h in range(2):
                m = 2 * mp + h
                po = pt[:, h * n:(h + 1) * n]
                for k in range(8):
                    mm(C, po, pb, Win[:, k, m * 128:(m + 1) * 128], [Winb[k]], H[:, k, 0:n], [Hb[k]], k == 0, False)
                mm(C, po, pb, brow[0:1, m * 128:(m + 1) * 128], [browb], P['one_row'][0:1, 0:n], [P['one_row_b']],
                   False, True)
            gelu_psum(C, pt, pb, Ui[:, 2 * mp:2 * mp + 2, 0:n], [Ubi[2 * mp], Ubi[2 * mp + 1]], GT, a=2)
        for s in range(n // 128):
            vh, vhb = vhs[s]
            for c4 in range(6):
                pt, pb = C.psum(512)
                for jj in range(4):
                    cc = c4 * 4 + jj
                    mm(C, pt[:, jj * 128:(jj + 1) * 128], pb, vh[:, cc * 128:(cc + 1) * 128], [vhb],
                       wsT[:, cc // 3, :], [wsTb], True, True)
                t, tb = GT.get()
                for jj in range(4):
                    cc = c4 * 4 + jj
                    kb.op('dve', lambda e, t=t, pt=pt, cc=cc, jj=jj: e.scalar_tensor_tensor(
                        out=t[:, jj * 128:(jj + 1) * 128], in0=pt[:, jj * 128:(jj + 1) * 128],
                        scalar=lng[:, cc:cc + 1], in1=bsbc[:, cc // 3, :], op0=ALU.mult, op1=ALU.add),
                        reads=pb + [lngb, bsbcb], writes=[tb])
                tv = t.rearrange("p (a t) -> p a t", a=4)
                kb.op('pool', lambda e, tv=tv, c4=c4, s=s, Ui=Ui: e.tensor_tensor(
                    out=Ui[:, c4 * 4:c4 * 4 + 4, s * 128:(s + 1) * 128],
                    in0=Ui[:, c4 * 4:c4 * 4 + 4, s * 128:(s + 1) * 128], in1=tv, op=ALU.mult),
                    reads=[tb] + Ubi[c4 * 4:c4 * 4 + 4], writes=Ubi[c4 * 4:c4 * 4 + 4])
        kb.dma('sp', pdv[:, :, col0:col0 + n], Ui[:, :, 0:n], reads=Ubi, writes=xbufs(T, 'pd', col0, n))


def phase_gmlp_b(C, P, T, l, j, src, dst, with_ctx):
    kb = C.kb
    C.new_phase()
    G = 256
    Wo = C.tile([24, 1024], BF16)
    Wob = bufs(24)
    load_weight(C, Wo, Wob, T['gm_w_out'][j], 24)
    PT = [C.tile([24, G], BF16) for _ in range(2)]
    PTb = bufs(2)
    Yf = C.tile([8, G], F32)
    Yfb = bufs(8)
    SQ = Small(C, 3, [2 * G], BF16)
    RS = Small(C, 2, [G], F32)
    TMP = Small(C, 4, [G], F32)
    XR = Small(C, 4, [G], F32)
    grp = groups(G, with_ctx)
    pdv = T['pd'].rearrange("(c p) t -> p c t", p=128)

    def load(i):
        col0, n, w = grp[i]
        kb.dma('sp', PT[i % 2][:, :, 0:n], pdv[:, :, col0:col0 + n], reads=xbufs(T, 'pd', col0, n),
               writes=[PTb[i % 2]])
    load(0)
    for i, (col0, n, w) in enumerate(grp):
        if i + 1 < len(grp):
            load(i + 1)
        pr = PostRes(C, P, n, Yf, Yfb, SQ, RS, TMP, XR)
        for mp in range(4):
            pt, pb = C.psum(2 * n)
            for h in range(2):
                m = 2 * mp + h
                for k in range(24):
                    mm(C, pt[:, h * n:(h + 1) * n], pb, Wo[:, k, m * 128:(m + 1) * 128], [Wob[k]],
                       PT[i % 2][:, k, 0:n], [PTb[i % 2]], k == 0, k == 23)
            pr.pair(2 * mp, pt, pb)
        pr.finish(l, 2, w, T[src], T[dst], col0, xbufs(T, src, col0, n), xbufs(T, dst, col0, n))


def phase_na_a(C, P, T, l, src):
    kb = C.kb
    C.new_phase()
    G = 256
    W = C.tile([8, 3072], BF16)
    Wb = bufs(8)
    load_weight(C, W, Wb, T['na_w_qkv'][0], 8)
    bqk = C.tile([24], F32)
    bqkb = Buf()
    kb.dma('sp', bqk, T['na_bqkvT'][:, :], writes=[bqkb])
    bq8 = C.tile([8], F32)
    kb.op('dve', lambda e: e.tensor_scalar(out=bq8, in0=bqk[:, 0:8], scalar1=0.125, scalar2=None, op0=ALU.mult),
          reads=[bqkb], writes=[bqkb], strict=True)
    brow = C.tile([1024], BF16)
    browb = Buf()
    kb.dma('pool', brow[0:1, :], T['na_b_qkv'][0:1, 2048:3072], writes=[browb])
    XL = [C.tile([8, G], F32) for _ in range(2)]
    XLb = bufs(2)
    H = C.tile([8, G], BF16)
    Hb = bufs(8)
    QT = [C.tile([8, G], BF16) for _ in range(2)]
    QTb = bufs(2)
    KT = [C.tile([8, G], BF16) for _ in range(2)]
    KTb = bufs(2)
    Vt = [C.tile([16, 65], BF16) for _ in range(3)]
    Vtb = bufs(3)
    for i in range(3):
        kb.op('pool', lambda e, i=i: e.memset(Vt[i], 1.0), writes=[Vtb[i]])
    SQ = Small(C, 3, [G], BF16)
    RS = Small(C, 2, [G], F32)
    TMP = Small(C, 3, [G], F32)
    grp = groups(G, True, full=True)
    srcv = T[src].rearrange("(c p) t -> p c t", p=128)
    qv = T['qT'].rearrange("(c p) t -> p c t", p=128)
    kv = T['kT'].rearrange("(c p) t -> p c t", p=128)

    def load(i):
        col0, n, w = grp[i]
        kb.dma('sp', XL[i % 2][:, :, 0:n], srcv[:, :, col0:col0 + n], reads=xbufs(T, src, col0, n),
               writes=[XLb[i % 2]])
    load(0)
    vi = 0
    for i, (col0, n, w) in enumerate(grp):
        if i + 1 < len(grp):
            load(i + 1)
        prenorm(C, P, XL[i % 2], XLb[i % 2], n, H, Hb, l, 0, 1, w, SQ, RS, TMP)
        for which, dstT, dstb in ((0, QT[i % 2], QTb[i % 2]), (1, KT[i % 2], KTb[i % 2])):
            for mp in range(4):
                pt, pb = C.psum(2 * n)
                for h in range(2):
                    m = 2 * mp + h
                    mcol = which * 1024 + m * 128
                    for k in range(8):
                        mm(C, pt[:, h * n:(h + 1) * n], pb, W[:, k, mcol:mcol + 128], [Wb[k]], H[:, k, 0:n], [Hb[k]],
                           k == 0, k == 7)
                for h in range(2):
                    m = 2 * mp + h
                    if which == 0:
                        kb.op('act', lambda e, pt=pt, h=h, m=m, dstT=dstT: e.activation(
                            out=dstT[:, m, 0:n], in_=pt[:, h * n:(h + 1) * n], func=AF.Identity, scale=0.125,
                            bias=bq8[:, m:m + 1]), reads=pb + [bqkb], writes=[dstb])
                    else:
                        kb.op('act', lambda e, pt=pt, h=h, m=m, dstT=dstT: e.activation(
                            out=dstT[:, m, 0:n], in_=pt[:, h * n:(h + 1) * n], func=AF.Identity,
                            bias=bqk[:, 8 + m:9 + m]), reads=pb + [bqkb], writes=[dstb])
        kb.dma('sp', qv[:, :, col0:col0 + n], QT[i % 2][:, :, 0:n], reads=[QTb[i % 2]], writes=xbufs(T, 'qT', col0, n))
        kb.dma('sp', kv[:, :, col0:col0 + n], KT[i % 2][:, :, 0:n], reads=[KTb[i % 2]], writes=xbufs(T, 'kT', col0, n))
        for s in range(n // 128):
            vt, vtb = Vt[vi % 3], Vtb[vi % 3]
            vi += 1
            for nq in range(2):
                pt, pb = C.psum(512)
                for k in range(8):
                    mm(C, pt, pb, H[:, k, s * 128:(s + 1) * 128], [Hb[k]],
                       W[:, k, 2048 + nq * 512:2048 + (nq + 1) * 512], [Wb[k]], k == 0, False)
                mm(C, pt, pb, P['one_row'][0:1, 0:128], [P['one_row_b']], brow[0:1, nq * 512:(nq + 1) * 512], [browb],
                   False, True)
                kb.op('act', lambda e, pt=pt, vt=vt, nq=nq: e.activation(
                    out=vt[:, nq * 8:(nq + 1) * 8, 0:64], in_=pt.rearrange("p (h d) -> p h d", h=8), func=AF.Identity),
                    reads=pb, writes=[vtb])
            t0 = col0 + s * 128
            kb.dma('sp', T['vD'][t0:t0 + 128, :], vt.rearrange("p h d -> p (h d)"), reads=[vtb],
                   writes=xbufs(T, 'vD', t0, 128))


NA_NEG = -30000.0


def na_bias_table(rpb):
    rpb = np.asarray(rpb, dtype=np.float32)
    qc = np.arange(64)[None, :]
    kc = np.arange(64)[:, None]
    ws = np.clip(qc - 8, 0, 48)
    valid = (kc >= ws) & (kc < ws + 16)
    dcol = np.clip(kc - qc, -15, 15) + 15
    tab = np.full((2, 64, 16, 16, 64), NA_NEG, dtype=np.float32)

    def fill(half, pid, d):
        vals = rpb[:, d, :][:, dcol]
        vals = np.where(valid[None], vals, NA_NEG)
        tab[half, :, pid, :, :] = vals.transpose(1, 0, 2)
    for d in range(14):
        fill(0, d, d)
        fill(1, d, d + 1)
    fill(1, 14, 3)
    fill(0, 15, 10)
    return np.ascontiguousarray(tab.reshape(128, 16 * 16 * 64))


def phase_na_b(C, P, T, l, src, dst):
    kb = C.kb
    C.new_phase()
    Wo = C.tile([8, 1024], BF16)
    Wob = bufs(8)
    load_weight(C, Wo, Wob, T['na_w_o'][0], 8)
    bo = C.tile([8], F32)
    bob = Buf()
    kb.dma('sp', bo, T['na_b_oT'][:, :], writes=[bob])
    BT = C.tile([16, 16, 64], BF16)
    BTb = bufs(16)
    btv = T['na_bt'].rearrange("p (i x) -> p i x", i=16)
    BTf = BT.rearrange("p i h q -> p i (h q)")
    for i in range(16):
        kb.dma('pool', BTf[:, i, :], btv[:, i, :], writes=[BTb[i]])
    KcT = C.tile([8, 256], BF16)
    Vc = C.tile([2, 1040], BF16)
    Kcb, Vcb = Buf(), Buf()
    kvd = T['kT'].rearrange("(c p) t -> p c t", p=128)
    qvd = T['qT'].rearrange("(c p) t -> p c t", p=128)
    kb.dma('sp', KcT, kvd[:, :, NL:NT], reads=xbufs(T, 'kT', NL, NCX), writes=[Kcb])
    kb.dma('sp', Vc, T['vD'][NL:NT, :].rearrange("(j p) c -> p j c", p=128), reads=xbufs(T, 'vD', NL, NCX),
           writes=[Vcb])
    KT = [C.tile([8, 1024], BF16) for _ in range(2)]
    KTb = bufs(2)
    Vw = [C.tile([8, 1040], BF16) for _ in range(2)]
    Vwb = bufs(2)
    QT = [C.tile([8, 512], BF16) for _ in range(2)]
    QTb = bufs(2)
    PTs = Small(C, 3, [448], BF16)
    Ot = Small(C, 2, [1024], BF16)
    RD = Small(C, 3, [8], F32)
    OT = [C.tile([8, 256], BF16) for _ in range(2)]
    OTb = bufs(2)
    Yf = C.tile([8, 256], F32)
    Yfb = bufs(8)
    SQ = Small(C, 3, [512], BF16)
    RS = Small(C, 2, [256], F32)
    TMP = Small(C, 4, [256], F32)
    XR = Small(C, 4, [256], F32)
    S_BANKS = [0, 1, 2]
    PV_BANKS = [3, 4, 5]
    C.default_banks = [6, 7]
    ident = P['ident']
    psT_all = C.ps.bitcast(BF16)

    def attend_block(qt, qtb, qcol, lat_tiles, ot, otb):
        nt = len(lat_tiles)
        ntot = nt + 2
        hgroups = [list(range(0, 7)), list(range(7, 14)), list(range(14, 16))]
        pend = None

        def do_pv(pt, ptb, po, pvb, h):
            for t, (ktf, ktb, bid, vf, vb) in enumerate(lat_tiles):
                mm(C, po, pvb, pt[:, t * 64:(t + 1) * 64], [ptb], vf(h), [vb], t == 0, False)
            for c in range(2):
                mm(C, po, pvb, pt[:, (nt + c) * 64:(nt + c + 1) * 64], [ptb], Vc[:, c, h * 65:(h + 1) * 65], [Vcb],
                   (nt == 0 and c == 0), c == 1)

        def do_norm(pv, pvb, hg):
            nh = len(hg)
            rd, rdb = RD.get()
            pv3 = pv[0:64, :].rearrange("p (h d) -> p h d", d=65)
            kb.op('dve', lambda e: e.reciprocal(out=rd[0:64, 0:nh], in_=pv3[:, :, 64]), reads=pvb, writes=[rdb])
            h0 = hg[0]
            kb.op('dve', lambda e: e.tensor_tensor(
                out=ot[0:64, h0 * 64:(h0 + nh) * 64].rearrange("p (h d) -> p h d", d=64), in0=pv3[:, :, 0:64],
                in1=rd[0:64, 0:nh].unsqueeze(2).broadcast_to([64, nh, 64]), op=ALU.mult),
                reads=pvb + [rdb], writes=[otb], strict=True)

        for hg in hgroups:
            pv, pvb = C.psum(len(hg) * 65, banks=PV_BANKS)
            for hh, h in enumerate(hg):
                hp, ch = h % 2, h // 2
                q_ap = qt[hp * 64:(hp + 1) * 64, ch, qcol:qcol + 64]
                st, stb = C.psum(ntot * 64, banks=S_BANKS)
                for t, (ktf, ktb, bid, vf, vb) in enumerate(lat_tiles):
                    mm(C, st[:, t * 64:(t + 1) * 64], stb, ktf(h), [ktb], q_ap, [qtb], True, False)
                    mm(C, st[:, t * 64:(t + 1) * 64], stb, ident, [P['ident_b']], BT[:, bid, h, :], [BTb[bid]],
                       False, True)
                for c in range(2):
                    mm(C, st[:, (nt + c) * 64:(nt + c + 1) * 64], stb,
                       KcT[hp * 64:(hp + 1) * 64, ch, c * 128:(c + 1) * 128], [Kcb], q_ap, [qtb], True, True)
                pt, ptb = PTs.get()
                pt = pt[:, 0:ntot * 64]
                kb.op('act', lambda e, pt=pt, st=st: e.activation(out=pt, in_=st, func=AF.Exp), reads=stb, writes=[ptb])
                if pend is not None:
                    do_pv(*pend[0])
                    if pend[1] is not None:
                        do_norm(*pend[1])
                last_in_group = hh == len(hg) - 1
                pend = ((pt, ptb, pv[0:64, hh * 65:(hh + 1) * 65], pvb, h), (pv, pvb, hg) if last_in_group else None)
        do_pv(*pend[0])
        do_norm(*pend[1])

    def transpose_block(ot, otb, OTt, OTtb, slot):
        pt, pb = C.psum(256)
        bank = int(pb[0].name[2:])
        ptb16 = psT_all[:, bank * 1024:bank * 1024 + 512]
        for c in range(8):
            C.kb.op('pe', lambda e, c=c: e.transpose(ptb16[:, c * 64:(c + 1) * 64], ot[0:64, c * 128:(c + 1) * 128],
                                                     ident[0:64, 0:64]),
                    reads=[otb, P['ident_b']], writes=pb)
        kb.op('act', lambda e: e.activation(out=OTt[:, :, slot * 64:(slot + 1) * 64],
                                            in_=ptb16.rearrange("p (c t) -> p c t", c=8), func=AF.Identity),
              reads=pb, writes=[OTtb])

    def project(OTt, OTtb, n, w, col0):
        pr = PostRes(C, P, n, Yf, Yfb, SQ, RS, TMP, XR)
        for mp in range(4):
            pt, pb = C.psum(2 * n, banks=PV_BANKS + S_BANKS)
            for h in range(2):
                m = 2 * mp + h
                for k in range(8):
                    mm(C, pt[:, h * n:(h + 1) * n], pb, Wo[:, k, m * 128:(m + 1) * 128], [Wob[k]], OTt[:, k, 0:n],
                       [OTtb], k == 0, k == 7)
            pr.pair(2 * mp, pt, pb, bias=bo, bias_b=bob)
        pr.finish(l, 2, w, T[src], T[dst], col0, xbufs(T, src, col0, n), xbufs(T, dst, col0, n))

    ngroups = 16 if MAXG is None else MAXG
    oti = 0
    for g in range(ngroups):
        lo = max(0, 8 * g - 4)
        hi = min(128, 8 * g + 12)
        kt, ktb = KT[g % 2], KTb[g % 2]
        vw, vwb = Vw[g % 2], Vwb[g % 2]
        qt, qtb = QT[g % 2], QTb[g % 2]
        nw = (hi - lo) * 64
        kb.dma('sp', kt[:, :, 0:nw], kvd[:, :, lo * 64:hi * 64], reads=xbufs(T, 'kT', lo * 64, nw), writes=[ktb])
        kb.dma('sp', vw[:, 0:nw // 128, :], T['vD'][lo * 64:hi * 64, :].rearrange("(j p) c -> p j c", p=128),
               reads=xbufs(T, 'vD', lo * 64, nw), writes=[vwb])
        kb.dma('sp', qt, qvd[:, :, g * 512:(g + 1) * 512], reads=xbufs(T, 'qT', g * 512, 512), writes=[qtb])
        for rl in range(8):
            r = 8 * g + rl
            rs = min(max(r - 4, 0), 120)
            d0 = rs - r + 7
            tiles = []
            if rs % 2 == 0:
                for i4 in range(4):
                    tiles.append(((rs - lo) // 2 + i4, d0 + 2 * i4))
            else:
                assert d0 == 3
                j0 = (rs - 1 - lo) // 2
                tiles = [(j0, 14), (j0 + 1, 4), (j0 + 2, 6), (j0 + 3, 8), (j0 + 4, 15)]
            lat = []
            for (j, bid) in tiles:
                lat.append((lambda h, j=j: kt[(h % 2) * 64:(h % 2 + 1) * 64, h // 2, j * 128:(j + 1) * 128], ktb, bid,
                            lambda h, j=j: vw[:, j, h * 65:(h + 1) * 65], vwb))
            ot, otb = Ot.get()
            attend_block(qt, qtb, rl * 64, lat, ot, otb)
            OTt, OTtb = OT[oti % 2], OTb[oti % 2]
            transpose_block(ot, otb, OTt, OTtb, rl % 4)
            if rl % 4 == 3:
                project(OTt, OTtb, 256, 0, g * 512 + (rl // 4) * 256)
                oti += 1
    qt, qtb = QT[0], QTb[0]
    kb.dma('sp', qt[:, :, 0:256], qvd[:, :, NL:NT], reads=xbufs(T, 'qT', NL, NCX), writes=[qtb])
    OTt, OTtb = OT[oti % 2], OTb[oti % 2]
    for blk in range(4):
        ot, otb = Ot.get()
        attend_block(qt, qtb, blk * 64, [], ot, otb)
        transpose_block(ot, otb, OTt, OTtb, blk)
    project(OTt, OTtb, 256, 1, NL)


def ml_rope_tables():
    inv = 10000.0 ** (-np.arange(0, 32, 2, dtype=np.float64) / 32.0)
    p = np.arange(128)
    d = p % 64
    axis = d // 32
    i = d % 16
    half = (d % 32) // 16
    t = np.arange(NL)
    pos = np.where(axis[:, None] == 0, (t // 64)[None, :], (t % 64)[None, :]).astype(np.float64)
    ang = pos * inv[i][:, None]
    Cc = np.ones((128, NT), np.float64)
    Ss = np.zeros((128, NT), np.float64)
    Cc[:, :NL] = np.cos(ang)
    Ss[:, :NL] = np.sin(ang) * np.where(half[:, None] == 0, -1.0, 1.0)
    tabs = np.stack([Cc, Ss, Cc * 0.125, Ss * 0.125], axis=0).astype(np.float32)
    col = np.arange(1024)
    dd = col % 64
    perm = np.where((dd % 32) < 16, col + 16, col - 16)
    return np.ascontiguousarray(tabs), perm


def phase_ml_a(C, P, T, l, src):
    kb = C.kb
    C.new_phase()
    G = 256
    W = C.tile([8, 3072], BF16)
    Wb = bufs(8)
    load_weight(C, W, Wb, T['ml_w_in'][0], 8)
    Wsw = C.tile([8, 1024], BF16)
    Wswb = bufs(8)
    load_weight(C, Wsw, Wswb, T['ml_w_qk_sw'], 8)
    Wg = C.tile([8, 32], BF16)
    Wgb = bufs(8)
    load_weight(C, Wg, Wgb, T['ml_w_gate2'], 8)
    bg = C.tile([32], BF16)
    bgb = Buf()
    kb.dma('pool', bg[0:1, :], T['ml_b_gate2'][0:1, :], writes=[bgb])
    XL = [C.tile([8, G], F32) for _ in range(2)]
    XLb = bufs(2)
    H = C.tile([8, G], BF16)
    Hb = bufs(8)
    QK = [C.tile([8, G], BF16) for _ in range(2)]
    QKb = bufs(2)
    OT = [C.tile([8, G], BF16) for _ in range(2)]
    OTb = bufs(2)
    TB = [C.tile([4, G], F32) for _ in range(2)]
    TBb = bufs(2)
    Vt = [C.tile([8, 129], BF16) for _ in range(3)]
    Vtb = bufs(3)
    for i in range(3):
        kb.op('pool', lambda e, i=i: e.memset(Vt[i], 1.0), writes=[Vtb[i]])
    Kt = [C.tile([512], BF16) for _ in range(2)]
    Ktb = bufs(2)
    Gt = [C.tile([32], F32) for _ in range(2)]
    Gtb = bufs(2)
    GE = Small(C, 2, [32], F32)
    SQ = Small(C, 3, [G], BF16)
    RS = Small(C, 2, [G], F32)
    TMP = Small(C, 4, [G], F32)
    grp = groups(G, True, full=True)
    srcv = T[src].rearrange("(c p) t -> p c t", p=128)
    qkv = T['mqk'].rearrange("(c p) t -> p c t", p=128)
    ov = T['moT'].rearrange("(c p) t -> p c t", p=128)
    tabv = T['ml_rope'].rearrange("a p t -> p a t")
    psT_all = C.ps.bitcast(BF16)
    ident = P['ident']

    def load(i):
        col0, n, w = grp[i]
        kb.dma('sp', XL[i % 2][:, :, 0:n], srcv[:, :, col0:col0 + n], reads=xbufs(T, src, col0, n),
               writes=[XLb[i % 2]])
        kb.dma('sp', TB[i % 2][:, :, 0:n], tabv[:, :, col0:col0 + n], writes=[TBb[i % 2]])
    load(0)
    vi = 0
    for i, (col0, n, w) in enumerate(grp):
        if i + 1 < len(grp):
            load(i + 1)
        prenorm(C, P, XL[i % 2], XLb[i % 2], n, H, Hb, l, 0, 1, w, SQ, RS, TMP)
        qk, qkb = QK[i % 2], QKb[i % 2]
        tb, tbb = TB[i % 2], TBb[i % 2]
        for m in range(8):
            pt, pb = C.psum(2 * n)
            for k in range(8):
                mm(C, pt[:, 0:n], pb, W[:, k, m * 128:(m + 1) * 128], [Wb[k]], H[:, k, 0:n], [Hb[k]], k == 0, k == 7)
            for k in range(8):
                mm(C, pt[:, n:2 * n], pb, Wsw[:, k, m * 128:(m + 1) * 128], [Wswb[k]], H[:, k, 0:n], [Hb[k]],
                   k == 0, k == 7)
            ti = 0 if m < 4 else 2
            t1, t1b = TMP.get()
            t2, t2b = TMP.get()
            kb.op('dve', lambda e, t1=t1, pt=pt, ti=ti, tb=tb: e.tensor_tensor(out=t1[:, 0:n], in0=pt[:, 0:n],
                                                                             in1=tb[:, ti, 0:n], op=ALU.mult),
                  reads=pb + [tbb], writes=[t1b])
            kb.op('dve', lambda e, t2=t2, pt=pt, ti=ti, tb=tb: e.tensor_tensor(out=t2[:, 0:n], in0=pt[:, n:2 * n],
                                                                             in1=tb[:, ti + 1, 0:n], op=ALU.mult),
                  reads=pb + [tbb], writes=[t2b])
            kb.op('pool', lambda e, t1=t1, t2=t2, m=m, qk=qk: e.tensor_tensor(out=qk[:, m, 0:n], in0=t1[:, 0:n],
                                                                            in1=t2[:, 0:n], op=ALU.add),
                  reads=[t1b, t2b], writes=[qkb])
        kb.dma('sp', qkv[:, :, col0:col0 + n], qk[:, :, 0:n], reads=[qkb], writes=xbufs(T, 'mqk', col0, n))
        ot, otb = OT[i % 2], OTb[i % 2]
        for mp in range(4):
            pt, pb = C.psum(2 * n)
            for h in range(2):
                m = 2 * mp + h
                for k in range(8):
                    mm(C, pt[:, h * n:(h + 1) * n], pb, W[:, k, 2048 + m * 128:2048 + (m + 1) * 128], [Wb[k]],
                       H[:, k, 0:n], [Hb[k]], k == 0, k == 7)
            kb.op('act', lambda e, pt=pt, mp=mp, ot=ot: e.activation(
                out=ot[:, 2 * mp:2 * mp + 2, 0:n], in_=pt.rearrange("p (a t) -> p a t", a=2), func=AF.Sigmoid),
                reads=pb, writes=[otb])
        kb.dma('sp', ov[:, :, col0:col0 + n], ot[:, :, 0:n], reads=[otb], writes=xbufs(T, 'moT', col0, n))
        for s in range(n // 128):
            t0 = col0 + s * 128
            vt, vtb = Vt[vi % 3], Vtb[vi % 3]
            for nq in range(2):
                pt, pb = C.psum(512)
                for k in range(8):
                    mm(C, pt, pb, H[:, k, s * 128:(s + 1) * 128], [Hb[k]],
                       W[:, k, 1024 + nq * 512:1024 + (nq + 1) * 512], [Wb[k]], k == 0, k == 7)
                kb.op('act', lambda e, pt=pt, vt=vt, nq=nq: e.activation(
                    out=vt[:, nq * 4:(nq + 1) * 4, 0:128], in_=pt.rearrange("p (h d) -> p h d", h=4),
                    func=AF.Identity), reads=pb, writes=[vtb])
            kb.dma('sp', T['mvD'][t0:t0 + 128, :], vt.rearrange("p h d -> p (h d)"), reads=[vtb],
                   writes=xbufs(T, 'mvD', t0, 128))
            kt, ktb = Kt[vi % 2], Ktb[vi % 2]
            pt, pb = C.psum(256)
            bank = int(pb[0].name[2:])
            ptb16 = psT_all[:, bank * 1024:bank * 1024 + 512]
            for c in range(4):
                kb.op('pe', lambda e, c=c, ptb16=ptb16, qk=qk, s=s: e.transpose(
                    ptb16[:, c * 128:(c + 1) * 128], qk[:, 4 + c, s * 128:(s + 1) * 128], ident),
                    reads=[qkb, P['ident_b']], writes=pb)
            kb.op('act', lambda e, kt=kt, ptb16=ptb16: e.activation(out=kt, in_=ptb16, func=AF.Identity), reads=pb,
                  writes=[ktb])
            kb.dma('sp', T['mkD'][t0:t0 + 128, :], kt, reads=[ktb], writes=xbufs(T, 'mkD', t0, 128))
            gt, gtb = Gt[vi % 2], Gtb[vi % 2]
            pt, pb = C.psum(32)
            for k in range(8):
                mm(C, pt, pb, H[:, k, s * 128:(s + 1) * 128], [Hb[k]], Wg[:, k, :], [Wgb[k]], k == 0, False)
            mm(C, pt, pb, P['one_row'][0:1, 0:128], [P['one_row_b']], bg[0:1, :], [bgb], False, True)
            ge, geb = GE.get()
            p4 = pt.rearrange("p (a b h) -> p a b h", a=2, b=2)
            g4 = gt.rearrange("p (a b h) -> p a b h", a=2, b=2)
            e4 = ge.rearrange("p (a b h) -> p a b h", a=2, b=2)
            kb.op('act', lambda e, p4=p4, e4=e4: e.activation(out=e4[:, :, 1, :], in_=p4[:, :, 1, :], func=AF.Exp,
                                                             scale=-1.0), reads=pb, writes=[geb])
            kb.op('act', lambda e, e4=e4: e.activation(out=e4[:, :, 1, :], in_=e4[:, :, 1, :], func=AF.Ln, bias=1.0),
                  reads=[geb], writes=[geb], strict=True)
            kb.op('dve', lambda e, g4=g4, e4=e4: e.tensor_scalar(out=g4[:, :, 1, :], in0=e4[:, :, 1, :], scalar1=-1.0,
                                                                scalar2=None, op0=ALU.mult), reads=[geb], writes=[gtb])
            kb.op('dve', lambda e, g4=g4, p4=p4: e.tensor_copy(out=g4[:, :, 0, :], in_=p4[:, :, 0, :]), reads=pb,
                  writes=[gtb])
            kb.dma('sp', T['mgD'][t0:t0 + 128, :], gt, reads=[gtb], writes=xbufs(T, 'mgD', t0, 128))
            vi += 1


def phase_ml_b(C, P, T):
    kb = C.kb
    C.new_phase()
    NEG = -30000.0
    cA = [C.tile([128], F32) for _ in range(2)]
    cB = [C.tile([128], F32) for _ in range(2)]
    cM = [C.tile([128], F32) for _ in range(2)]
    cI = C.tile([128], F32)
    cO = C.tile([128], F32)
    onesb = C.tile([128], BF16)
    cb = Buf()
    for t_, val in ((cA[0], 1.0), (cA[1], 1.0), (cB[0], 1.0), (cB[1], 1.0), (cM[0], 0.0), (cM[1], 0.0), (cI, 0.0),
                    (cO, 1.0)):
        kb.op('pool', lambda e, t_=t_, val=val: e.memset(t_, val), writes=[cb])
    kb.op('pool', lambda e: e.memset(onesb, 1.0), writes=[cb])

    def asel(t_, cmp, fill, base=0, cm=1, pat=-1):
        kb.op('pool', lambda e: e.affine_select(out=t_, in_=t_, pattern=[[pat, 128]], compare_op=cmp, fill=fill,
                                                base=base, channel_multiplier=cm), reads=[cb], writes=[cb],
              strict=True)
    asel(cA[0], ALU.is_gt, 0.0, base=0, cm=1, pat=-1)
    asel(cA[1], ALU.is_gt, 0.0, base=0, cm=-1, pat=1)
    asel(cB[0], ALU.is_gt, 0.0, base=1, cm=-1, pat=1)
    asel(cB[1], ALU.is_gt, 0.0, base=1, cm=1, pat=-1)
    asel(cM[0], ALU.is_gt, NEG, base=1, cm=-1, pat=1)
    asel(cM[1], ALU.is_gt, NEG, base=1, cm=1, pat=-1)
    asel(cI, ALU.not_equal, 1.0)
    NB = 2

    def alloc_dir():
        St = C.tile([8, 129], F32)
        Cbf = C.tile([8, 128], BF16)
        Nbc = C.tile([8, 128], BF16)
        Stb, Cbfb, Nbcb = Buf(), Buf(), Buf()
        NB = 2
        Q = [C.tile([8, 128], BF16) for _ in range(NB)]
        K_ = [C.tile([8, 128], BF16) for _ in range(NB)]
        KT = [C.tile([8, 64], BF16) for _ in range(NB)]
        V = [C.tile([8, 129], BF16) for _ in range(NB)]
        GA = [C.tile([32], F32) for _ in range(NB)]
        Qb, Kb_, KTb, Vb, GAb = bufs(NB), bufs(NB), bufs(NB), bufs(NB), bufs(NB)
        R = C.tile([8, 128], F32)
        R2 = C.tile([8, 128], F32)
        Rb, R2b = Buf(), Buf()
        E = C.tile([8, 128], F32)
        EB = C.tile([8, 128], F32)
        Eb, EBb = Buf(), Buf()
        AT = C.tile([8, 128], BF16)
        ATb = Buf()
        KW = C.tile([8, 64], BF16)
        KWb = Buf()
        SM = Small(C, 2, [32], F32)
        NUM = C.tile([8, 128], F32)
        DEN = C.tile([8, 128], F32)
        NUMb, DENb = Buf(), Buf()
        HO = [C.tile([8, 128], F32) for _ in range(2)]
        HOb = bufs(2)
        return dict(St=St, Cbf=Cbf, Nbc=Nbc, Stb=Stb, Cbfb=Cbfb, Nbcb=Nbcb, Q=Q, K_=K_, KT=KT, V=V, GA=GA, Qb=Qb, Kb_=Kb_, KTb=KTb, Vb=Vb, GAb=GAb, R=R, R2=R2, Rb=Rb, R2b=R2b, E=E, EB=EB, Eb=Eb, EBb=EBb, AT=AT, ATb=ATb, KW=KW, KWb=KWb, SM=SM, NUM=NUM, DEN=DEN, NUMb=NUMb, DENb=DENb, HO=HO, HOb=HOb, it=0, hidx=0)
    SD = [alloc_dir(), alloc_dir()]
    mq = T['mqk']
    ntl = 64 if MAXG is None else MAXG
    seqs = [[64, 65] + list(range(ntl)), [65, 64] + list(range(ntl - 1, -1, -1))]
    for dr in range(2):
        sd = SD[dr]
        for t_ in (sd['St'], sd['Cbf'], sd['Nbc']):
            kb.op('pool', lambda e, t_=t_: e.memset(t_, 0.0), reads=[], writes=[sd['Stb'], sd['Cbfb'], sd['Nbcb']])

    def tile_step(dr, ti):
        sd = SD[dr]
        St = sd['St']
        Cbf = sd['Cbf']
        Nbc = sd['Nbc']
        Stb = sd['Stb']
        Cbfb = sd['Cbfb']
        Nbcb = sd['Nbcb']
        Q = sd['Q']
        K_ = sd['K_']
        KT = sd['KT']
        V = sd['V']
        GA = sd['GA']
        Qb = sd['Qb']
        Kb_ = sd['Kb_']
        KTb = sd['KTb']
        Vb = sd['Vb']
        GAb = sd['GAb']
        R = sd['R']
        R2 = sd['R2']
        Rb = sd['Rb']
        R2b = sd['R2b']
        E = sd['E']
        EB = sd['EB']
        Eb = sd['Eb']
        EBb = sd['EBb']
        AT = sd['AT']
        ATb = sd['ATb']
        KW = sd['KW']
        KWb = sd['KWb']
        SM = sd['SM']
        NUM = sd['NUM']
        DEN = sd['DEN']
        NUMb = sd['NUMb']
        DENb = sd['DENb']
        HO = sd['HO']
        HOb = sd['HOb']
        hD = T['mhf'] if dr == 0 else T['mhb']
        hkey = 'mhf' if dr == 0 else 'mhb'
        t0 = ti * 128
        b = sd['it'] % NB
        sd['it'] += 1
        q, k_, kt, v, ga = Q[b], K_[b], KT[b], V[b], GA[b]
        kb.dma('sp', q[0:64], mq[0:512, t0:t0 + 128].rearrange("(h d) t -> d h t", d=64),
               reads=xbufs(T, 'mqk', t0, 128), writes=[Qb[b]])
        kb.dma('sp', k_[0:64], mq[512:1024, t0:t0 + 128].rearrange("(h d) t -> d h t", d=64),
               reads=xbufs(T, 'mqk', t0, 128), writes=[Kb_[b]])
        kb.dma('sp', kt, T['mkD'][t0:t0 + 128, :].rearrange("p (h d) -> p h d", h=8),
               reads=xbufs(T, 'mkD', t0, 128), writes=[KTb[b]])
        kb.dma('sp', v, T['mvD'][t0:t0 + 128, :].rearrange("p (h d) -> p h d", h=8),
               reads=xbufs(T, 'mvD', t0, 128), writes=[Vb[b]])
        kb.dma('sp', ga, T['mgD'][t0:t0 + 128, :], reads=xbufs(T, 'mgD', t0, 128), writes=[GAb[b]])
        ig = ga[:, dr * 16:dr * 16 + 8]
        lf = ga[:, dr * 16 + 8:dr * 16 + 16]
        kb.op('dve', lambda e, lf=lf, dr=dr: e.tensor_tensor(out=R, in0=cB[dr].unsqueeze(1).broadcast_to([128, 8, 128]),
                                                      in1=lf.unsqueeze(2).broadcast_to([128, 8, 128]),
                                                      op=ALU.mult), reads=[cb, GAb[b]], writes=[Rb])
        kb.op('pool', lambda e, ig=ig, dr=dr: e.tensor_tensor(out=R2, in0=cM[dr].unsqueeze(1).broadcast_to([128, 8, 128]),
                                                       in1=ig.unsqueeze(2).broadcast_to([128, 8, 128]),
                                                       op=ALU.add), reads=[cb, GAb[b]], writes=[R2b])
        psm, psmb = C.psum(16)
        mm(C, psm[:, 0:8], psmb, cA[dr], [cb], lf, [GAb[b]], True, True)
        mm(C, psm[:, 8:16], psmb, cO, [cb], lf, [GAb[b]], True, True)
        sm, smb = SM.get()
        kb.op('dve', lambda e, sm=sm, psm=psm, ig=ig: e.tensor_tensor(out=sm[:, 0:8], in0=psm[:, 0:8], in1=ig,
                                                                     op=ALU.add), reads=psmb + [GAb[b]],
              writes=[smb])
        kb.op('act', lambda e, sm=sm: e.activation(out=sm[:, 0:8], in_=sm[:, 0:8], func=AF.Exp), reads=[smb],
              writes=[smb])
        kb.op('act', lambda e, sm=sm, psm=psm: e.activation(out=sm[:, 8:16], in_=psm[:, 8:16], func=AF.Exp),
              reads=psmb, writes=[smb], strict=True)
        yield
        pE, pB, pS = [], [], []
        for hf in range(2):
            pe_, peb = C.psum(512)
            Rv = R[:, hf * 4:(hf + 1) * 4, :].rearrange("p h t -> p (h t)")
            R2v = R2[:, hf * 4:(hf + 1) * 4, :].rearrange("p h t -> p (h t)")
            mm(C, pe_, peb, cA[dr], [cb], Rv, [Rb], True, False)
            mm(C, pe_, peb, cI, [cb], R2v, [R2b], False, True)
            kb.op('act', lambda e, pe_=pe_, hf=hf: e.activation(
                out=E[:, hf * 4:(hf + 1) * 4, :].rearrange("p h t -> p (h t)"), in_=pe_, func=AF.Exp),
                reads=peb, writes=[Eb])
            pb_, pbb = C.psum(512)
            mm(C, pb_, pbb, cO, [cb], Rv, [Rb], True, True)
            kb.op('act', lambda e, pb_=pb_, hf=hf: e.activation(
                out=EB[:, hf * 4:(hf + 1) * 4, :].rearrange("p h t -> p (h t)"), in_=pb_, func=AF.Exp),
                reads=pbb, writes=[EBb])
        yield
        for hf in range(2):
            ps_, psb_ = C.psum(512)
            for hh in range(4):
                h = hf * 4 + hh
                mm(C, ps_[:, hh * 128:(hh + 1) * 128], psb_, k_[0:64, h, :], [Kb_[b]], q[0:64, h, :], [Qb[b]],
                   True, True)
            kb.op('dve', lambda e, ps_=ps_, hf=hf: e.tensor_tensor(
                out=AT[:, hf * 4:(hf + 1) * 4, :].rearrange("p h t -> p (h t)"), in0=ps_,
                in1=E[:, hf * 4:(hf + 1) * 4, :].rearrange("p h t -> p (h t)"), op=ALU.mult),
                reads=psb_ + [Eb], writes=[ATb])
        yield
        ho, hob = HO[sd['hidx'] % 2], HOb[sd['hidx'] % 2]
        sd['hidx'] += 1
        for hf in range(2):
            hs = slice(hf * 4, (hf + 1) * 4)
            pn, pnb = C.psum(512)
            for hh in range(4):
                h = hf * 4 + hh
                mm(C, pn[:, hh * 128:(hh + 1) * 128], pnb, v[:, h, 0:128], [Vb[b]], AT[:, h, :], [ATb], True, True)
            pd_, pdb = C.psum(512)
            mm(C, pd_, pdb, onesb, [cb], AT[:, hs, :].rearrange("p h t -> p (h t)"), [ATb], True, True)
            pi, pib = C.psum(512)
            for hh in range(4):
                h = hf * 4 + hh
                mm(C, pi[:, hh * 128:(hh + 1) * 128], pib, Cbf[0:64, h, :], [Cbfb], q[0:64, h, :], [Qb[b]],
                   True, True)
            pq, pqb = C.psum(512)
            for hh in range(4):
                h = hf * 4 + hh
                mm(C, pq[:, hh * 128:(hh + 1) * 128], pqb, Nbc[0:64, h, :], [Nbcb], q[0:64, h, :], [Qb[b]],
                   True, True)
            yield
            ebv = EB[:, hs, :].rearrange("p h t -> p (h t)")
            numv = NUM[:, hs, :].rearrange("p h t -> p (h t)")
            denv = DEN[:, hs, :].rearrange("p h t -> p (h t)")
            hov = ho[:, hs, :].rearrange("p h t -> p (h t)")
            kb.op('dve', lambda e, pi=pi, ebv=ebv, numv=numv: e.tensor_tensor(out=numv, in0=pi, in1=ebv,
                                                                             op=ALU.mult),
                  reads=pib + [EBb], writes=[NUMb])
            kb.op('dve', lambda e, pn=pn, numv=numv: e.tensor_tensor(out=numv, in0=pn, in1=numv, op=ALU.add),
                  reads=pnb + [NUMb], writes=[NUMb])
            kb.op('dve', lambda e, pq=pq, ebv=ebv, denv=denv: e.tensor_tensor(out=denv, in0=pq, in1=ebv,
                                                                             op=ALU.mult),
                  reads=pqb + [EBb], writes=[DENb])
            kb.op('dve', lambda e, pd_=pd_, denv=denv: e.tensor_tensor(out=denv, in0=pd_, in1=denv, op=ALU.add),
                  reads=pdb + [DENb], writes=[DENb])
            kb.op('act', lambda e, denv=denv: e.activation(out=denv, in_=denv, func=AF.Abs), reads=[DENb],
                  writes=[DENb])
            kb.op('dve', lambda e, denv=denv: e.tensor_scalar(out=denv, in0=denv, scalar1=1.0, scalar2=None,
                                                              op0=ALU.max), reads=[DENb], writes=[DENb])
            kb.op('dve', lambda e, denv=denv: e.reciprocal(out=denv, in_=denv), reads=[DENb], writes=[DENb])
            kb.op('pool', lambda e, denv=denv, numv=numv, hov=hov: e.tensor_tensor(out=hov, in0=numv, in1=denv,
                                                                                  op=ALU.mult),
                  reads=[DENb, NUMb], writes=[hob])
        yield
        kb.dma('sp', hD[:, t0:t0 + 128].rearrange("(h p) t -> p h t", p=128), ho, reads=[hob],
               writes=xbufs(T, hkey, t0, 128))
        sm_w = sm[:, 0:8]
        kb.op('dve', lambda e, sm_w=sm_w, kt=kt: e.tensor_tensor(
            out=KW, in0=kt, in1=sm_w.unsqueeze(2).broadcast_to([128, 8, 64]), op=ALU.mult),
            reads=[KTb[b], smb], writes=[KWb])
        for (h0, nh) in ((0, 3), (3, 3), (6, 2)):
            pu, pub = C.psum(nh * 129)
            for hh in range(nh):
                h = h0 + hh
                mm(C, pu[0:64, hh * 129:(hh + 1) * 129], pub, KW[:, h, :], [KWb], v[:, h, :], [Vb[b]], True, True)
            stv = St[0:64, h0:h0 + nh, :]
            kb.op('dve', lambda e, stv=stv, sm=sm, h0=h0, nh=nh: e.tensor_tensor(
                out=stv, in0=stv, in1=sm[0:64, 8 + h0:8 + h0 + nh].unsqueeze(2).broadcast_to([64, nh, 129]),
                op=ALU.mult), reads=[Stb, smb], writes=[Stb], strict=True)
            kb.op('dve', lambda e, stv=stv, pu=pu, nh=nh: e.tensor_tensor(
                out=stv, in0=pu[0:64, :].rearrange("p (h d) -> p h d", h=nh), in1=stv, op=ALU.add),
                reads=pub + [Stb], writes=[Stb], strict=True)
        kb.op('act', lambda e: e.activation(out=Cbf[0:64], in_=St[0:64, :, 0:128], func=AF.Identity),
              reads=[Stb], writes=[Cbfb])
        kb.op('pool', lambda e: e.tensor_copy(out=Nbc[0:64], in_=St[0:64, :, 128:129].broadcast_to([64, 8, 128])),
              reads=[Stb], writes=[Nbcb])

    for step in range(len(seqs[0])):
        alive = [tile_step(dr, seqs[dr][step]) for dr in range(2)]
        while alive:
            for g in list(alive):
                try:
                    next(g)
                except StopIteration:
                    alive.remove(g)


def phase_ml_c(C, P, T, l, src, dst):
    kb = C.kb
    C.new_phase()
    G = 256
    Wo = C.tile([8, 1024], BF16)
    Wob = bufs(8)
    load_weight(C, Wo, Wob, T['ml_w_out'][0], 8)
    ng = C.tile([8], F32)
    ngb = Buf()
    kb.dma('sp', ng, T['ml_norm_gT'][:, :], writes=[ngb])
    ones128 = C.tile([128], BF16)
    o1b = Buf()
    kb.op('pool', lambda e: e.memset(ones128, 1.0 / 128), writes=[o1b])
    HF = [C.tile([8, G], F32) for _ in range(2)]
    HB = [C.tile([8, G], F32) for _ in range(2)]
    OG = [C.tile([8, G], BF16) for _ in range(2)]
    HFb, HBb, OGb = bufs(2), bufs(2), bufs(2)
    Hm = C.tile([8, G], BF16)
    Hmb = bufs(8)
    Yf = C.tile([8, G], F32)
    Yfb = bufs(8)
    SQ = Small(C, 3, [2 * G], BF16)
    RS = Small(C, 3, [G], F32)
    TMP = Small(C, 4, [G], F32)
    XR = Small(C, 4, [G], F32)
    grp = groups(G, True)
    hfv = T['mhf'].rearrange("(c p) t -> p c t", p=128)
    hbv = T['mhb'].rearrange("(c p) t -> p c t", p=128)
    ov = T['moT'].rearrange("(c p) t -> p c t", p=128)

    def load(i):
        col0, n, w = grp[i]
        kb.dma('sp', HF[i % 2][:, :, 0:n], hfv[:, :, col0:col0 + n], reads=xbufs(T, 'mhf', col0, n),
               writes=[HFb[i % 2]])
        kb.dma('sp', HB[i % 2][:, :, 0:n], hbv[:, :, col0:col0 + n], reads=xbufs(T, 'mhb', col0, n),
               writes=[HBb[i % 2]])
        kb.dma('sp', OG[i % 2][:, :, 0:n], ov[:, :, col0:col0 + n], reads=xbufs(T, 'moT', col0, n),
               writes=[OGb[i % 2]])
    load(0)
    for i, (col0, n, w) in enumerate(grp):
        if i + 1 < len(grp):
            load(i + 1)
        hf, hb, og = HF[i % 2], HB[i % 2], OG[i % 2]
        kb.op('pool', lambda e, hf=hf, hb=hb: e.tensor_tensor(out=hf[:, :, 0:n], in0=hf[:, :, 0:n], in1=hb[:, :, 0:n],
                                                             op=ALU.add), reads=[HFb[i % 2], HBb[i % 2]],
              writes=[HFb[i % 2]])
        for c in range(8):
            s_, sb_ = SQ.get()
            kb.op('act', lambda e, s_=s_, c=c, hf=hf: e.activation(out=s_[:, 0:n], in_=hf[:, c, 0:n], func=AF.Square),
                  reads=[HFb[i % 2]], writes=[sb_])
            pst, pb = C.psum(n)
            mm(C, pst, pb, ones128, [o1b], s_[:, 0:n], [sb_], True, True)
            rs, rb = rstd_from_psum(C, P, pst, pb, RS)
            t, tb = TMP.get()
            kb.op('dve', lambda e, t=t, c=c, hf=hf, rs=rs: e.scalar_tensor_tensor(
                out=t[:, 0:n], in0=hf[:, c, 0:n], scalar=ng[:, c:c + 1], in1=rs, op0=ALU.mult, op1=ALU.mult),
                reads=[HFb[i % 2], rb, ngb], writes=[tb])
            kb.op('pool', lambda e, t=t, c=c, og=og: e.tensor_tensor(out=Hm[:, c, 0:n], in0=t[:, 0:n],
                                                                    in1=og[:, c, 0:n], op=ALU.mult),
                  reads=[tb, OGb[i % 2]], writes=[Hmb[c]])
        pr = PostRes(C, P, n, Yf, Yfb, SQ, RS, TMP, XR)
        for mp in range(4):
            pt, pb = C.psum(2 * n)
            for h in range(2):
                m = 2 * mp + h
                for k in range(8):
                    mm(C, pt[:, h * n:(h + 1) * n], pb, Wo[:, k, m * 128:(m + 1) * 128], [Wob[k]], Hm[:, k, 0:n],
                       [Hmb[k]], k == 0, k == 7)
            pr.pair(2 * mp, pt, pb)
        pr.finish(l, 2, w, T[src], T[dst], col0, xbufs(T, src, col0, n), xbufs(T, dst, col0, n))


def build_program(plan, dbg=False):
    nc = bass.Bass("TRN2", target_bir_lowering=False)
    es = ExitStack()
    T = {}

    def dram(name, shape, dt, kind):
        T[name] = nc.dram_tensor(name, shape, dt, kind=kind).ap()
        return T[name]

    dram('xin', [D, NT], F32, "ExternalInput")
    dram('cvec', [128, 8, 2], F32, "ExternalInput")
    dram('ada_w', [4, D, 6 * D], F32, "ExternalInput")
    dram('ada_bT', [128, 4, 48], F32, "ExternalInput")
    dram('norm_gT', [128, 4, 4, 8], F32, "ExternalInput")
    dram('ffn_w1', [4, D, 4 * D], F32, "ExternalInput")
    dram('ffn_w2', [4, 4 * D, D], F32, "ExternalInput")
    dram('gm_w_in', [2, D, 6 * D], F32, "ExternalInput")
    dram('gm_b_in', [2, 6 * D], F32, "ExternalInput")
    dram('gm_ln_gT', [2, 128, 24], F32, "ExternalInput")
    dram('gm_wsT', [2, 128, 8 * 128], F32, "ExternalInput")
    dram('gm_bs_bc', [2, 128, 8 * 128], F32, "ExternalInput")
    dram('gm_w_out', [2, 3 * D, D], F32, "ExternalInput")
    dram('na_w_qkv', [1, D, 3 * D], F32, "ExternalInput")
    dram('na_b_qkv', [1, 3 * D], F32, "ExternalInput")
    dram('na_bqkvT', [128, 24], F32, "ExternalInput")
    dram('na_bt', [128, 16 * 16 * 64], F32, "ExternalInput")
    dram('na_w_o', [1, D, D], F32, "ExternalInput")
    dram('na_b_oT', [128, 8], F32, "ExternalInput")
    dram('qT', [D, NT], BF16, "Internal")
    dram('kT', [D, NT], BF16, "Internal")
    dram('vD', [NT, 1040], BF16, "Internal")
    dram('ml_w_in', [1, D, 3 * D], F32, "ExternalInput")
    dram('ml_w_qk_sw', [D, D], F32, "ExternalInput")
    dram('ml_w_gate2', [D, 32], F32, "ExternalInput")
    dram('ml_b_gate2', [1, 32], F32, "ExternalInput")
    dram('ml_rope', [4, 128, NT], F32, "ExternalInput")
    dram('ml_norm_gT', [128, 8], F32, "ExternalInput")
    dram('ml_w_out', [1, D, D], F32, "ExternalInput")
    dram('mqk', [D, NT], BF16, "Internal")
    dram('moT', [D, NT], BF16, "Internal")
    dram('mvD', [NT, 8 * 129], BF16, "Internal")
    dram('mkD', [NT, 512], BF16, "Internal")
    dram('mgD', [NT, 32], F32, "Internal")
    dram('mhf', [D, NT], F32, "ExternalOutput" if dbg else "Internal")
    dram('mhb', [D, NT], F32, "ExternalOutput" if dbg else "Internal")
    dram('yout', [D, NL], F32, "ExternalOutput")
    dram('xs', [D, NT], F32, "ExternalOutput" if dbg else "Internal")
    dram('pd', [3 * D, NT], BF16, "ExternalOutput" if dbg else "Internal")
    if dbg:
        dram('dbg_vh', [128, 3072], BF16, "ExternalOutput")
        dram('dbg_mv', [128, 8], F32, "ExternalOutput")
        dram('dbg_zg', [128, 3072], F32, "ExternalOutput")
    for k in ['xin', 'xs', 'pd', 'yout', 'qT', 'kT', 'vD', 'mqk', 'moT', 'mvD', 'mkD', 'mgD', 'mhf', 'mhb']:
        T[k + '_b'] = bufs(NT // 128, k)
    C = Ctx(nc, es)
    P = phase_consts(C, T)
    for ph in plan:
        kind = ph[0]
        if kind == 'ffn':
            _, l, src, dst, with_ctx, final = ph
            phase_ffn(C, P, T, l, src, dst, with_ctx, final)
        elif kind == 'gmlp':
            _, l, j, src, dst, with_ctx = ph
            phase_gmlp_a(C, P, T, l, j, src, with_ctx)
            phase_gmlp_b(C, P, T, l, j, src, dst, with_ctx)
        elif kind == 'na':
            _, l, src, dst = ph
            if NA_PARTS & 1:
                phase_na_a(C, P, T, l, src)
            if NA_PARTS & 2:
                phase_na_b(C, P, T, l, src, dst)
        elif kind == 'ml':
            _, l, src, dst = ph
            phase_ml_a(C, P, T, l, src)
            phase_ml_b(C, P, T)
            phase_ml_c(C, P, T, l, src, dst)
        else:
            raise ValueError(kind)
    C.kb.barrier()
    C.kb.emit(es)
    es.close()
    return nc, C


def host_shared(inp):
    f = np.float32
    m = {}
    m['ada_w'] = inp['ada_w']
    m['ada_bT'] = np.ascontiguousarray(inp['ada_b'].reshape(4, 48, 128).transpose(2, 0, 1), dtype=f)
    m['norm_gT'] = np.ascontiguousarray(inp['norm_g'].reshape(4, 4, 8, 128).transpose(3, 0, 1, 2), dtype=f)
    m['ffn_w1'] = inp['ffn_w1']
    m['ffn_w2'] = inp['ffn_w2']
    m['gm_w_in'] = inp['gm_w_in']
    m['gm_b_in'] = inp['gm_b_in']
    m['gm_w_out'] = inp['gm_w_out']
    m['gm_ln_gT'] = np.ascontiguousarray(inp['gm_ln_g'].reshape(2, 24, 128).transpose(0, 2, 1), dtype=f)
    m['gm_wsT'] = np.ascontiguousarray(inp['gm_ws'].transpose(0, 3, 1, 2).reshape(2, 128, 1024), dtype=f)
    m['na_w_qkv'] = inp['na_w_qkv']
    m['na_b_qkv'] = inp['na_b_qkv']
    m['na_bqkvT'] = np.ascontiguousarray(inp['na_b_qkv'][0].reshape(24, 128).T, dtype=f)
    m['na_bt'] = na_bias_table(inp['na_rpb'][0])
    m['na_w_o'] = inp['na_w_o']
    m['na_b_oT'] = np.ascontiguousarray(inp['na_b_o'][0].reshape(8, 128).T, dtype=f)
    tabs, perm = ml_rope_tables()
    m['ml_w_in'] = inp['ml_w_in']
    m['ml_w_qk_sw'] = np.ascontiguousarray(inp['ml_w_in'][0][:, :1024][:, perm], dtype=f)
    wg = inp['ml_w_gate'][0]
    m['ml_w_gate2'] = np.ascontiguousarray(np.concatenate([wg[0], wg[1]], axis=1), dtype=f)
    m['ml_b_gate2'] = np.ascontiguousarray(inp['ml_b_gate'][0].reshape(1, 32), dtype=f)
    m['ml_rope'] = tabs
    m['ml_norm_gT'] = np.ascontiguousarray(inp['ml_norm_g'][0].reshape(8, 128).T, dtype=f)
    m['ml_w_out'] = inp['ml_w_out']
    m['gm_bs_bc'] = np.ascontiguousarray(np.broadcast_to(inp['gm_bs'].reshape(2, 1, 1024), (2, 128, 1024)), dtype=f)
    return m


def host_inputs(inp, b, shared=None):
    f = np.float32
    m = dict(shared if shared is not None else host_shared(inp))
    m['xin'] = np.ascontiguousarray(np.concatenate([inp['x'][b].T, inp['ctx'][b].T], axis=1), dtype=f)
    cv = np.stack([inp['c'][b], inp['c_ctx']], axis=-1)
    m['cvec'] = np.ascontiguousarray(cv.reshape(8, 128, 2).transpose(1, 0, 2), dtype=f)
    return m


FULL_PLAN = [
    ('gmlp', 0, 0, 'xin', 'xs', True), ('ffn', 0, 'xs', 'xs', True, False),
    ('na', 1, 'xs', 'xs'), ('ffn', 1, 'xs', 'xs', True, False),
    ('ml', 2, 'xs', 'xs'), ('ffn', 2, 'xs', 'xs', True, False),
    ('gmlp', 3, 1, 'xs', 'xs', False), ('ffn', 3, 'xs', 'xs', False, True),
]


def kernel(**inputs):
    inp = {k: np.asarray(v) for k, v in inputs.items()}
    nc, C = build_program(FULL_PLAN)
    shared = host_shared(inp)
    in_maps = [host_inputs(inp, b, shared) for b in range(8)]
    res = run_bass_kernel_spmd(nc, in_maps, core_ids=list(range(8)))
    out = np.stack([np.ascontiguousarray(np.asarray(res.results[b]['yout']).T) for b in range(8)], axis=0)
    return out.astype(np.float32)
```
